# Optimizing a Trainium2 kernel written in Bass

```python
import jax
import jax.numpy as jnp
from jax import lax
import numpy as np


D_MODEL = 1024
BATCH = 16
SEQ = 2048
DEPTH = 4

HEAD_DIM = 64
ATTN_Q_HEADS = 8
ATTN_KV_HEADS = 2
ATTN_GROUP = ATTN_Q_HEADS // ATTN_KV_HEADS
ATTN_WIDTH = ATTN_Q_HEADS * HEAD_DIM
ATTN_KV_WIDTH = ATTN_KV_HEADS * HEAD_DIM
WINDOW = 128
ATTN_BLOCK = 128
RWKV_HEADS = 8
RWKV_WIDTH = RWKV_HEADS * HEAD_DIM
DECAY_LORA = 64
ICLR_LORA = 64
GATE_LORA = 128
RWKV_SIZES = (RWKV_WIDTH, RWKV_WIDTH, RWKV_WIDTH, DECAY_LORA, ICLR_LORA, GATE_LORA)
RWKV_COLS = sum(RWKV_SIZES)
RWKV_LN_EPS = 64e-5
RET_HEADS = 8
RET_WIDTH = RET_HEADS * HEAD_DIM
RET_CHUNK = 128
ROPE_BASE = 10000.0
N_BRANCH = 3
IN_SIZES = (ATTN_WIDTH, ATTN_KV_WIDTH, ATTN_KV_WIDTH, RWKV_COLS,
            RET_WIDTH, RET_WIDTH, RET_WIDTH, RET_WIDTH, N_BRANCH * D_MODEL)
IN_WIDTH = sum(IN_SIZES)
D_FF = -((-8 * D_MODEL) // (3 * 256)) * 256
NORM_EPS = 1e-6

kernel_name = 'hybrid_swa_rwkv7_retention_block'


def _split(t, sizes):
    out, start = [], 0
    for s in sizes:
        out.append(t[..., start:start + s])
        start += s
    return out


def _rms(x, eps=NORM_EPS):
    xf = x.astype(jnp.float32)
    return (xf * lax.rsqrt(jnp.mean(xf * xf, axis=-1, keepdims=True) + eps)).astype(x.dtype)


def sliding_window_attention(q, k, v, sinks):
    B, T = q.shape[0], q.shape[1]
    C = ATTN_BLOCK
    NB = T // C
    qb = q.reshape(B, NB, C, ATTN_KV_HEADS, ATTN_GROUP, HEAD_DIM)

    def band(t):
        tb = t.reshape(B, NB, C, ATTN_KV_HEADS, HEAD_DIM)
        prev = jnp.concatenate([jnp.zeros_like(tb[:, :1]), tb[:, :-1]], axis=1)
        return jnp.concatenate([prev, tb], axis=2)

    kb, vb = band(k), band(v)
    s = jnp.einsum('bnqhgd,bnshd->bnhgqs', qb, kb).astype(jnp.float32) * (HEAD_DIM ** -0.5)
    qi = jnp.arange(C)[:, None]
    si = jnp.arange(2 * C)[None, :]
    rel = qi + C - si
    key_pos = jnp.arange(NB)[:, None, None] * C + si[None] - C
    mask = (rel >= 0) & (rel < WINDOW) & (key_pos >= 0)
    s = jnp.where(mask[None, :, None, None], s, -jnp.inf)
    sink = sinks.astype(jnp.float32).reshape(ATTN_KV_HEADS, ATTN_GROUP)[None, None, :, :, None, None]
    m = jnp.maximum(jnp.max(s, axis=-1, keepdims=True), sink)
    p = jnp.exp(s - m)
    p = p / (jnp.sum(p, axis=-1, keepdims=True) + jnp.exp(sink - m))
    o = jnp.einsum('bnhgqs,bnshd->bnqhgd', p.astype(v.dtype), vb)
    return o.reshape(B, T, ATTN_WIDTH)


def rwkv7_time_mix(h, shift_mu, w0, w2, a0, a2, g2, k_k, k_a, r_k, lnx_g, lnx_b):
    B, T = h.shape[0], h.shape[1]
    H, N = RWKV_HEADS, HEAD_DIM
    h_prev = jnp.pad(h, ((0, 0), (1, 0), (0, 0)))[:, :-1]
    z = h + shift_mu * (h_prev - h)
    r, k, v, wd, ad, gd = _split(z, RWKV_SIZES)
    w_log = -jax.nn.softplus(-(w0 + jnp.tanh(wd) @ w2)) - 0.5
    decay = jnp.exp(-jnp.exp(w_log.astype(jnp.float32)))
    a = jax.nn.sigmoid(a0 + ad @ a2)
    g = jax.nn.sigmoid(gd) @ g2
    kk = (k * k_k).reshape(B, T, H, N).astype(jnp.float32)
    kk = kk / jnp.maximum(jnp.linalg.norm(kk, axis=-1, keepdims=True), 1e-12)
    k = k * (1.0 + (a - 1.0) * k_a)

    def heads(t):
        return t.reshape(B, T, H, N).astype(jnp.float32)

    r_h, k_h, v_h, a_h, w_h = heads(r), heads(k), heads(v), heads(a), heads(decay)
    xs = tuple(t.transpose(1, 0, 2, 3) for t in (r_h, w_h, k_h, v_h, -kk, kk * a_h))

    def step(S, inp):
        r_t, w_t, k_t, v_t, a_t, b_t = inp
        Sa = jnp.einsum('bhvk,bhk->bhv', S, a_t)
        S = S * w_t[:, :, None, :] + Sa[..., None] * b_t[:, :, None, :] + v_t[..., None] * k_t[:, :, None, :]
        return S, jnp.einsum('bhvk,bhk->bhv', S, r_t)

    S0 = jnp.zeros((B, H, N, N), jnp.float32)
    _, y = lax.scan(step, S0, xs)
    y = y.transpose(1, 0, 2, 3)
    mu = jnp.mean(y, axis=-1, keepdims=True)
    var = jnp.mean(jnp.square(y - mu), axis=-1, keepdims=True)
    y = ((y - mu) * lax.rsqrt(var + RWKV_LN_EPS)).reshape(B, T, RWKV_WIDTH)
    y = y * lnx_g.astype(jnp.float32) + lnx_b.astype(jnp.float32)
    bonus = jnp.sum(r_h * k_h * r_k.astype(jnp.float32), axis=-1, keepdims=True) * v_h
    y = (y + bonus.reshape(B, T, RWKV_WIDTH)) * g.astype(jnp.float32)
    return y.astype(h.dtype)


def _rotary(x, pos):
    half = HEAD_DIM // 2
    inv_freq = 1.0 / (ROPE_BASE ** (jnp.arange(half, dtype=jnp.float32) * 2.0 / HEAD_DIM))
    ang = pos.astype(jnp.float32)[:, None] * inv_freq[None, :]
    cos, sin = jnp.cos(ang)[:, None, :], jnp.sin(ang)[:, None, :]
    x1, x2 = x[..., :half], x[..., half:]
    return jnp.concatenate([x1 * cos - x2 * sin, x2 * cos + x1 * sin], axis=-1)


def retention(q, k, v, gate):
    B, T = q.shape[0], q.shape[1]
    H, d, C = RET_HEADS, HEAD_DIM, RET_CHUNK
    NC = T // C
    pos = jnp.arange(T)
    qh = _rotary(q.reshape(B, T, H, d).astype(jnp.float32), pos)
    kh = _rotary(k.reshape(B, T, H, d).astype(jnp.float32), pos) * (d ** -0.5)
    vh = v.reshape(B, T, H, d).astype(jnp.float32)
    log_gamma = jnp.log1p(-jnp.power(2.0, -5.0 - jnp.arange(H, dtype=jnp.float32)))
    idx = jnp.arange(C, dtype=jnp.float32)
    diff = idx[:, None] - idx[None, :]
    dmat = jnp.where(diff >= 0, jnp.exp(log_gamma[:, None, None] * jnp.maximum(diff, 0.0)), 0.0)
    xi = jnp.exp(log_gamma[:, None] * (idx[None, :] + 1.0))
    zeta = jnp.exp(log_gamma[:, None] * (C - 1.0 - idx[None, :]))
    chunk_decay = jnp.exp(log_gamma * C)
    qc = qh.reshape(B, NC, C, H, d)
    kc = kh.reshape(B, NC, C, H, d)
    vc = vh.reshape(B, NC, C, H, d)
    s = jnp.einsum('bnihd,bnjhd->bnhij', qc, kc) * dmat
    inner = jnp.einsum('bnhij,bnjhe->bnihe', s, vc)
    kv = jnp.einsum('bnjhd,bnjhe,hj->nbhde', kc, vc, zeta)

    def step(R, kv_n):
        return R * chunk_decay[None, :, None, None] + kv_n, R

    _, r_prev = lax.scan(step, jnp.zeros((B, H, d, d), jnp.float32), kv)
    cross = jnp.einsum('bnihd,nbhde,hi->bnihe', qc, r_prev, xi)
    o = _rms((inner + cross).reshape(B, T, H, d)).reshape(B, T, RET_WIDTH)
    o = o * jax.nn.silu(gate.astype(jnp.float32))
    return o.astype(q.dtype)


def hybrid_layer(x, norm1_g, w_in, attn_q_norm_g, attn_k_norm_g, attn_sinks, w_attn_o,
                 rwkv_shift_mu, rwkv_w0, rwkv_w2, rwkv_a0, rwkv_a2, rwkv_g2, rwkv_k_k, rwkv_k_a,
                 rwkv_r_k, rwkv_lnx_g, rwkv_lnx_b, w_rwkv_o, w_ret_o, w_out,
                 norm2_g, w_ffn_gate, w_ffn_up, w_ffn_down):
    B, T, D = x.shape
    h = _rms(x) * norm1_g
    proj = h @ w_in
    aq, ak, av, rw, rq, rk, rv, rg, gates = _split(proj, IN_SIZES)
    aq = _rms(aq.reshape(B, T, ATTN_Q_HEADS, HEAD_DIM)) * attn_q_norm_g
    ak = _rms(ak.reshape(B, T, ATTN_KV_HEADS, HEAD_DIM)) * attn_k_norm_g
    av = av.reshape(B, T, ATTN_KV_HEADS, HEAD_DIM)
    o_a = sliding_window_attention(aq, ak, av, attn_sinks) @ w_attn_o
    o_b = rwkv7_time_mix(rw, rwkv_shift_mu, rwkv_w0, rwkv_w2, rwkv_a0, rwkv_a2, rwkv_g2,
                         rwkv_k_k, rwkv_k_a, rwkv_r_k, rwkv_lnx_g, rwkv_lnx_b) @ w_rwkv_o
    o_c = retention(rq, rk, rv, rg) @ w_ret_o
    g = jax.nn.sigmoid(gates).reshape(B, T, N_BRANCH, D)
    mixed = g[:, :, 0] * o_a + g[:, :, 1] * o_b + g[:, :, 2] * o_c
    x = x + mixed @ w_out
    h2 = _rms(x) * norm2_g
    x = x + (jax.nn.silu(h2 @ w_ffn_gate) * (h2 @ w_ffn_up)) @ w_ffn_down
    return x


def setup_inputs(seed: int = 0) -> dict:
    key = jax.random.key(seed)
    ks = jax.random.split(key, 25)
    L = DEPTH

    def nrm(k, shape, scale):
        return jax.random.normal(k, shape, jnp.float32) * scale

    return {
        'x': nrm(ks[0], (BATCH, SEQ, D_MODEL), 1.0),
        'norm1_g': 1.0 + nrm(ks[1], (L, D_MODEL), 0.02),
        'w_in': nrm(ks[2], (L, D_MODEL, IN_WIDTH), D_MODEL ** -0.5),
        'attn_q_norm_g': 1.0 + nrm(ks[3], (L, HEAD_DIM), 0.02),
        'attn_k_norm_g': 1.0 + nrm(ks[4], (L, HEAD_DIM), 0.02),
        'attn_sinks': nrm(ks[5], (L, ATTN_Q_HEADS), 0.5),
        'w_attn_o': nrm(ks[6], (L, ATTN_WIDTH, D_MODEL), ATTN_WIDTH ** -0.5),
        'rwkv_shift_mu': jax.random.uniform(ks[7], (L, RWKV_COLS), jnp.float32),
        'rwkv_w0': jax.random.uniform(ks[8], (L, RWKV_WIDTH), jnp.float32, minval=-6.0, maxval=1.0),
        'rwkv_w2': nrm(ks[9], (L, DECAY_LORA, RWKV_WIDTH), 0.1 * DECAY_LORA ** -0.5),
        'rwkv_a0': nrm(ks[10], (L, RWKV_WIDTH), 0.1),
        'rwkv_a2': nrm(ks[11], (L, ICLR_LORA, RWKV_WIDTH), ICLR_LORA ** -0.5),
        'rwkv_g2': nrm(ks[12], (L, GATE_LORA, RWKV_WIDTH), GATE_LORA ** -0.5),
        'rwkv_k_k': 0.85 + nrm(ks[13], (L, RWKV_WIDTH), 0.02),
        'rwkv_k_a': 1.0 + nrm(ks[14], (L, RWKV_WIDTH), 0.02),
        'rwkv_r_k': nrm(ks[15], (L, RWKV_HEADS, HEAD_DIM), 0.1),
        'rwkv_lnx_g': 1.0 + nrm(ks[16], (L, RWKV_WIDTH), 0.02),
        'rwkv_lnx_b': nrm(ks[17], (L, RWKV_WIDTH), 0.02),
        'w_rwkv_o': nrm(ks[18], (L, RWKV_WIDTH, D_MODEL), RWKV_WIDTH ** -0.5),
        'w_ret_o': nrm(ks[19], (L, RET_WIDTH, D_MODEL), RET_WIDTH ** -0.5),
        'w_out': nrm(ks[20], (L, D_MODEL, D_MODEL), D_MODEL ** -0.5),
        'norm2_g': 1.0 + nrm(ks[21], (L, D_MODEL), 0.02),
        'w_ffn_gate': nrm(ks[22], (L, D_MODEL, D_FF), D_MODEL ** -0.5),
        'w_ffn_up': nrm(ks[23], (L, D_MODEL, D_FF), D_MODEL ** -0.5),
        'w_ffn_down': nrm(ks[24], (L, D_FF, D_MODEL), D_FF ** -0.5),
    }


def reference(x, norm1_g, w_in, attn_q_norm_g, attn_k_norm_g, attn_sinks, w_attn_o,
              rwkv_shift_mu, rwkv_w0, rwkv_w2, rwkv_a0, rwkv_a2, rwkv_g2, rwkv_k_k, rwkv_k_a,
              rwkv_r_k, rwkv_lnx_g, rwkv_lnx_b, w_rwkv_o, w_ret_o, w_out,
              norm2_g, w_ffn_gate, w_ffn_up, w_ffn_down):
    for l in range(DEPTH):
        x = hybrid_layer(x, norm1_g[l], w_in[l], attn_q_norm_g[l], attn_k_norm_g[l], attn_sinks[l],
                         w_attn_o[l], rwkv_shift_mu[l], rwkv_w0[l], rwkv_w2[l], rwkv_a0[l], rwkv_a2[l],
                         rwkv_g2[l], rwkv_k_k[l], rwkv_k_a[l], rwkv_r_k[l], rwkv_lnx_g[l], rwkv_lnx_b[l],
                         w_rwkv_o[l], w_ret_o[l], w_out[l], norm2_g[l], w_ffn_gate[l], w_ffn_up[l],
                         w_ffn_down[l])
    return x
```

```python
from contextlib import ExitStack
import numpy as np
import concourse.bass as bass
import concourse.mybir as mybir
from concourse.bass_utils import run_bass_kernel_spmd

F32 = mybir.dt.float32
BF16 = mybir.dt.bfloat16
AF = mybir.ActivationFunctionType
OP = mybir.AluOpType
AX = mybir.AxisListType

D = 1024
KD = 8
TB = 512
FF = 2816
KF = 22
NPIECE = 44
PW = 4096
NVEC = 48
LOG_DECAY_C = -float(np.exp(-0.5))


class Buf:
    def __init__(self, name):
        self.name = name
        self.w = None
        self.r = {}
        self.dsem = None
        self.dcnt = 0


class Tl:
    def __init__(self, t, shape, buf=None, name=""):
        self.t = t
        self.shape = list(shape)
        self.rs = int(np.prod(shape[1:]))
        self.buf = buf or Buf(name)

    def ap(self, p0, npart, off, dims):
        return bass.AP(self.t, p0 * self.rs + off, [[self.rs, npart]] + [list(d) for d in dims])

    def __getitem__(self, idx):
        return self.t[idx]


class Prog:
    ENGS = ["pe", "dve", "act", "pool", "sp"]

    def __init__(self, nc, es):
        self.nc = nc
        self.es = es
        self.sem = {e: es.enter_context(nc.semaphore("s_" + e)) for e in self.ENGS}
        self.cnt = {e: 0 for e in self.ENGS}
        self.ins = {e: [] for e in self.ENGS}
        self.seen = {e: {} for e in self.ENGS}
        self.semobj = dict(self.sem)
        self.ndsem = 0
        self.nwaits = 0
        self.tags = {}

    def _waits(self, eng, deps):
        need = {}
        for d in deps:
            if d is None:
                continue
            k, v = d
            if k == eng and eng == "pe":
                continue
            if v > need.get(k, 0):
                need[k] = v
        out = []
        for k, v in need.items():
            if self.seen[eng].get(k, 0) >= v:
                continue
            self.seen[eng][k] = v
            out.append((self.semobj[k], v))
        self.nwaits += len(out)
        return out

    def op(self, eng, fn, reads=(), writes=(), signal=True):
        deps = []
        for b in reads:
            deps.append(b.buf.w)
        for b in writes:
            deps.append(b.buf.w)
            deps.extend(b.buf.r.items())
        waits = self._waits(eng, deps)
        val = self.cnt[eng] + 1
        if signal:
            self.cnt[eng] = val
        import sys as _sys
        fr = _sys._getframe(1)
        while fr.f_code.co_name in ("op", "mm", "tr", "act", "tt", "ts", "stt", "copy", "red", "memset", "recip", "<lambda>"):
            fr = fr.f_back
        tag = "%s:%d" % (fr.f_code.co_name, fr.f_lineno)
        self.ins[eng].append((waits, fn, (self.sem[eng], 1) if signal else None, tag))
        for b in reads:
            if b.buf.r.get(eng, 0) < val:
                b.buf.r[eng] = val
        for b in writes:
            b.buf.w = (eng, val)
            b.buf.r = {}

    def _dsem(self, buf):
        if buf.dsem is None:
            buf.dsem = "d%d" % self.ndsem
            self.ndsem += 1
            self.semobj[buf.dsem] = self.es.enter_context(self.nc.semaphore(buf.dsem))
        return buf.dsem

    def dma(self, q, out, in_, tile, load=True):
        b = tile.buf
        k = self._dsem(b)
        deps = [b.w]
        if load:
            deps.extend(b.r.items())
        waits = self._waits(q, deps)
        b.dcnt += 16
        self.ins[q].append((waits, lambda e: e.dma_start(out=out, in_=in_), (self.semobj[k], 16), "dma"))
        if load:
            b.w = (k, b.dcnt)
            b.r = {}
        else:
            b.r[k] = b.dcnt

    def inherit(self, dsts, srcs):
        acc = {}
        for s_ in srcs:
            items = list(s_.buf.r.items())
            if s_.buf.w is not None:
                items.append(s_.buf.w)
            for k, v in items:
                if v > acc.get(k, 0):
                    acc[k] = v
        for d_ in dsts:
            d_.buf.w = None
            d_.buf.r = dict(acc)

    def finish(self, block, final_bufs):
        waits = []
        for b in final_bufs:
            if b.buf.dsem is not None:
                waits.append((self.semobj[b.buf.dsem], b.buf.dcnt))
        self.ins["sp"].append((waits, None, None, "end"))
        engmap = {"pe": block.tensor, "dve": block.vector, "act": block.scalar,
                  "pool": block.gpsimd, "sp": block.sync}
        for e in self.ENGS:
            lst = self.ins[e]

            def body(eng, lst=lst):
                for waits, fn, inc, tag in lst:
                    for s, v in waits:
                        eng.wait_ge(s, v)
                    if fn is None:
                        continue
                    i = fn(eng)
                    try:
                        self.tags[str(i.ins.name)] = tag
                    except Exception:
                        pass
                    if inc is not None:
                        i.then_inc(inc[0], inc[1])
            engmap[e](body)

    def mm(self, out, lhsT, rhs, start, stop, reads, writes, signal=None):
        if signal is None:
            signal = stop
        self.op("pe", lambda e: e.matmul(out, lhsT=lhsT, rhs=rhs, start=start, stop=stop),
                reads, writes, signal)

    def tr(self, out, in_, ident, reads, writes, signal=True):
        self.op("pe", lambda e: e.transpose(out, in_, ident), reads, writes, signal)

    def act(self, out, in_, func, reads, writes, scale=1.0, bias=None):
        if bias is None:
            self.op("act", lambda e: e.activation(out=out, in_=in_, func=func, scale=scale), reads, writes)
        else:
            self.op("act", lambda e: e.activation(out=out, in_=in_, func=func, scale=scale, bias=bias),
                    reads, writes)

    def tt(self, out, in0, in1, op, reads, writes, eng="dve"):
        self.op(eng, lambda e: e.tensor_tensor(out=out, in0=in0, in1=in1, op=op), reads, writes)

    def ts(self, out, in0, s1, op0, reads, writes, s2=None, op1=None, eng="dve"):
        if op1 is None:
            self.op(eng, lambda e: e.tensor_scalar(out=out, in0=in0, scalar1=s1, scalar2=None, op0=op0),
                    reads, writes)
        else:
            self.op(eng, lambda e: e.tensor_scalar(out=out, in0=in0, scalar1=s1, scalar2=s2, op0=op0, op1=op1),
                    reads, writes)

    def stt(self, out, in0, scalar, in1, op0, op1, reads, writes):
        self.op("dve", lambda e: e.scalar_tensor_tensor(out=out, in0=in0, scalar=scalar, in1=in1,
                                                        op0=op0, op1=op1), reads, writes)

    def copy(self, out, in_, reads, writes, eng="dve"):
        if eng == "act":
            self.op("act", lambda e: e.copy(out=out, in_=in_), reads, writes)
        else:
            self.op(eng, lambda e: e.tensor_copy(out=out, in_=in_), reads, writes)

    def red(self, out, in_, op, reads, writes):
        self.op("dve", lambda e: e.tensor_reduce(out=out, in_=in_, op=op, axis=AX.X), reads, writes)

    def memset(self, ap, val, writes, eng="dve"):
        self.op(eng, lambda e: e.memset(ap, val), [], writes)

    def recip(self, out, in_, reads, writes):
        self.op("dve", lambda e: e.reciprocal(out=out, in_=in_), reads, writes)


def _bf(a):
    import ml_dtypes
    return np.asarray(a, dtype=np.float32).astype(ml_dtypes.bfloat16)


def make_consts(T):
    c = {}
    idx = np.arange(128)
    ident = np.eye(128, dtype=np.float32)
    s = np.arange(64)[:, None]
    t = np.arange(64)[None, :]
    tri1 = np.concatenate([(s < t), (s <= t)], axis=1).astype(np.float32)
    tri3 = np.concatenate([(s > t), (s > t)], axis=1).astype(np.float32)
    tri = np.zeros((128, 256), np.float32)
    tri[:64, :128] = tri1 * LOG_DECAY_C
    tri[:64, 128:] = tri3 * LOG_DECAY_C
    half = 32
    inv_freq = 1.0 / (10000.0 ** (np.arange(half, dtype=np.float32) * 2.0 / 64))
    pos = np.arange(T, dtype=np.float32)
    ang = pos[None, :] * inv_freq[:, None]
    cosT = np.cos(ang)[idx % 32]
    sinT = np.sin(ang)[idx % 32] * np.where((idx % 64) < 32, -1.0, 1.0)[:, None]
    H = 8
    log_gamma = np.log1p(-np.power(2.0, -5.0 - np.arange(H, dtype=np.float64)))
    i = np.arange(128, dtype=np.float64)
    xi = np.exp(log_gamma[:, None] * (i[None, :] + 1.0))
    kf = (64 ** -0.5) * np.exp(-log_gamma[:, None] * (i[None, :] + 1.0))
    gc = np.exp(log_gamma * 128.0)
    XI = np.zeros((128, 4, 128), np.float32)
    KFt = np.zeros((128, 4, 128), np.float32)
    GC = np.zeros((128, 4), np.float32)
    for h in range(H):
        rows = slice((h % 2) * 64, (h % 2) * 64 + 64)
        XI[rows, h // 2, :] = xi[h][None, :]
        KFt[rows, h // 2, :] = kf[h][None, :]
        GC[rows, h // 2] = gc[h]
    ones = np.full((128, 64), LOG_DECAY_C, np.float32)
    c["cf"] = np.concatenate([ident, tri, XI.reshape(128, -1), KFt.reshape(128, -1), GC, ones], axis=1)
    c["rot"] = np.concatenate([cosT, sinT], axis=1).astype(np.float32)
    identb = np.eye(128, dtype=np.float32)
    blk64 = (idx[:, None] // 64 == idx[None, :] // 64).astype(np.float32) / 64.0
    onesD = np.full((128, 128), 1.0 / 1024.0, np.float32)
    mdiag = (idx[:, None] <= idx[None, :]).astype(np.float32)
    mprev = (idx[:, None] > idx[None, :]).astype(np.float32)
    perm = np.zeros((128, 128), np.float32)
    for p in range(128):
        q = p + 32 if (p % 64) < 32 else p - 32
        perm[q, p] = 1.0
    m4 = np.zeros((128, 128), np.float32)
    ss = np.arange(64)[:, None]
    tt = np.arange(64)[None, :]
    for w in range(2):
        m4[w * 64:(w + 1) * 64, 0:64] = (ss < tt)
        m4[w * 64:(w + 1) * 64, 64:128] = (ss <= tt)
    mst = np.zeros((128, 64), np.float32)
    mst[:64] = (np.arange(64)[:, None] > np.arange(64)[None, :])
    ones128 = np.ones((128, 128), np.float32)
    c["cb"] = _bf(np.concatenate([identb, blk64, onesD, mdiag, mprev, perm, m4, mst, ones128], axis=1))
    return c


CF_IDENT, CF_TRI, CF_XI, CF_KF, CF_GC, CF_ONES = 0, 128, 384, 896, 1408, 1412
CF_W = 1412 + 64
CB_IDENT, CB_BLK, CB_OND, CB_MD, CB_MP, CB_PERM, CB_M4, CB_MST, CB_ONES = 0, 128, 256, 384, 512, 640, 768, 896, 960
CB_W = 960 + 128


def _fm(W):
    K, N = W.shape
    kc = K // 128
    out = np.zeros((128, PW), np.float32)
    out[:, :kc * N] = W.reshape(kc, 128, N).transpose(1, 0, 2).reshape(128, kc * N)
    return out


def _hm(W, c0):
    out = np.zeros((128, PW), np.float32)
    out[:64] = W[:, c0:c0 + 512].reshape(8, 64, 512).transpose(1, 0, 2).reshape(64, 4096)
    return out


def pack_layer(inp, l):
    w_in = inp["w_in"][l]
    P = []
    P.append(_fm(w_in[:, 0:512]))
    akv = np.concatenate([w_in[:, 512:576], w_in[:, 512:576], w_in[:, 576:640], w_in[:, 576:640],
                          w_in[:, 640:768]], axis=1)
    P.append(_fm(akv))
    P.append(_fm(w_in[:, 2560:3072]))
    P.append(_fm(w_in[:, 3072:3584]))
    P.append(_fm(w_in[:, 3584:4096]))
    P.append(_fm(w_in[:, 4096:4608]))
    P.append(_fm(w_in[:, 768:1280]))
    P.append(_fm(w_in[:, 1280:1792]))
    P.append(_fm(w_in[:, 1792:2304]))
    P.append(_fm(w_in[:, 2304:2560]))
    for b, wo in enumerate([inp["w_attn_o"][l], inp["w_rwkv_o"][l], inp["w_ret_o"][l]]):
        for hf in range(2):
            P.append(_hm(wo, hf * 512))
            c0 = 4608 + b * 1024 + hf * 512
            P.append(_fm(w_in[:, c0:c0 + 512]))
    for hf in range(2):
        P.append(_fm(inp["w_out"][l][:, hf * 512:(hf + 1) * 512]))
    for i in range(6):
        c0 = i * 512
        c1 = min(c0 + 512, FF)
        P.append(_fm(inp["w_ffn_gate"][l][:, c0:c1]))
        P.append(_fm(inp["w_ffn_up"][l][:, c0:c1]))
    wd = inp["w_ffn_down"][l]
    for m in range(8):
        P.append(_fm(wd[:, m * 128:(m + 1) * 128]))
    assert len(P) == NPIECE
    return np.stack(P)


def pack_vecs(inp, L):
    v = np.zeros((128, L, NVEC), np.float32)
    idx = np.arange(128)

    def fm(a):
        return a.reshape(-1, 128).T

    for l in range(L):
        v[:, l, 0:8] = fm(inp["norm1_g"][l])
        v[:, l, 8:16] = fm(inp["norm2_g"][l])
        mu = inp["rwkv_shift_mu"][l]
        v[:, l, 16:28] = fm(mu[0:1536])
        v[:64, l, 28] = mu[1536:1600]
        v[:64, l, 29] = mu[1600:1664]
        v[:, l, 30] = mu[1664:1792]
        v[:, l, 31:35] = fm(inp["rwkv_k_k"][l])
        v[:, l, 35:39] = fm(inp["rwkv_k_a"][l])
        v[:, l, 39:43] = fm(inp["rwkv_r_k"][l].reshape(-1))
        v[:, l, 43] = inp["attn_q_norm_g"][l][idx % 64]
        v[:, l, 44] = inp["attn_k_norm_g"][l][idx % 64]
    return v


def pack_small(inp, L):
    lw = np.zeros((128, L, 3, 512), np.float32)
    rows = np.zeros((L, 4, 512), np.float32)
    for l in range(L):
        lw[:64, l, 0] = inp["rwkv_w2"][l]
        lw[:64, l, 1] = inp["rwkv_a2"][l]
        lw[:, l, 2] = inp["rwkv_g2"][l]
        rows[l, 0] = inp["rwkv_w0"][l]
        rows[l, 1] = inp["rwkv_a0"][l]
        rows[l, 2] = inp["rwkv_lnx_g"][l]
        rows[l, 3] = inp["rwkv_lnx_b"][l]
    sinks = np.asarray(inp["attn_sinks"], np.float32)[:L].reshape(L, 8)
    return lw, rows, sinks


def build(NSEQ, T, L, debug=False):
    nc = bass.Bass("TRN2", target_bir_lowering=False)
    NTB = T // TB
    NT = NSEQ * T
    xT_d = nc.dram_tensor("xT", [D, NT], F32, kind="ExternalInput").ap()
    wpk_d = nc.dram_tensor("wpk", [L, NPIECE, 128, PW], F32, kind="ExternalInput").ap()
    vec_d = nc.dram_tensor("vecs", [128, L * NVEC], F32, kind="ExternalInput").ap()
    lw_d = nc.dram_tensor("lw", [128, L * 1536], F32, kind="ExternalInput").ap()
    rows_d = nc.dram_tensor("rows", [L, 2048], F32, kind="ExternalInput").ap()
    sink_d = nc.dram_tensor("sinks", [1, L * 8], F32, kind="ExternalInput").ap()
    cf_d = nc.dram_tensor("cf", [128, CF_W], F32, kind="ExternalInput").ap()
    cb_d = nc.dram_tensor("cb", [128, CB_W], BF16, kind="ExternalInput").ap()
    rot_d = nc.dram_tensor("rot", [128, 2 * T], F32, kind="ExternalInput").ap()
    yT_d = nc.dram_tensor("yT", [D, NT], F32, kind="ExternalOutput").ap()
    dbg_d = None
    if debug:
        dbg_d = nc.dram_tensor("dbg", [3, 64, 8 * TB], BF16, kind="ExternalOutput").ap()

    es = ExitStack()
    with es:
        P = Prog(nc, es)

        def sb(name, shape, dt=F32):
            return Tl(es.enter_context(nc.sbuf_tensor("s_" + name, list(shape), dt)), shape, name=name)

        def view(base, name, shape, dt, col0_bytes):
            raise NotImplementedError

        rvtm = sb("rvtm", [128, 4, 512], BF16)
        cf = sb("cf", [128, CF_W])
        cb = sb("cb", [128, CB_W], BF16)
        rotc = sb("rotc", [128, 2 * T], BF16)
        vecs = sb("vecs", [128, L * NVEC])
        lwb = sb("lwb", [128, 1536], BF16)
        rowf = sb("rowf", [128, 1024])
        rv33 = sb("rv33", [64, 1024])
        rhi = sb("rhi", [64, 1024], BF16)
        HL = sb("HL", [64, 1024], BF16)
        sinkx = sb("sinkx", [128, L * 8])
        eps6 = sb("eps6", [128, 1])
        P.dma("sp", cf.t[:, :], cf_d, cf)
        P.dma("sp", cb.t[:, :], cb_d, cb)
        P.dma("sp", vecs.t[:, :], vec_d, vecs)
        for hh in range(2):
            P.dma("pool", rotc.ap(0, 128, hh * T, [[T // 2, 2], [1, T // 2]]) if T > 2048 else rotc.ap(0, 128, hh * T, [[1, T]]),
                  bass.AP(rot_d.tensor, hh * T, [[2 * T, 128], [1, T]]), rotc)
        P.dma("sp", sinkx.t[:, :], bass.AP(sink_d.tensor, 0, [[0, 128], [1, L * 8]]), sinkx)
        P.act(sinkx.t[:, :], sinkx.t[:, :], AF.Exp, [sinkx], [sinkx])
        P.memset(eps6.t[:, :], 1e-6, [eps6])
        P.memset(HL.t[:, :], 0.0, [HL])

        def cba(col, w, p0=0, npart=128):
            return cb.ap(p0, npart, col, [[1, w]])

        def vcol(l, j, p0=0, npart=128):
            return vecs.ap(p0, npart, l * NVEC + j, [[1, 1]])

        xT = sb("xT", [128, KD, TB])
        hT = sb("hT", [128, KD, TB], BF16)
        NW = 2
        wring = [sb("w%d" % i, [128, PW], BF16) for i in range(NW)]
        ps = [Tl(es.enter_context(nc.psum_tensor("ps%d" % i, [128, 512], F32)), [128, 512], name="ps%d" % i)
              for i in range(8)]
        psb = [Tl(p.t.bitcast(BF16), [128, 1024], buf=p.buf) for p in ps]
        pctr = [0]

        def psum():
            i = pctr[0] % 8
            pctr[0] += 1
            return ps[i], psb[i]

        oT = sb("oT", [64, 8, TB], BF16)
        B1 = sb("B1", [128, 4096], BF16)
        BIG = sb("BIG", [128, 3 * 4096], BF16)
        mixed = sb("mixed", [128, KD, TB], BF16)
        tmpA = sb("tmpA", [128, TB])
        tmpB = sb("tmpB", [128, TB])
        tmpD = sb("tmpD", [128, TB], BF16)
        rgtm = sb("rgtm", [128, 4, 512], BF16)
        vtm = sb("vtm", [128, 4, 128], BF16)
        PT = [sb("PT0", [128, 1024], BF16), None]
        ktm = sb("ktm", [128, 4, 512], BF16)
        sT = sb("sT", [128, 1024], BF16)
        PT[1] = sT
        rwsb = sb("rwsb", [128, TB + 1])
        wdx = sb("wdx", [64, 2 * TB], BF16)
        adx = sb("adx", [64, 2 * TB], BF16)
        gdx = sb("gdx", [128, 2 * TB], BF16)
        a_tm = sb("a_tm", [128, 512])
        sig = sb("sig", [128, 512])
        g_tm = sb("g_tm", [128, 512])
        E3 = sb("E3", [128, 512])
        E2 = sb("E2", [128, 512])
        Q1 = sb("Q1", [128, 512])
        nk = sb("nk", [128, 512])
        Ysb = nk
        rstd = tmpB
        X3 = sb("X3", [128, 512], BF16)
        X2 = sb("X2", [128, 512], BF16)
        UV = sb("UV", [128, 512], BF16)
        obt = sb("obt", [128, 512], BF16)
        octm = obt
        X3T = sb("X3T", [64, 8, 128], BF16)
        X1T = sb("X1T", [64, 8, 128], BF16)
        S_sb = sb("S_sb", [128, 8, 128], BF16)
        Apow = [sb("Apow%d" % i, [64, 8, 64], BF16) for i in range(2)]
        Npow = [sb("Npow%d" % i, [64, 8, 64], BF16) for i in range(2)]
        Xf = sb("Xf", [64, 8, 64])
        Xb = sb("Xb", [64, 8, 64], BF16)
        sm = sb("sm", [128, 64])
        st_k = [sb("stk%d" % l, [128, 2, 128], BF16) for l in range(L)]
        st_v = [sb("stv%d" % l, [128, 128], BF16) for l in range(L)]
        st_R = [sb("stR%d" % l, [128, 4, 64]) for l in range(L)]
        st_Rb = [sb("stRb%d" % l, [128, 8, 64], BF16) for l in range(L)]
        st_H = [sb("stH%d" % l, [64, 8, 64]) for l in range(L)]
        st_Hb = [sb("stHb%d" % l, [64, 8, 64], BF16) for l in range(L)]
        st_sh = [sb("stsh%d" % l, [128, 16]) for l in range(L)]

        wq = {"i": 0}
        tbi = [0]

        def wpiece(l, j):
            tl = wring[wq["i"] % NW]
            wq["i"] += 1
            P.dma("pool", tl.ap(0, 128, 0, [[2048, 2], [1, 2048]]),
                  bass.AP(wpk_d.tensor, (l * NPIECE + j) * 128 * PW, [[PW, 128], [2048, 2], [1, 2048]]), tl)
            return tl

        def rmsnorm(l, gcol):
            sq = BIG
            P.act(sq.ap(0, 128, 0, [[1, KD * TB]]), xT.ap(0, 128, 0, [[1, KD * TB]]), AF.Square, [xT], [sq])
            pt, _ = psum()
            for k in range(KD):
                P.mm(pt.t[:, :], cba(CB_OND, 128), sq.ap(0, 128, k * TB, [[1, TB]]), k == 0, k == KD - 1, [cb, sq], [pt])
            P.act(rstd.t[:, :], pt.t[:, :], AF.Ln, [pt, eps6], [rstd], bias=eps6.t[:, 0:1])
            P.act(rstd.t[:, :], rstd.t[:, :], AF.Exp, [rstd], [rstd], scale=-0.5)
            for k in range(KD):
                P.stt(hT.t[:, k, :], xT.t[:, k, :], vcol(l, gcol + k), rstd.t[:, :], OP.mult, OP.mult,
                      [xT, vecs, rstd], [hT])

        def proj_fm(w, ncol, cb_fn, src=None, M=128):
            src = src or hT
            for m in range(ncol // M):
                pt, ptb = psum()
                for k in range(KD):
                    P.mm(pt.ap(0, M, 0, [[1, TB]]), w.ap(0, 128, k * ncol + m * M, [[1, M]]),
                         src.t[:, k, :], k == 0, k == KD - 1, [w, src], [pt])
                cb_fn(m, pt)

        def headnorm(pt, dst_ap, gcolap, dstT):
            P.act(tmpD.t[:, :], pt.t[:, :], AF.Square, [pt], [tmpD])
            p2, _ = psum()
            P.mm(p2.t[:, :], cba(CB_BLK, 128), tmpD.t[:, :], True, True, [cb, tmpD], [p2])
            P.act(tmpA.t[:, :], p2.t[:, :], AF.Ln, [p2, eps6], [tmpA], bias=eps6.t[:, 0:1])
            P.act(tmpA.t[:, :], tmpA.t[:, :], AF.Exp, [tmpA], [tmpA], scale=-0.5)
            P.stt(dst_ap, pt.t[:, :], gcolap, tmpA.t[:, :], OP.mult, OP.mult, [pt, vecs, tmpA], [dstT])

        def merge_branch(l, b):
            for hf in range(2):
                wo = wpiece(l, 10 + b * 4 + hf * 2)
                wg = wpiece(l, 11 + b * 4 + hf * 2)
                for m in range(4):
                    pg, _ = psum()
                    for k in range(KD):
                        P.mm(pg.t[:, :], wg.ap(0, 128, k * 512 + m * 128, [[1, 128]]), hT.t[:, k, :],
                             k == 0, k == KD - 1, [wg, hT], [pg])
                    po, _ = psum()
                    for h in range(8):
                        P.mm(po.t[:, :], wo.ap(0, 64, h * 512 + m * 128, [[1, 128]]), oT.t[:, h, :],
                             h == 0, h == 7, [wo, oT], [po])
                    P.act(tmpA.t[:, :], pg.t[:, :], AF.Sigmoid, [pg], [tmpA])
                    mi = hf * 4 + m
                    if b == 0:
                        P.tt(mixed.t[:, mi, :], po.t[:, :], tmpA.t[:, :], OP.mult, [po, tmpA], [mixed])
                    else:
                        P.tt(tmpB.t[:, :], po.t[:, :], tmpA.t[:, :], OP.mult, [po, tmpA], [tmpB])
                        P.tt(mixed.t[:, mi, :], mixed.t[:, mi, :], tmpB.t[:, :], OP.add, [mixed, tmpB], [mixed])
            if debug:
                P.dma("sp", dbg_d[b], oT.ap(0, 64, 0, [[1, 8 * TB]]), oT, load=False)

        class _Stop(Exception):
            pass

        def stage(i):
            import os as _os
            if int(_os.environ.get("KSTOP", "99")) < i:
                raise _Stop()

        def attention(l, first):
            try:
                attention_(l, first)
            except _Stop:
                pass

        def attention_(l, first):
            import os as _os
            if "KSTOP" in _os.environ:
                P.memset(oT.ap(0, 64, 0, [[1, 8 * TB]]), 0.0, [oT])
            w = wpiece(l, 0)
            proj_fm(w, 512, lambda m, pt: headnorm(pt, B1.ap(0, 128, m * 512, [[1, 512]]), vcol(l, 43), B1))
            stage(2)
            w = wpiece(l, 1)
            for m in range(2):
                pt, _ = psum()
                for k in range(KD):
                    P.mm(pt.t[:, :], w.ap(0, 128, k * 384 + m * 128, [[1, 128]]), hT.t[:, k, :],
                         k == 0, k == KD - 1, [w, hT], [pt])
                headnorm(pt, B1.ap(0, 128, 2048 + m * 512, [[1, 512]]), vcol(l, 44), B1)
            stage(3)
            for n in range(4):
                pt, _ = psum()
                for k in range(KD):
                    P.mm(pt.ap(0, 128, 0, [[1, 128]]), hT.t[:, k, n * 128:(n + 1) * 128],
                         w.ap(0, 128, k * 384 + 256, [[1, 128]]), k == 0, k == KD - 1, [w, hT], [pt])
                P.copy(vtm.t[:, n, :], pt.t[:, 0:128], [pt], [vtm], eng="act")
            stage(4)
            for n in range(4):
                blocks = []
                if not (first and n == 0):
                    blocks.append(0)
                blocks.append(1)
                pts = {}
                for jb in blocks:
                    pa, _ = psum()
                    pb, _ = psum()
                    for h in range(8):
                        g = h // 4
                        base = (h % 2) * 64
                        if jb == 1:
                            kap = B1.ap(base, 64, 2048 + g * 512 + n * 128, [[1, 128]])
                            kr = [B1]
                        elif n == 0:
                            kap = st_k[l].ap(base, 64, g * 128, [[1, 128]])
                            kr = [st_k[l]]
                        else:
                            kap = B1.ap(base, 64, 2048 + g * 512 + (n - 1) * 128, [[1, 128]])
                            kr = [B1]
                        qap = B1.ap(base, 64, (h // 2) * 512 + n * 128, [[1, 128]])
                        pt = pa if h % 2 == 0 else pb
                        P.mm(pt.ap(0, 128, (h // 2) * 128, [[1, 128]]), kap, qap, True, True,
                             kr + [B1], [pt], signal=(h // 2 == 3))
                    pts[jb] = (pa, pb)
                stage(5)
                for jb in blocks:
                    for par, pt in enumerate(pts[jb]):
                        P.act(PT[jb].ap(0, 128, par * 128, [[256, 4], [1, 128]]), pt.ap(0, 128, 0, [[128, 4], [1, 128]]),
                              AF.Exp, [pt], [PT[jb]], scale=0.125)
                    stage(6)
                    mcol = CB_MP if jb == 0 else CB_MD
                    P.tt(PT[jb].ap(0, 128, 0, [[128, 8], [1, 128]]), PT[jb].ap(0, 128, 0, [[128, 8], [1, 128]]),
                         cb.ap(0, 128, mcol, [[0, 8], [1, 128]]), OP.mult, [PT[jb], cb], [PT[jb]])
                stage(7)
                for g in range(2):
                    po, _ = psum()
                    pd, _ = psum()
                    for bi, jb in enumerate(blocks):
                        if jb == 1:
                            vap = vtm.ap(0, 128, n * 128 + g * 64, [[1, 64]])
                            vr = [vtm]
                        elif n == 0:
                            vap = st_v[l].ap(0, 128, g * 64, [[1, 64]])
                            vr = [st_v[l]]
                        else:
                            vap = vtm.ap(0, 128, (n - 1) * 128 + g * 64, [[1, 64]])
                            vr = [vtm]
                        rhs = PT[jb].ap(0, 128, g * 512, [[1, 512]])
                        P.mm(po.ap(0, 64, 0, [[1, 512]]), vap, rhs, bi == 0, bi == len(blocks) - 1, vr + [PT[jb]], [po])
                        P.mm(pd.ap(0, 64, 0, [[1, 512]]), cba(CB_ONES, 64), rhs, bi == 0, bi == len(blocks) - 1,
                             [cb, PT[jb]], [pd])
                    stage(8)
                    P.tt(tmpA.ap(0, 64, 0, [[128, 4], [1, 128]]), pd.ap(0, 64, 0, [[128, 4], [1, 128]]),
                         sinkx.ap(0, 64, l * 8 + g * 4, [[1, 4], [0, 128]]), OP.add, [pd, sinkx], [tmpA])
                    P.recip(tmpA.ap(0, 64, 0, [[1, 512]]), tmpA.ap(0, 64, 0, [[1, 512]]), [tmpA], [tmpA])
                    P.tt(oT.ap(0, 64, g * 4 * TB + n * 128, [[TB, 4], [1, 128]]),
                         po.ap(0, 64, 0, [[128, 4], [1, 128]]), tmpA.ap(0, 64, 0, [[128, 4], [1, 128]]),
                         OP.mult, [po, tmpA], [oT])
            for g in range(2):
                P.copy(st_k[l].t[:, g, :], B1.ap(0, 128, 2048 + g * 512 + 384, [[1, 128]]), [B1], [st_k[l]], eng="act")
            P.copy(st_v[l].t[:, :], vtm.t[:, 3, :], [vtm], [st_v[l]], eng="act")

        def retention(l):
            try:
                retention_(l)
            except _Stop:
                pass

        def retention_(l):
            import os as _os
            if "KSTOP" in _os.environ:
                P.memset(oT.ap(0, 64, 0, [[1, 8 * TB]]), 0.0, [oT])

            def rotary(pt, dst_ap, fac_col, m):
                P.copy(tmpD.t[:, :], pt.t[:, :], [pt], [tmpD], eng="act")
                p2, _ = psum()
                P.mm(p2.t[:, :], cba(CB_PERM, 128), tmpD.t[:, :], True, True, [cb, tmpD], [p2])
                import os as _os
                KR = _os.environ.get("KROT", "")
                if KR == "1":
                    P.copy(tmpA.t[:, :], p2.t[:, :], [p2], [tmpA])
                    P.copy(dst_ap, tmpA.ap(0, 128, 0, [[128, 4], [1, 128]]), [tmpA], [B1])
                    return
                if KR == "3":
                    P.tt(tmpA.t[:, :], pt.t[:, :], cf.ap(0, 128, 0, [[1, 512]]), OP.mult, [pt, cf], [tmpA])
                    P.tt(tmpB.t[:, :], p2.t[:, :], cf.ap(0, 128, 512, [[1, 512]]), OP.mult, [p2, cf], [tmpB])
                else:
                    P.copy(E3.t[:, :], pt.t[:, :], [pt], [E3], eng="act")
                    P.tt(tmpA.t[:, :], E3.t[:, :], rotc.ap(0, 128, tbi[0] * TB, [[1, TB]]), OP.mult, [E3, rotc], [tmpA])
                    P.tt(tmpB.t[:, :], p2.t[:, :], rotc.ap(0, 128, T + tbi[0] * TB, [[1, TB]]), OP.mult, [p2, rotc], [tmpB])
                P.tt(tmpA.t[:, :], tmpA.t[:, :], tmpB.t[:, :], OP.add, [tmpA, tmpB], [tmpA])
                if KR == "2":
                    P.copy(dst_ap, tmpA.ap(0, 128, 0, [[128, 4], [1, 128]]), [tmpA], [B1])
                    return
                P.tt(dst_ap, tmpA.ap(0, 128, 0, [[128, 4], [1, 128]]),
                     cf.ap(0, 128, fac_col + m * 128, [[0, 4], [1, 128]]), OP.mult, [tmpA, cf], [B1])

            w = wpiece(l, 2)
            proj_fm(w, 512, lambda m, pt: rotary(pt, B1.ap(0, 128, m * 512, [[128, 4], [1, 128]]), CF_XI, m))
            stage(10)
            w = wpiece(l, 3)
            proj_fm(w, 512, lambda m, pt: rotary(pt, B1.ap(0, 128, 2048 + m * 512, [[128, 4], [1, 128]]), CF_KF, m))
            stage(11)
            for n in range(4):
                _, ptb = psum()
                for m in range(4):
                    P.tr(ptb.ap(0, 128, m * 128, [[1, 128]]), B1.ap(0, 128, 2048 + m * 512 + n * 128, [[1, 128]]),
                         cba(CB_IDENT, 128), [B1, cb], [ptb], signal=(m == 3))
                P.copy(ktm.t[:, n, :], ptb.t[:, 0:512], [ptb], [ktm], eng="act")
            stage(12)
            w = wpiece(l, 4)
            for n in range(4):
                pt, _ = psum()
                for k in range(KD):
                    P.mm(pt.t[:, :], hT.t[:, k, n * 128:(n + 1) * 128], w.ap(0, 128, k * 512, [[1, 512]]),
                         k == 0, k == KD - 1, [w, hT], [pt])
                P.copy(rvtm.t[:, n, :], pt.t[:, :], [pt], [rvtm], eng="act")
            w = wpiece(l, 5)
            for n in range(4):
                pt, _ = psum()
                for k in range(KD):
                    P.mm(pt.t[:, :], hT.t[:, k, n * 128:(n + 1) * 128], w.ap(0, 128, k * 512, [[1, 512]]),
                         k == 0, k == KD - 1, [w, hT], [pt])
                P.act(rgtm.t[:, n, :], pt.t[:, :], AF.Silu, [pt], [rgtm])
            stage(13)
            for n in range(4):
                pa, _ = psum()
                pb, _ = psum()
                for h in range(8):
                    base = (h % 2) * 64
                    kap = B1.ap(base, 64, 2048 + (h // 2) * 512 + n * 128, [[1, 128]])
                    qap = B1.ap(base, 64, (h // 2) * 512 + n * 128, [[1, 128]])
                    pt = pa if h % 2 == 0 else pb
                    P.mm(pt.ap(0, 128, (h // 2) * 128, [[1, 128]]), kap, qap, True, True, [B1], [pt], signal=(h // 2 == 3))
                for par, pt in enumerate((pa, pb)):
                    P.act(sT.ap(0, 128, par * 128, [[256, 4], [1, 128]]), pt.ap(0, 128, 0, [[128, 4], [1, 128]]),
                          AF.Copy, [pt], [sT])
                P.tt(sT.ap(0, 128, 0, [[128, 8], [1, 128]]), sT.ap(0, 128, 0, [[128, 8], [1, 128]]),
                     cb.ap(0, 128, CB_MD, [[0, 8], [1, 128]]), OP.mult, [sT, cb], [sT])
                stage(14)
                po, _ = psum()
                for h in range(8):
                    o_ap = po.ap(0, 128, h * 64, [[1, 64]])
                    P.mm(o_ap, sT.ap(0, 128, h * 128, [[1, 128]]), rvtm.ap(0, 128, n * 512 + h * 64, [[1, 64]]),
                         True, False, [rvtm, sT], [po], signal=False)
                    P.mm(o_ap, B1.ap(0, 128, (h // 2) * 512 + n * 128, [[1, 128]]), st_Rb[l].ap(0, 128, h * 64, [[1, 64]]),
                         False, True, [st_Rb[l], B1], [po], signal=(h == 7))
                stage(15)
                pk0, _ = psum()
                pk1, _ = psum()
                for h in range(8):
                    base = (h % 2) * 64
                    pk = pk0 if h % 2 == 0 else pk1
                    P.mm(pk.ap(base, 64, (h // 2) * 64, [[1, 64]]), ktm.ap(0, 128, n * 512 + h * 64, [[1, 64]]),
                         rvtm.ap(0, 128, n * 512 + h * 64, [[1, 64]]), True, True, [ktm, rvtm], [pk], signal=(h >= 6))
                P.tt(st_R[l].ap(0, 64, 0, [[64, 4], [1, 64]]), st_R[l].ap(0, 64, 0, [[64, 4], [1, 64]]),
                     pk0.ap(0, 64, 0, [[64, 4], [1, 64]]), OP.add, [st_R[l], pk0], [st_R[l]])
                P.tt(st_R[l].ap(64, 64, 0, [[64, 4], [1, 64]]), st_R[l].ap(64, 64, 0, [[64, 4], [1, 64]]),
                     pk1.ap(64, 64, 0, [[64, 4], [1, 64]]), OP.add, [st_R[l], pk1], [st_R[l]])
                P.tt(st_R[l].t[:, :, :], st_R[l].t[:, :, :], cf.ap(0, 128, CF_GC, [[1, 4], [0, 64]]), OP.mult,
                     [st_R[l], cf], [st_R[l]])
                stage(16)
                P.act(tmpA.t[:, :], po.t[:, :], AF.Square, [po], [tmpA])
                P.red(sm.ap(0, 128, 0, [[1, 8]]), tmpA.ap(0, 128, 0, [[64, 8], [1, 64]]), OP.add, [tmpA], [sm])
                P.ts(sm.ap(0, 128, 0, [[1, 8]]), sm.ap(0, 128, 0, [[1, 8]]), 1.0 / 64, OP.mult, [sm], [sm], s2=1e-6, op1=OP.add)
                P.act(sm.ap(0, 128, 0, [[1, 8]]), sm.ap(0, 128, 0, [[1, 8]]), AF.Ln, [sm], [sm])
                P.act(sm.ap(0, 128, 0, [[1, 8]]), sm.ap(0, 128, 0, [[1, 8]]), AF.Exp, [sm], [sm], scale=-0.5)
                P.tt(tmpB.ap(0, 128, 0, [[64, 8], [1, 64]]), po.ap(0, 128, 0, [[64, 8], [1, 64]]),
                     sm.ap(0, 128, 0, [[1, 8], [0, 64]]), OP.mult, [po, sm], [tmpB])
                P.tt(octm.t[:, :], tmpB.t[:, :], rgtm.t[:, n, :], OP.mult, [tmpB, rgtm], [octm])
                _, pto = psum()
                for h in range(8):
                    P.tr(pto.ap(0, 64, h * 128, [[1, 128]]), octm.ap(0, 128, h * 64, [[1, 64]]), cba(CB_IDENT, 128),
                         [octm, cb], [pto], signal=(h == 7))
                P.copy(oT.ap(0, 64, n * 128, [[TB, 8], [1, 128]]), pto.ap(0, 64, 0, [[128, 8], [1, 128]]), [pto], [oT])
                P.copy(st_Rb[l].ap(0, 64, 0, [[128, 4], [1, 64]]), st_R[l].ap(0, 64, 0, [[64, 4], [1, 64]]),
                       [st_R[l]], [st_Rb[l]], eng="act")
                P.copy(st_Rb[l].ap(64, 64, 64, [[128, 4], [1, 64]]), st_R[l].ap(64, 64, 0, [[64, 4], [1, 64]]),
                       [st_R[l]], [st_Rb[l]], eng="act")

        def rwkv(l):
            ZR, ZK, ZV, KK, KA, RR = 0, 2048, 4096, 6144, 8192, 10240

            def shifted(pt, M, j, mucol, out_ap, outT):
                P.copy(rwsb.ap(0, M, 1, [[1, TB]]), pt.ap(0, M, 0, [[1, TB]]), [pt], [rwsb], eng="act")
                P.copy(rwsb.ap(0, M, 0, [[1, 1]]), st_sh[l].ap(0, M, j, [[1, 1]]), [st_sh[l]], [rwsb])
                P.copy(st_sh[l].ap(0, M, j, [[1, 1]]), rwsb.ap(0, M, TB, [[1, 1]]), [rwsb], [st_sh[l]])
                P.tt(tmpA.ap(0, M, 0, [[1, TB]]), rwsb.ap(0, M, 0, [[1, TB]]), rwsb.ap(0, M, 1, [[1, TB]]), OP.subtract,
                     [rwsb], [tmpA])
                P.stt(out_ap, tmpA.ap(0, M, 0, [[1, TB]]), vcol(l, mucol, 0, M), rwsb.ap(0, M, 1, [[1, TB]]),
                      OP.mult, OP.add, [tmpA, vecs, rwsb], [outT])

            for ti, zoff in enumerate((ZR, ZK, ZV)):
                w = wpiece(l, 6 + ti)
                proj_fm(w, 512, lambda m, pt, ti=ti, zoff=zoff: shifted(
                    pt, 128, ti * 4 + m, 16 + ti * 4 + m, BIG.ap(0, 128, zoff + m * 512, [[1, TB]]), BIG))
            for m in range(4):
                zk = BIG.ap(0, 128, ZK + m * 512, [[1, TB]])
                zr = BIG.ap(0, 128, ZR + m * 512, [[1, TB]])
                P.ts(BIG.ap(0, 128, KK + m * 512, [[1, TB]]), zk, vcol(l, 31 + m), OP.mult, [BIG, vecs], [BIG])
                P.ts(BIG.ap(0, 128, KA + m * 512, [[1, TB]]), zk, vcol(l, 35 + m), OP.mult, [BIG, vecs], [BIG])
                P.ts(BIG.ap(0, 128, RR + m * 512, [[1, TB]]), zr, vcol(l, 39 + m), OP.mult, [BIG, vecs], [BIG])
            w = wpiece(l, 9)
            for ji, (c0, M, dst, fn) in enumerate(((0, 64, wdx, AF.Tanh), (64, 64, adx, AF.Copy), (128, 128, gdx, AF.Sigmoid))):
                pt, _ = psum()
                for k in range(KD):
                    P.mm(pt.ap(0, M, 0, [[1, TB]]), w.ap(0, 128, k * 256 + c0, [[1, M]]), hT.t[:, k, :],
                         k == 0, k == KD - 1, [w, hT], [pt])
                shifted(pt, M, 12 + ji, 28 + ji, tmpB.ap(0, M, 0, [[1, TB]]), tmpB)
                for dup in range(2):
                    P.act(dst.ap(0, M, dup * 64, [[128, 8], [1, 64]]), tmpB.ap(0, M, 0, [[64, 8], [1, 64]]), fn, [tmpB], [dst])

            for ci in range(TB // 64):
                t0 = ci * 64
                pa_, _ = psum()
                P.mm(pa_.t[:, :], adx.ap(0, 64, 2 * t0, [[1, 128]]), lwb.ap(0, 64, 512, [[1, 512]]), True, False,
                     [adx, lwb], [pa_], signal=False)
                P.mm(pa_.t[:, :], cba(CB_ONES, 128, 0, 33), HL.ap(0, 33, 512, [[1, 512]]), False, True, [cb, HL], [pa_])
                P.act(a_tm.t[:, :], pa_.t[:, :], AF.Sigmoid, [pa_], [a_tm])
                pw_, _ = psum()
                P.mm(pw_.t[:, :], wdx.ap(0, 64, 2 * t0, [[1, 128]]), lwb.ap(0, 64, 0, [[1, 512]]), True, False,
                     [wdx, lwb], [pw_], signal=False)
                P.mm(pw_.t[:, :], cba(CB_ONES, 128, 0, 33), HL.ap(0, 33, 0, [[1, 512]]), False, True, [cb, HL], [pw_])
                P.act(sig.t[:, :], pw_.t[:, :], AF.Sigmoid, [pw_], [sig])
                pg_, _ = psum()
                P.mm(pg_.t[:, :], gdx.ap(0, 128, 2 * t0, [[1, 128]]), lwb.ap(0, 128, 1024, [[1, 512]]), True, True,
                     [gdx, lwb], [pg_])
                P.copy(g_tm.t[:, :], pg_.t[:, :], [pg_], [g_tm], eng="act")
                pc1, _ = psum()
                P.mm(pc1.t[:, :], cf.ap(0, 64, CF_TRI, [[1, 128]]), sig.ap(0, 64, 0, [[1, 512]]), True, True, [cf, sig], [pc1])
                P.act(E3.t[:, :], pc1.t[:, :], AF.Exp, [pc1], [E3])
                pc3, _ = psum()
                P.mm(pc3.t[:, :], cf.ap(0, 64, CF_TRI + 128, [[1, 128]]), sig.ap(0, 64, 0, [[1, 512]]), True, True,
                     [cf, sig], [pc3])
                P.act(E2.t[:, :], pc3.t[:, :], AF.Exp, [pc3], [E2])
                pgc, _ = psum()
                for h in range(8):
                    P.mm(pgc.ap(0, 64, h * 2, [[1, 2]]), sig.ap(0, 64, h * 64, [[1, 64]]), cf.ap(0, 64, CF_ONES, [[1, 2]]),
                         True, True, [sig, cf], [pgc], signal=(h == 7))
                P.act(sm.ap(0, 64, 16, [[1, 8]]), pgc.ap(0, 64, 0, [[2, 8]]), AF.Exp, [pgc], [sm])
                P.act(sm.ap(0, 64, 24, [[1, 8]]), pgc.ap(0, 64, 0, [[2, 8]]), AF.Exp, [pgc], [sm], scale=-1.0)
                tps = {}
                shared = None
                for name, off in (("kk", KK), ("k", ZK), ("ka", KA), ("r", ZR), ("rr", RR), ("v", ZV)):
                    if name == "k":
                        ptb = shared
                    else:
                        _, ptb = psum()
                    if name == "kk":
                        shared = ptb
                    p0 = 0 if name == "kk" else 64
                    for m in range(4):
                        P.tr(ptb.ap(p0, 64, m * 128, [[1, 128]]), BIG.ap(0, 128, off + m * 512 + t0, [[1, 64]]),
                             cba(CB_IDENT, 128), [BIG, cb], [ptb], signal=(m == 3))
                    tps[name] = ptb
                kkp = tps["kk"]
                P.act(tmpA.ap(0, 64, 0, [[1, 512]]), kkp.ap(0, 64, 0, [[1, 512]]), AF.Square, [kkp], [tmpA])
                P.red(sm.ap(0, 64, 0, [[1, 8]]), tmpA.ap(0, 64, 0, [[64, 8], [1, 64]]), OP.add, [tmpA], [sm])
                P.act(sm.ap(0, 64, 8, [[1, 8]]), sm.ap(0, 64, 0, [[1, 8]]), AF.Sqrt, [sm], [sm])
                P.ts(sm.ap(0, 64, 8, [[1, 8]]), sm.ap(0, 64, 8, [[1, 8]]), 1e-12, OP.max, [sm], [sm])
                P.recip(sm.ap(0, 64, 8, [[1, 8]]), sm.ap(0, 64, 8, [[1, 8]]), [sm], [sm])
                P.stt(nk.ap(0, 64, 0, [[64, 8], [1, 64]]), kkp.ap(0, 64, 0, [[64, 8], [1, 64]]), -1.0,
                      sm.ap(0, 64, 8, [[1, 8], [0, 64]]), OP.mult, OP.mult, [kkp, sm], [nk])
                P.tt(X3.ap(0, 64, 0, [[1, 512]]), nk.ap(0, 64, 0, [[1, 512]]), E3.ap(0, 64, 0, [[1, 512]]), OP.mult,
                     [nk, E3], [X3])
                P.stt(Q1.ap(0, 64, 0, [[1, 512]]), nk.ap(0, 64, 0, [[1, 512]]), -1.0, a_tm.ap(0, 64, 0, [[1, 512]]),
                      OP.mult, OP.mult, [nk, a_tm], [Q1])
                P.stt(tmpB.ap(64, 64, 0, [[1, 512]]), a_tm.ap(64, 64, 0, [[1, 512]]), 1.0, tps["ka"].ap(64, 64, 0, [[1, 512]]),
                      OP.subtract, OP.mult, [a_tm, tps["ka"]], [tmpB])
                P.tt(Q1.ap(64, 64, 0, [[1, 512]]), tmpB.ap(64, 64, 0, [[1, 512]]), tps["k"].ap(64, 64, 0, [[1, 512]]), OP.add,
                     [tmpB, tps["k"]], [Q1])
                P.tt(X3.ap(64, 64, 0, [[1, 512]]), tps["r"].ap(64, 64, 0, [[1, 512]]), E3.ap(64, 64, 0, [[1, 512]]), OP.mult,
                     [tps["r"], E3], [X3])
                P.copy(UV.ap(64, 64, 0, [[1, 512]]), tps["v"].ap(64, 64, 0, [[1, 512]]), [tps["v"]], [UV], eng="act")
                P.tt(tmpB.ap(64, 64, 0, [[1, 512]]), tps["rr"].ap(64, 64, 0, [[1, 512]]), Q1.ap(64, 64, 0, [[1, 512]]), OP.mult,
                     [tps["rr"], Q1], [tmpB])
                P.red(sm.ap(64, 64, 32, [[1, 8]]), tmpB.ap(64, 64, 0, [[64, 8], [1, 64]]), OP.add, [tmpB], [sm])
                P.tt(X2.t[:, :], Q1.t[:, :], E2.t[:, :], OP.mult, [Q1, E2], [X2])
                _, pt3 = psum()
                for h in range(8):
                    P.tr(pt3.ap(0, 64, h * 128, [[1, 128]]), X3.ap(0, 128, h * 64, [[1, 64]]), cba(CB_IDENT, 128),
                         [X3, cb], [pt3], signal=(h == 7))
                P.copy(X3T.ap(0, 64, 0, [[1, 1024]]), pt3.ap(0, 64, 0, [[1, 1024]]), [pt3], [X3T], eng="act")
                _, pt2 = psum()
                for h in range(8):
                    P.tr(pt2.ap(0, 64, h * 128, [[1, 128]]), X2.ap(0, 128, h * 64, [[1, 64]]), cba(CB_IDENT, 128),
                         [X2, cb], [pt2], signal=(h == 7))
                P.tt(X1T.ap(0, 64, 0, [[128, 8], [1, 128]]), pt2.ap(0, 64, 0, [[128, 8], [1, 128]]),
                     sm.ap(0, 64, 24, [[1, 8], [0, 128]]), OP.mult, [pt2, sm], [X1T])
                pS = [psum()[0], psum()[0]]
                for h in range(8):
                    pt = pS[h // 4]
                    P.mm(pt.ap(0, 128, (h % 4) * 128, [[1, 128]]), X1T.ap(0, 64, h * 128, [[1, 128]]),
                         X3T.ap(0, 64, h * 128, [[1, 128]]), True, True, [X1T, X3T], [pt], signal=(h % 4 == 3))
                for half in range(2):
                    P.tt(S_sb.ap(0, 128, half * 512, [[128, 4], [1, 128]]), pS[half].ap(0, 128, 0, [[128, 4], [1, 128]]),
                         cb.ap(0, 128, CB_M4, [[0, 4], [1, 128]]), OP.mult, [pS[half], cb], [S_sb])
                pA, _ = psum()
                for h in range(8):
                    P.mm(pA.ap(0, 64, h * 64, [[1, 64]]), X3T.ap(0, 64, h * 128, [[1, 64]]), X1T.ap(0, 64, h * 128, [[1, 64]]),
                         True, True, [X3T, X1T], [pA], signal=(h == 7))
                P.tt(Apow[0].ap(0, 64, 0, [[64, 8], [1, 64]]), pA.ap(0, 64, 0, [[64, 8], [1, 64]]),
                     cb.ap(0, 64, CB_MST, [[0, 8], [1, 64]]), OP.mult, [pA, cb], [Apow[0]])
                pX, _ = psum()
                pX2, _ = psum()
                for h in range(8):
                    P.mm(pX.ap(0, 64, h * 64, [[1, 64]]), X3T.ap(0, 64, h * 128, [[1, 64]]),
                         st_Hb[l].ap(0, 64, h * 64, [[1, 64]]), True, True, [X3T, st_Hb[l]], [pX], signal=(h == 7))
                for h in range(8):
                    P.mm(pX2.ap(0, 64, h * 64, [[1, 64]]), S_sb.ap(64, 64, h * 128, [[1, 64]]),
                         UV.ap(64, 64, h * 64, [[1, 64]]), True, True, [S_sb, UV], [pX2], signal=(h == 7))
                P.copy(Xf.ap(0, 64, 0, [[1, 512]]), pX.ap(0, 64, 0, [[1, 512]]), [pX], [Xf], eng="act")
                P.tt(Xf.ap(0, 64, 0, [[1, 512]]), Xf.ap(0, 64, 0, [[1, 512]]), pX2.ap(0, 64, 0, [[1, 512]]), OP.add,
                     [Xf, pX2], [Xf])
                P.copy(Xb.ap(0, 64, 0, [[1, 512]]), Xf.ap(0, 64, 0, [[1, 512]]), [Xf], [Xb], eng="act")
                for i in range(6):
                    if i == 0:
                        def Nap(h, c0=0, w=64):
                            return S_sb.ap(0, 64, h * 128 + c0, [[1, w]])
                        Nt = S_sb
                    else:
                        def Nap(h, c0=0, w=64, i=i):
                            return Npow[i % 2].ap(0, 64, h * 64 + c0, [[1, w]])
                        Nt = Npow[i % 2]
                    At = Apow[i % 2]
                    pY, _ = psum()
                    for h in range(8):
                        P.mm(pY.ap(0, 64, h * 64, [[1, 64]]), Nap(h), Xb.ap(0, 64, h * 64, [[1, 64]]), True, True,
                             [Nt, Xb], [pY], signal=(h == 7))
                    P.tt(Xf.ap(0, 64, 0, [[1, 512]]), Xf.ap(0, 64, 0, [[1, 512]]), pY.ap(0, 64, 0, [[1, 512]]), OP.add,
                         [Xf, pY], [Xf])
                    if i < 5:
                        P.copy(Xb.ap(0, 64, 0, [[1, 512]]), Xf.ap(0, 64, 0, [[1, 512]]), [Xf], [Xb], eng="act")
                        pN, _ = psum()
                        pA2, _ = psum()
                        for h in range(8):
                            P.mm(pN.ap(0, 64, h * 64, [[1, 64]]), At.ap(0, 64, h * 64, [[1, 64]]), Nap(h), True, True,
                                 [At, Nt], [pN], signal=(h == 7))
                        for h in range(8):
                            P.mm(pA2.ap(0, 64, h * 64, [[1, 64]]), Nap(h), At.ap(0, 64, h * 64, [[1, 64]]), True, True,
                                 [At, Nt], [pA2], signal=(h == 7))
                        P.copy(Npow[(i + 1) % 2].ap(0, 64, 0, [[1, 512]]), pN.ap(0, 64, 0, [[1, 512]]), [pN],
                               [Npow[(i + 1) % 2]], eng="act")
                        P.copy(Apow[(i + 1) % 2].ap(0, 64, 0, [[1, 512]]), pA2.ap(0, 64, 0, [[1, 512]]), [pA2],
                               [Apow[(i + 1) % 2]])
                    else:
                        P.copy(UV.ap(0, 64, 0, [[1, 512]]), Xf.ap(0, 64, 0, [[1, 512]]), [Xf], [UV], eng="act")
                pYo, _ = psum()
                for h in range(8):
                    o_ap = pYo.ap(64, 64, h * 64, [[1, 64]])
                    P.mm(o_ap, X3T.ap(0, 64, h * 128 + 64, [[1, 64]]), st_Hb[l].ap(0, 64, h * 64, [[1, 64]]), True, False,
                         [X3T, st_Hb[l]], [pYo], signal=False)
                    P.mm(o_ap, S_sb.ap(0, 128, h * 128 + 64, [[1, 64]]), UV.ap(0, 128, h * 64, [[1, 64]]), False, True,
                         [S_sb, UV], [pYo], signal=(h == 7))
                pH, _ = psum()
                for h in range(8):
                    P.mm(pH.ap(0, 64, h * 64, [[1, 64]]), X2.ap(0, 128, h * 64, [[1, 64]]), UV.ap(0, 128, h * 64, [[1, 64]]),
                         True, True, [X2, UV], [pH], signal=(h == 7))
                P.tt(st_H[l].ap(0, 64, 0, [[64, 8], [1, 64]]), st_H[l].ap(0, 64, 0, [[64, 8], [1, 64]]),
                     sm.ap(0, 64, 16, [[1, 8], [0, 64]]), OP.mult, [st_H[l], sm], [st_H[l]])
                P.tt(st_H[l].ap(0, 64, 0, [[1, 512]]), st_H[l].ap(0, 64, 0, [[1, 512]]), pH.ap(0, 64, 0, [[1, 512]]), OP.add,
                     [st_H[l], pH], [st_H[l]])
                P.copy(st_Hb[l].ap(0, 64, 0, [[1, 512]]), st_H[l].ap(0, 64, 0, [[1, 512]]), [st_H[l]], [st_Hb[l]], eng="act")
                R64 = (64, 64)
                P.copy(Ysb.ap(64, 64, 0, [[1, 512]]), pYo.ap(64, 64, 0, [[1, 512]]), [pYo], [Ysb], eng="act")
                P.red(sm.ap(64, 64, 40, [[1, 8]]), Ysb.ap(64, 64, 0, [[64, 8], [1, 64]]), OP.add, [Ysb], [sm])
                P.act(tmpA.ap(64, 64, 0, [[1, 512]]), Ysb.ap(64, 64, 0, [[1, 512]]), AF.Square, [Ysb], [tmpA])
                P.red(sm.ap(64, 64, 48, [[1, 8]]), tmpA.ap(64, 64, 0, [[64, 8], [1, 64]]), OP.add, [tmpA], [sm])
                P.ts(sm.ap(64, 64, 40, [[1, 8]]), sm.ap(64, 64, 40, [[1, 8]]), 1.0 / 64, OP.mult, [sm], [sm])
                P.tt(sm.ap(64, 64, 56, [[1, 8]]), sm.ap(64, 64, 40, [[1, 8]]), sm.ap(64, 64, 40, [[1, 8]]), OP.mult, [sm], [sm])
                P.stt(sm.ap(64, 64, 48, [[1, 8]]), sm.ap(64, 64, 48, [[1, 8]]), 1.0 / 64, sm.ap(64, 64, 56, [[1, 8]]),
                      OP.mult, OP.subtract, [sm], [sm])
                P.ts(sm.ap(64, 64, 48, [[1, 8]]), sm.ap(64, 64, 48, [[1, 8]]), 64e-5, OP.add, [sm], [sm])
                P.act(sm.ap(64, 64, 48, [[1, 8]]), sm.ap(64, 64, 48, [[1, 8]]), AF.Sqrt, [sm], [sm])
                P.recip(sm.ap(64, 64, 48, [[1, 8]]), sm.ap(64, 64, 48, [[1, 8]]), [sm], [sm])
                Y3 = Ysb.ap(64, 64, 0, [[64, 8], [1, 64]])
                P.tt(Y3, Y3, sm.ap(64, 64, 40, [[1, 8], [0, 64]]), OP.subtract, [Ysb, sm], [Ysb])
                P.tt(Y3, Y3, sm.ap(64, 64, 48, [[1, 8], [0, 64]]), OP.mult, [Ysb, sm], [Ysb])
                Y2 = Ysb.ap(64, 64, 0, [[1, 512]])
                P.tt(Y2, Y2, rowf.ap(64, 64, 0, [[1, 512]]), OP.mult, [Ysb, rowf], [Ysb])
                P.tt(Y2, Y2, rowf.ap(64, 64, 512, [[1, 512]]), OP.add, [Ysb, rowf], [Ysb])
                P.tt(tmpA.ap(64, 64, 0, [[64, 8], [1, 64]]), UV.ap(64, 64, 0, [[64, 8], [1, 64]]),
                     sm.ap(64, 64, 32, [[1, 8], [0, 64]]), OP.mult, [UV, sm], [tmpA])
                P.tt(Y2, Y2, tmpA.ap(64, 64, 0, [[1, 512]]), OP.add, [Ysb, tmpA], [Ysb])
                P.tt(obt.ap(64, 64, 0, [[1, 512]]), Y2, g_tm.ap(64, 64, 0, [[1, 512]]), OP.mult, [Ysb, g_tm], [obt])
                _, pto = psum()
                for h in range(8):
                    P.tr(pto.ap(0, 64, h * 64, [[1, 64]]), obt.ap(64, 64, h * 64, [[1, 64]]),
                         cb.ap(64, 64, CB_IDENT + 64, [[1, 64]]), [obt, cb], [pto], signal=(h == 7))
                P.copy(oT.ap(0, 64, t0, [[TB, 8], [1, 64]]), pto.ap(0, 64, 0, [[64, 8], [1, 64]]), [pto], [oT])

        for s in range(NSEQ):
            for tb in range(NTB):
                c0 = s * T + tb * TB
                first = (tb == 0)
                tbi[0] = tb
                P.dma("sp", xT.t[:, :, :], bass.AP(xT_d.tensor, c0, [[NT, 128], [128 * NT, KD], [1, TB]]), xT)
                for l in range(L):
                    if first:
                        for stt_ in (st_R[l], st_Rb[l], st_H[l], st_Hb[l], st_sh[l]):
                            P.memset(stt_.ap(0, stt_.shape[0], 0, [[1, stt_.rs]]), 0.0, [stt_])
                    P.dma("sp", rowf.t[:, :], bass.AP(rows_d.tensor, l * 2048 + 1024, [[0, 128], [1, 1024]]), rowf)
                    P.dma("pool", lwb.t[:, :], lw_d[:, l * 1536:(l + 1) * 1536], lwb)
                    P.dma("sp", rv33.t[0:33, :], bass.AP(rows_d.tensor, l * 2048, [[0, 33], [1, 1024]]), rv33)
                    P.copy(rhi.t[0:33, :], rv33.t[0:33, :], [rv33], [rhi])
                    P.tt(rv33.t[0:33, :], rv33.t[0:33, :], rhi.t[0:33, :], OP.subtract, [rv33, rhi], [rv33])
                    P.copy(HL.t[0:1, :], rhi.t[0:1, :], [rhi], [HL])
                    P.copy(HL.t[32:33, :], rv33.t[32:33, :], [rv33], [HL])
                    rmsnorm(l, 0)
                    import os as _os
                    SK = _os.environ.get("KSKIP", "")
                    if "a" not in SK:
                        attention(l, first)
                        merge_branch(l, 0)
                    else:
                        P.memset(mixed.ap(0, 128, 0, [[1, KD * TB]]), 0.0, [mixed])
                        if "m" in SK:
                            P.memset(oT.ap(0, 64, 0, [[1, 8 * TB]]), 0.0, [oT])
                            merge_branch(l, 0)
                    if "c" not in SK:
                        retention(l)
                        merge_branch(l, 2)
                    if "b" not in SK:
                        rwkv(l)
                        merge_branch(l, 1)
                    for hf in range(2):
                        w = wpiece(l, 22 + hf)

                        def resid(m, pt, hf=hf):
                            mi = hf * 4 + m
                            P.tt(xT.t[:, mi, :], xT.t[:, mi, :], pt.t[:, :], OP.add, [xT, pt], [xT])
                        proj_fm(w, 512, resid, src=mixed)
                    rmsnorm(l, 8)
                    actT = BIG
                    for i in range(6):
                        ncol = 512 if i < 5 else 256
                        wg = wpiece(l, 24 + 2 * i)
                        wu = wpiece(l, 25 + 2 * i)
                        for m in range(ncol // 128):
                            pg, _ = psum()
                            pu, _ = psum()
                            for k in range(KD):
                                P.mm(pg.t[:, :], wg.ap(0, 128, k * ncol + m * 128, [[1, 128]]), hT.t[:, k, :],
                                     k == 0, k == KD - 1, [wg, hT], [pg])
                            for k in range(KD):
                                P.mm(pu.t[:, :], wu.ap(0, 128, k * ncol + m * 128, [[1, 128]]), hT.t[:, k, :],
                                     k == 0, k == KD - 1, [wu, hT], [pu])
                            P.act(tmpA.t[:, :], pg.t[:, :], AF.Silu, [pg], [tmpA])
                            fi = i * 4 + m
                            P.tt(actT.ap(0, 128, fi * 512, [[1, 512]]), pu.t[:, :], tmpA.t[:, :], OP.mult, [pu, tmpA], [actT])
                    for m in range(8):
                        w = wpiece(l, 36 + m)
                        pt, _ = psum()
                        for k in range(KF):
                            P.mm(pt.t[:, :], w.ap(0, 128, k * 128, [[1, 128]]), actT.ap(0, 128, k * 512, [[1, 512]]),
                                 k == 0, k == KF - 1, [w, actT], [pt])
                        P.tt(xT.t[:, m, :], xT.t[:, m, :], pt.t[:, :], OP.add, [xT, pt], [xT])
                P.dma("sp", bass.AP(yT_d.tensor, c0, [[NT, 128], [128 * NT, KD], [1, TB]]), xT.t[:, :, :], xT, load=False)
        with nc.Block() as block:
            P.finish(block, [xT, oT])
        P.stats = {e: len(P.ins[e]) for e in P.ENGS}
        nc._prog_stats = (P.stats, P.nwaits, P.ndsem)
        nc._tags = P.tags
        nc._P = P
    return nc


def prep_inputs(inp, x_cores, T, L):
    consts = make_consts(T)
    wpk = np.stack([pack_layer(inp, l) for l in range(L)])
    vecs = pack_vecs(inp, L).reshape(128, L * NVEC)
    lw, rows, sinks = pack_small(inp, L)
    shared = {"wpk": wpk, "vecs": np.ascontiguousarray(vecs), "lw": np.ascontiguousarray(lw.reshape(128, L * 1536)),
              "rows": np.ascontiguousarray(rows.reshape(L, 2048)), "sinks": np.ascontiguousarray(sinks.reshape(1, L * 8)),
              "cf": consts["cf"], "cb": consts["cb"], "rot": consts["rot"]}
    maps = []
    for xc in x_cores:
        xT = np.ascontiguousarray(xc.reshape(-1, D).T)
        m = dict(shared)
        m["xT"] = xT
        maps.append(m)
    return maps


def kernel(**inputs):
    inp = {k: np.asarray(v, dtype=np.float32) for k, v in inputs.items()}
    x = inp["x"]
    B, T, _ = x.shape
    L = inp["w_in"].shape[0]
    ncores = 8
    nseq = B // ncores
    nc = build(nseq, T, L)
    maps = prep_inputs(inp, [x[c * nseq:(c + 1) * nseq] for c in range(ncores)], T, L)
    res = run_bass_kernel_spmd(nc, maps, core_ids=list(range(ncores)))
    out = np.empty((B, T, D), np.float32)
    for c in range(ncores):
        yT = np.asarray(res.results[c]["yT"])
        out[c * nseq:(c + 1) * nseq] = yT.T.reshape(nseq, T, D)
    return out
```

```python
from contextlib import ExitStack
import numpy as np
import concourse.bass as bass
import concourse.mybir as mybir
from concourse.bass_utils import run_bass_kernel_spmd

F32 = mybir.dt.float32
BF16 = mybir.dt.bfloat16
AF = mybir.ActivationFunctionType
OP = mybir.AluOpType
AX = mybir.AxisListType

D = 1024
KD = 8
TB = 512
FF = 2816
KF = 22
NPIECE = 44
PW = 4096
NVEC = 48
LOG_DECAY_C = -float(np.exp(-0.5))


class Buf:
    def __init__(self, name):
        self.name = name
        self.w = None
        self.r = {}
        self.dsem = None
        self.dcnt = 0


class Tl:
    def __init__(self, t, shape, buf=None, name=""):
        self.t = t
        self.shape = list(shape)
        self.rs = int(np.prod(shape[1:]))
        self.buf = buf or Buf(name)

    def ap(self, p0, npart, off, dims):
        return bass.AP(self.t, p0 * self.rs + off, [[self.rs, npart]] + [list(d) for d in dims])

    def __getitem__(self, idx):
        return self.t[idx]


class Prog:
    ENGS = ["pe", "dve", "act", "pool", "sp"]

    def __init__(self, nc, es):
        self.nc = nc
        self.es = es
        self.sem = {e: es.enter_context(nc.semaphore("s_" + e)) for e in self.ENGS}
        self.cnt = {e: 0 for e in self.ENGS}
        self.ins = {e: [] for e in self.ENGS}
        self.seen = {e: {} for e in self.ENGS}
        self.semobj = dict(self.sem)
        self.ndsem = 0
        self.nwaits = 0
        self.tags = {}

    def _waits(self, eng, deps):
        need = {}
        for d in deps:
            if d is None:
                continue
            k, v = d
            if k == eng and eng == "pe":
                continue
            if v > need.get(k, 0):
                need[k] = v
        out = []
        for k, v in need.items():
            if self.seen[eng].get(k, 0) >= v:
                continue
            self.seen[eng][k] = v
            out.append((self.semobj[k], v))
        self.nwaits += len(out)
        return out

    def op(self, eng, fn, reads=(), writes=(), signal=True):
        deps = []
        for b in reads:
            deps.append(b.buf.w)
        for b in writes:
            deps.append(b.buf.w)
            deps.extend(b.buf.r.items())
        waits = self._waits(eng, deps)
        val = self.cnt[eng] + 1
        if signal:
            self.cnt[eng] = val
        import sys as _sys
        fr = _sys._getframe(1)
        while fr.f_code.co_name in ("op", "mm", "tr", "act", "tt", "ts", "stt", "copy", "red", "memset", "recip", "<lambda>"):
            fr = fr.f_back
        tag = "%s:%d" % (fr.f_code.co_name, fr.f_lineno)
        self.ins[eng].append((waits, fn, (self.sem[eng], 1) if signal else None, tag))
        for b in reads:
            if b.buf.r.get(eng, 0) < val:
                b.buf.r[eng] = val
        for b in writes:
            b.buf.w = (eng, val)
            b.buf.r = {}

    def _dsem(self, buf):
        if buf.dsem is None:
            buf.dsem = "d%d" % self.ndsem
            self.ndsem += 1
            self.semobj[buf.dsem] = self.es.enter_context(self.nc.semaphore(buf.dsem))
        return buf.dsem

    def dma(self, q, out, in_, tile, load=True):
        b = tile.buf
        k = self._dsem(b)
        deps = [b.w]
        if load:
            deps.extend(b.r.items())
        waits = self._waits(q, deps)
        b.dcnt += 16
        self.ins[q].append((waits, lambda e: e.dma_start(out=out, in_=in_), (self.semobj[k], 16), "dma"))
        if load:
            b.w = (k, b.dcnt)
            b.r = {}
        else:
            b.r[k] = b.dcnt

    def inherit(self, dsts, srcs):
        acc = {}
        for s_ in srcs:
            items = list(s_.buf.r.items())
            if s_.buf.w is not None:
                items.append(s_.buf.w)
            for k, v in items:
                if v > acc.get(k, 0):
                    acc[k] = v
        for d_ in dsts:
            d_.buf.w = None
            d_.buf.r = dict(acc)

    def finish(self, block, final_bufs):
        waits = []
        for b in final_bufs:
            if b.buf.dsem is not None:
                waits.append((self.semobj[b.buf.dsem], b.buf.dcnt))
        self.ins["sp"].append((waits, None, None, "end"))
        engmap = {"pe": block.tensor, "dve": block.vector, "act": block.scalar,
                  "pool": block.gpsimd, "sp": block.sync}
        for e in self.ENGS:
            lst = self.ins[e]

            def body(eng, lst=lst):
                for waits, fn, inc, tag in lst:
                    for s, v in waits:
                        eng.wait_ge(s, v)
                    if fn is None:
                        continue
                    i = fn(eng)
                    try:
                        self.tags[str(i.ins.name)] = tag
                    except Exception:
                        pass
                    if inc is not None:
                        i.then_inc(inc[0], inc[1])
            engmap[e](body)

    def mm(self, out, lhsT, rhs, start, stop, reads, writes, signal=None):
        if signal is None:
            signal = stop
        self.op("pe", lambda e: e.matmul(out, lhsT=lhsT, rhs=rhs, start=start, stop=stop),
                reads, writes, signal)

    def tr(self, out, in_, ident, reads, writes, signal=True):
        self.op("pe", lambda e: e.transpose(out, in_, ident), reads, writes, signal)

    def act(self, out, in_, func, reads, writes, scale=1.0, bias=None):
        if bias is None:
            self.op("act", lambda e: e.activation(out=out, in_=in_, func=func, scale=scale), reads, writes)
        else:
            self.op("act", lambda e: e.activation(out=out, in_=in_, func=func, scale=scale, bias=bias),
                    reads, writes)

    def tt(self, out, in0, in1, op, reads, writes, eng="dve"):
        self.op(eng, lambda e: e.tensor_tensor(out=out, in0=in0, in1=in1, op=op), reads, writes)

    def ts(self, out, in0, s1, op0, reads, writes, s2=None, op1=None, eng="dve"):
        if op1 is None:
            self.op(eng, lambda e: e.tensor_scalar(out=out, in0=in0, scalar1=s1, scalar2=None, op0=op0),
                    reads, writes)
        else:
            self.op(eng, lambda e: e.tensor_scalar(out=out, in0=in0, scalar1=s1, scalar2=s2, op0=op0, op1=op1),
                    reads, writes)

    def stt(self, out, in0, scalar, in1, op0, op1, reads, writes):
        self.op("dve", lambda e: e.scalar_tensor_tensor(out=out, in0=in0, scalar=scalar, in1=in1,
                                                        op0=op0, op1=op1), reads, writes)

    def copy(self, out, in_, reads, writes, eng="dve"):
        if eng == "act":
            self.op("act", lambda e: e.copy(out=out, in_=in_), reads, writes)
        else:
            self.op(eng, lambda e: e.tensor_copy(out=out, in_=in_), reads, writes)

    def red(self, out, in_, op, reads, writes):
        self.op("dve", lambda e: e.tensor_reduce(out=out, in_=in_, op=op, axis=AX.X), reads, writes)

    def memset(self, ap, val, writes, eng="dve"):
        self.op(eng, lambda e: e.memset(ap, val), [], writes)

    def recip(self, out, in_, reads, writes):
        self.op("dve", lambda e: e.reciprocal(out=out, in_=in_), reads, writes)


def _bf(a):
    import ml_dtypes
    return np.asarray(a, dtype=np.float32).astype(ml_dtypes.bfloat16)


def make_consts(T):
    c = {}
    idx = np.arange(128)
    ident = np.eye(128, dtype=np.float32)
    s = np.arange(64)[:, None]
    t = np.arange(64)[None, :]
    tri1 = np.concatenate([(s < t), (s <= t)], axis=1).astype(np.float32)
    tri3 = np.concatenate([(s > t), (s > t)], axis=1).astype(np.float32)
    tri = np.zeros((128, 256), np.float32)
    tri[:64, :128] = tri1 * LOG_DECAY_C
    tri[:64, 128:] = tri3 * LOG_DECAY_C
    half = 32
    inv_freq = 1.0 / (10000.0 ** (np.arange(half, dtype=np.float32) * 2.0 / 64))
    pos = np.arange(T, dtype=np.float32)
    ang = pos[None, :] * inv_freq[:, None]
    cosT = np.cos(ang)[idx % 32]
    sinT = np.sin(ang)[idx % 32] * np.where((idx % 64) < 32, -1.0, 1.0)[:, None]
    H = 8
    log_gamma = np.log1p(-np.power(2.0, -5.0 - np.arange(H, dtype=np.float64)))
    i = np.arange(128, dtype=np.float64)
    xi = np.exp(log_gamma[:, None] * (i[None, :] + 1.0))
    kf = (64 ** -0.5) * np.exp(-log_gamma[:, None] * (i[None, :] + 1.0))
    gc = np.exp(log_gamma * 128.0)
    XI = np.zeros((128, 4, 128), np.float32)
    KFt = np.zeros((128, 4, 128), np.float32)
    GC = np.zeros((128, 4), np.float32)
    for h in range(H):
        rows = slice((h % 2) * 64, (h % 2) * 64 + 64)
        XI[rows, h // 2, :] = xi[h][None, :]
        KFt[rows, h // 2, :] = kf[h][None, :]
        GC[rows, h // 2] = gc[h]
    ones = np.full((128, 64), LOG_DECAY_C, np.float32)
    c["cf"] = np.concatenate([ident, tri, XI.reshape(128, -1), KFt.reshape(128, -1), GC, ones], axis=1)
    c["rot"] = np.concatenate([cosT, sinT], axis=1).astype(np.float32)
    identb = np.eye(128, dtype=np.float32)
    blk64 = (idx[:, None] // 64 == idx[None, :] // 64).astype(np.float32) / 64.0
    onesD = np.full((128, 128), 1.0 / 1024.0, np.float32)
    mdiag = (idx[:, None] <= idx[None, :]).astype(np.float32)
    mprev = (idx[:, None] > idx[None, :]).astype(np.float32)
    perm = np.zeros((128, 128), np.float32)
    for p in range(128):
        q = p + 32 if (p % 64) < 32 else p - 32
        perm[q, p] = 1.0
    m4 = np.zeros((128, 128), np.float32)
    ss = np.arange(64)[:, None]
    tt = np.arange(64)[None, :]
    for w in range(2):
        m4[w * 64:(w + 1) * 64, 0:64] = (ss < tt)
        m4[w * 64:(w + 1) * 64, 64:128] = (ss <= tt)
    mst = np.zeros((128, 64), np.float32)
    mst[:64] = (np.arange(64)[:, None] > np.arange(64)[None, :])
    ones128 = np.ones((128, 128), np.float32)
    c["cb"] = _bf(np.concatenate([identb, blk64, onesD, mdiag, mprev, perm, m4, mst, ones128], axis=1))
    return c


CF_IDENT, CF_TRI, CF_XI, CF_KF, CF_GC, CF_ONES = 0, 128, 384, 896, 1408, 1412
CF_W = 1412 + 64
CB_IDENT, CB_BLK, CB_OND, CB_MD, CB_MP, CB_PERM, CB_M4, CB_MST, CB_ONES = 0, 128, 256, 384, 512, 640, 768, 896, 960
CB_W = 960 + 128


def _fm(W):
    K, N = W.shape
    kc = K // 128
    out = np.zeros((128, PW), np.float32)
    out[:, :kc * N] = W.reshape(kc, 128, N).transpose(1, 0, 2).reshape(128, kc * N)
    return out


def _hm(W, c0):
    out = np.zeros((128, PW), np.float32)
    out[:64] = W[:, c0:c0 + 512].reshape(8, 64, 512).transpose(1, 0, 2).reshape(64, 4096)
    return out


def pack_layer(inp, l):
    w_in = inp["w_in"][l]
    P = []
    P.append(_fm(w_in[:, 0:512]))
    akv = np.concatenate([w_in[:, 512:576], w_in[:, 512:576], w_in[:, 576:640], w_in[:, 576:640],
                          w_in[:, 640:768]], axis=1)
    P.append(_fm(akv))
    P.append(_fm(w_in[:, 2560:3072]))
    P.append(_fm(w_in[:, 3072:3584]))
    P.append(_fm(w_in[:, 3584:4096]))
    P.append(_fm(w_in[:, 4096:4608]))
    P.append(_fm(w_in[:, 768:1280]))
    P.append(_fm(w_in[:, 1280:1792]))
    P.append(_fm(w_in[:, 1792:2304]))
    P.append(_fm(w_in[:, 2304:2560]))
    for b, wo in enumerate([inp["w_attn_o"][l], inp["w_rwkv_o"][l], inp["w_ret_o"][l]]):
        for hf in range(2):
            P.append(_hm(wo, hf * 512))
            c0 = 4608 + b * 1024 + hf * 512
            P.append(_fm(w_in[:, c0:c0 + 512]))
    for hf in range(2):
        P.append(_fm(inp["w_out"][l][:, hf * 512:(hf + 1) * 512]))
    for i in range(6):
        c0 = i * 512
        c1 = min(c0 + 512, FF)
        P.append(_fm(inp["w_ffn_gate"][l][:, c0:c1]))
        P.append(_fm(inp["w_ffn_up"][l][:, c0:c1]))
    wd = inp["w_ffn_down"][l]
    for m in range(8):
        P.append(_fm(wd[:, m * 128:(m + 1) * 128]))
    assert len(P) == NPIECE
    return np.stack(P)


def pack_vecs(inp, L):
    v = np.zeros((128, L, NVEC), np.float32)
    idx = np.arange(128)

    def fm(a):
        return a.reshape(-1, 128).T

    for l in range(L):
        v[:, l, 0:8] = fm(inp["norm1_g"][l])
        v[:, l, 8:16] = fm(inp["norm2_g"][l])
        mu = inp["rwkv_shift_mu"][l]
        v[:, l, 16:28] = fm(mu[0:1536])
        v[:64, l, 28] = mu[1536:1600]
        v[:64, l, 29] = mu[1600:1664]
        v[:, l, 30] = mu[1664:1792]
        v[:, l, 31:35] = fm(inp["rwkv_k_k"][l])
        v[:, l, 35:39] = fm(inp["rwkv_k_a"][l])
        v[:, l, 39:43] = fm(inp["rwkv_r_k"][l].reshape(-1))
        v[:, l, 43] = inp["attn_q_norm_g"][l][idx % 64]
        v[:, l, 44] = inp["attn_k_norm_g"][l][idx % 64]
    return v


def pack_small(inp, L):
    lw = np.zeros((128, L, 3, 512), np.float32)
    rows = np.zeros((L, 4, 512), np.float32)
    for l in range(L):
        lw[:64, l, 0] = inp["rwkv_w2"][l]
        lw[:64, l, 1] = inp["rwkv_a2"][l]
        lw[:, l, 2] = inp["rwkv_g2"][l]
        rows[l, 0] = inp["rwkv_w0"][l]
        rows[l, 1] = inp["rwkv_a0"][l]
        rows[l, 2] = inp["rwkv_lnx_g"][l]
        rows[l, 3] = inp["rwkv_lnx_b"][l]
    sinks = np.asarray(inp["attn_sinks"], np.float32)[:L].reshape(L, 8)
    return lw, rows, sinks


def build(NSEQ, T, L, debug=False):
    nc = bass.Bass("TRN2", target_bir_lowering=False)
    NTB = T // TB
    NT = NSEQ * T
    xT_d = nc.dram_tensor("xT", [D, NT], F32, kind="ExternalInput").ap()
    wpk_d = nc.dram_tensor("wpk", [L, NPIECE, 128, PW], F32, kind="ExternalInput").ap()
    vec_d = nc.dram_tensor("vecs", [128, L * NVEC], F32, kind="ExternalInput").ap()
    lw_d = nc.dram_tensor("lw", [128, L * 1536], F32, kind="ExternalInput").ap()
    rows_d = nc.dram_tensor("rows", [L, 2048], F32, kind="ExternalInput").ap()
    sink_d = nc.dram_tensor("sinks", [1, L * 8], F32, kind="ExternalInput").ap()
    cf_d = nc.dram_tensor("cf", [128, CF_W], F32, kind="ExternalInput").ap()
    cb_d = nc.dram_tensor("cb", [128, CB_W], BF16, kind="ExternalInput").ap()
    rot_d = nc.dram_tensor("rot", [128, 2 * T], F32, kind="ExternalInput").ap()
    yT_d = nc.dram_tensor("yT", [D, NT], F32, kind="ExternalOutput").ap()
    dbg_d = None
    if debug:
        dbg_d = nc.dram_tensor("dbg", [3, 64, 8 * TB], BF16, kind="ExternalOutput").ap()

    es = ExitStack()
    with es:
        P = Prog(nc, es)

        def sb(name, shape, dt=F32):
            return Tl(es.enter_context(nc.sbuf_tensor("s_" + name, list(shape), dt)), shape, name=name)

        def view(base, name, shape, dt, col0_bytes):
            raise NotImplementedError

        rvtm = sb("rvtm", [128, 4, 512], BF16)
        cf = sb("cf", [128, CF_W])
        cb = sb("cb", [128, CB_W], BF16)
        rotc = sb("rotb", [128, 2 * TB], BF16)
        vecs = sb("vecs", [128, L * NVEC])
        lwb = sb("lwb", [128, 1536], BF16)
        rowf = sb("rowf", [128, 1024])
        rv33 = sb("rv33", [64, 1024])
        HL = sb("HL", [64, 1024], BF16)
        sinkx = sb("sinkx", [128, L * 8])
        eps6 = sb("eps6", [128, 1])
        P.dma("sp", cf.t[:, :], cf_d, cf)
        P.dma("sp", cb.t[:, :], cb_d, cb)
        P.dma("sp", vecs.t[:, :], vec_d, vecs)
        P.dma("sp", sinkx.t[:, :], bass.AP(sink_d.tensor, 0, [[0, 128], [1, L * 8]]), sinkx)
        P.act(sinkx.t[:, :], sinkx.t[:, :], AF.Exp, [sinkx], [sinkx])
        P.memset(eps6.t[:, :], 1e-6, [eps6])
        P.memset(HL.t[:, :], 0.0, [HL])

        def cba(col, w, p0=0, npart=128):
            return cb.ap(p0, npart, col, [[1, w]])

        def vcol(l, j, p0=0, npart=128):
            return vecs.ap(p0, npart, l * NVEC + j, [[1, 1]])

        xT = sb("xT", [128, KD, TB])
        hT = sb("hT", [128, KD, TB], BF16)
        NW = 3
        wring = [sb("w%d" % i, [128, PW], BF16) for i in range(NW)]
        ps = [Tl(es.enter_context(nc.psum_tensor("ps%d" % i, [128, 512], F32)), [128, 512], name="ps%d" % i)
              for i in range(8)]
        psb = [Tl(p.t.bitcast(BF16), [128, 1024], buf=p.buf) for p in ps]
        pctr = [0]

        def psum():
            i = pctr[0] % 8
            pctr[0] += 1
            return ps[i], psb[i]

        oT = sb("oT", [64, 8, TB], BF16)
        B1 = sb("B1", [128, 4096], BF16)
        BIG = sb("BIG", [128, 3 * 4096], BF16)
        mixed = sb("mixed", [128, KD, TB], BF16)
        tmpA = sb("tmpA", [128, TB])
        tmpB = sb("tmpB", [128, TB])
        tmpD = sb("tmpD", [128, TB], BF16)
        rgtm = sb("rgtm", [128, 4, 512], BF16)
        vtm = sb("vtm", [128, 4, 128], BF16)
        PT = [sb("PT0", [128, 1024], BF16), None]
        ktm = sb("ktm", [128, 4, 512], BF16)
        sT = sb("sT", [128, 1024], BF16)
        PT[1] = sT
        rhi = sT
        rwsb = sb("rwsb", [128, TB + 1])
        wdx = sb("wdx", [64, 2 * TB], BF16)
        adx = sb("adx", [64, 2 * TB], BF16)
        gdx = sb("gdx", [128, 2 * TB], BF16)
        a_tm = sb("a_tm", [128, 512])
        sig = sb("sig", [128, 512])
        g_tm = sb("g_tm", [128, 512])
        E3 = sb("E3", [128, 512])
        E2 = sb("E2", [128, 512])
        Q1 = sb("Q1", [128, 512])
        nk = sb("nk", [128, 512])
        Ysb = nk
        rstd = tmpB
        X3 = sb("X3", [128, 512], BF16)
        X2 = sb("X2", [128, 512], BF16)
        UV = sb("UV", [128, 512], BF16)
        obt = sb("obt", [128, 512], BF16)
        octm = obt
        X3T = sb("X3T", [64, 8, 128], BF16)
        X1T = sb("X1T", [64, 8, 128], BF16)
        S_sb = sb("S_sb", [128, 8, 128], BF16)
        Apow = [sb("Apow%d" % i, [64, 8, 64], BF16) for i in range(2)]
        Npow = [sb("Npow%d" % i, [64, 8, 64], BF16) for i in range(2)]
        Xf = sb("Xf", [64, 8, 64])
        Xb = sb("Xb", [64, 8, 64], BF16)
        sm = sb("sm", [128, 64])
        st_k = [sb("stk%d" % l, [128, 2, 128], BF16) for l in range(L)]
        st_v = [sb("stv%d" % l, [128, 128], BF16) for l in range(L)]
        st_R = [sb("stR%d" % l, [128, 4, 64]) for l in range(L)]
        st_Rb = [sb("stRb%d" % l, [128, 8, 64], BF16) for l in range(L)]
        st_H = [sb("stH%d" % l, [64, 8, 64]) for l in range(L)]
        st_Hb = [sb("stHb%d" % l, [64, 8, 64], BF16) for l in range(L)]
        st_sh = [sb("stsh%d" % l, [128, 16]) for l in range(L)]

        wq = {"i": 0}
        tbi = [0]

        def wpiece(l, j):
            tl = wring[wq["i"] % NW]
            wq["i"] += 1
            P.dma("pool", tl.ap(0, 128, 0, [[2048, 2], [1, 2048]]),
                  bass.AP(wpk_d.tensor, (l * NPIECE + j) * 128 * PW, [[PW, 128], [2048, 2], [1, 2048]]), tl)
            return tl

        def rmsnorm(l, gcol):
            sq = BIG
            P.act(sq.ap(0, 128, 0, [[1, KD * TB]]), xT.ap(0, 128, 0, [[1, KD * TB]]), AF.Square, [xT], [sq])
            pt, _ = psum()
            for k in range(KD):
                P.mm(pt.t[:, :], cba(CB_OND, 128), sq.ap(0, 128, k * TB, [[1, TB]]), k == 0, k == KD - 1, [cb, sq], [pt])
            P.act(rstd.t[:, :], pt.t[:, :], AF.Ln, [pt, eps6], [rstd], bias=eps6.t[:, 0:1])
            P.act(rstd.t[:, :], rstd.t[:, :], AF.Exp, [rstd], [rstd], scale=-0.5)
            for k in range(KD):
                P.stt(hT.t[:, k, :], xT.t[:, k, :], vcol(l, gcol + k), rstd.t[:, :], OP.mult, OP.mult,
                      [xT, vecs, rstd], [hT])

        def proj_fm(w, ncol, cb_fn, src=None, M=128):
            src = src or hT
            for m in range(ncol // M):
                pt, ptb = psum()
                for k in range(KD):
                    P.mm(pt.ap(0, M, 0, [[1, TB]]), w.ap(0, 128, k * ncol + m * M, [[1, M]]),
                         src.t[:, k, :], k == 0, k == KD - 1, [w, src], [pt])
                cb_fn(m, pt)

        def headnorm(pt, dst_ap, gcolap, dstT):
            P.act(tmpD.t[:, :], pt.t[:, :], AF.Square, [pt], [tmpD])
            p2, _ = psum()
            P.mm(p2.t[:, :], cba(CB_BLK, 128), tmpD.t[:, :], True, True, [cb, tmpD], [p2])
            P.act(tmpA.t[:, :], p2.t[:, :], AF.Ln, [p2, eps6], [tmpA], bias=eps6.t[:, 0:1])
            P.act(tmpA.t[:, :], tmpA.t[:, :], AF.Exp, [tmpA], [tmpA], scale=-0.5)
            P.stt(dst_ap, pt.t[:, :], gcolap, tmpA.t[:, :], OP.mult, OP.mult, [pt, vecs, tmpA], [dstT])

        def merge_branch(l, b):
            for hf in range(2):
                wo = wpiece(l, 10 + b * 4 + hf * 2)
                wg = wpiece(l, 11 + b * 4 + hf * 2)
                for m in range(4):
                    pg, _ = psum()
                    for k in range(KD):
                        P.mm(pg.t[:, :], wg.ap(0, 128, k * 512 + m * 128, [[1, 128]]), hT.t[:, k, :],
                             k == 0, k == KD - 1, [wg, hT], [pg])
                    po, _ = psum()
                    for h in range(8):
                        P.mm(po.t[:, :], wo.ap(0, 64, h * 512 + m * 128, [[1, 128]]), oT.t[:, h, :],
                             h == 0, h == 7, [wo, oT], [po])
                    P.act(tmpA.t[:, :], pg.t[:, :], AF.Sigmoid, [pg], [tmpA])
                    mi = hf * 4 + m
                    if b == 0:
                        P.tt(mixed.t[:, mi, :], po.t[:, :], tmpA.t[:, :], OP.mult, [po, tmpA], [mixed])
                    else:
                        P.tt(tmpB.t[:, :], po.t[:, :], tmpA.t[:, :], OP.mult, [po, tmpA], [tmpB])
                        P.tt(mixed.t[:, mi, :], mixed.t[:, mi, :], tmpB.t[:, :], OP.add, [mixed, tmpB], [mixed])
            if debug:
                P.dma("sp", dbg_d[b], oT.ap(0, 64, 0, [[1, 8 * TB]]), oT, load=False)

        class _Stop(Exception):
            pass

        def stage(i):
            import os as _os
            if int(_os.environ.get("KSTOP", "99")) < i:
                raise _Stop()

        def attention(l, first):
            try:
                attention_(l, first)
            except _Stop:
                pass

        def attention_(l, first):
            import os as _os
            if "KSTOP" in _os.environ:
                P.memset(oT.ap(0, 64, 0, [[1, 8 * TB]]), 0.0, [oT])
            w = wpiece(l, 0)
            proj_fm(w, 512, lambda m, pt: headnorm(pt, B1.ap(0, 128, m * 512, [[1, 512]]), vcol(l, 43), B1))
            stage(2)
            w = wpiece(l, 1)
            for m in range(2):
                pt, _ = psum()
                for k in range(KD):
                    P.mm(pt.t[:, :], w.ap(0, 128, k * 384 + m * 128, [[1, 128]]), hT.t[:, k, :],
                         k == 0, k == KD - 1, [w, hT], [pt])
                headnorm(pt, B1.ap(0, 128, 2048 + m * 512, [[1, 512]]), vcol(l, 44), B1)
            stage(3)
            for n in range(4):
                pt, _ = psum()
                for k in range(KD):
                    P.mm(pt.ap(0, 128, 0, [[1, 128]]), hT.t[:, k, n * 128:(n + 1) * 128],
                         w.ap(0, 128, k * 384 + 256, [[1, 128]]), k == 0, k == KD - 1, [w, hT], [pt])
                P.copy(vtm.t[:, n, :], pt.t[:, 0:128], [pt], [vtm], eng="act")
            stage(4)
            for n in range(4):
                blocks = []
                if not (first and n == 0):
                    blocks.append(0)
                blocks.append(1)
                pts = {}
                for jb in blocks:
                    pa, _ = psum()
                    pb, _ = psum()
                    for h in range(8):
                        g = h // 4
                        base = (h % 2) * 64
                        if jb == 1:
                            kap = B1.ap(base, 64, 2048 + g * 512 + n * 128, [[1, 128]])
                            kr = [B1]
                        elif n == 0:
                            kap = st_k[l].ap(base, 64, g * 128, [[1, 128]])
                            kr = [st_k[l]]
                        else:
                            kap = B1.ap(base, 64, 2048 + g * 512 + (n - 1) * 128, [[1, 128]])
                            kr = [B1]
                        qap = B1.ap(base, 64, (h // 2) * 512 + n * 128, [[1, 128]])
                        pt = pa if h % 2 == 0 else pb
                        P.mm(pt.ap(0, 128, (h // 2) * 128, [[1, 128]]), kap, qap, True, True,
                             kr + [B1], [pt], signal=(h // 2 == 3))
                    pts[jb] = (pa, pb)
                stage(5)
                for jb in blocks:
                    for par, pt in enumerate(pts[jb]):
                        P.act(PT[jb].ap(0, 128, par * 128, [[256, 4], [1, 128]]), pt.ap(0, 128, 0, [[128, 4], [1, 128]]),
                              AF.Exp, [pt], [PT[jb]], scale=0.125)
                    stage(6)
                    mcol = CB_MP if jb == 0 else CB_MD
                    P.tt(PT[jb].ap(0, 128, 0, [[128, 8], [1, 128]]), PT[jb].ap(0, 128, 0, [[128, 8], [1, 128]]),
                         cb.ap(0, 128, mcol, [[0, 8], [1, 128]]), OP.mult, [PT[jb], cb], [PT[jb]])
                stage(7)
                for g in range(2):
                    po, _ = psum()
                    pd, _ = psum()
                    for bi, jb in enumerate(blocks):
                        if jb == 1:
                            vap = vtm.ap(0, 128, n * 128 + g * 64, [[1, 64]])
                            vr = [vtm]
                        elif n == 0:
                            vap = st_v[l].ap(0, 128, g * 64, [[1, 64]])
                            vr = [st_v[l]]
                        else:
                            vap = vtm.ap(0, 128, (n - 1) * 128 + g * 64, [[1, 64]])
                            vr = [vtm]
                        rhs = PT[jb].ap(0, 128, g * 512, [[1, 512]])
                        P.mm(po.ap(0, 64, 0, [[1, 512]]), vap, rhs, bi == 0, bi == len(blocks) - 1, vr + [PT[jb]], [po])
                        P.mm(pd.ap(0, 64, 0, [[1, 512]]), cba(CB_ONES, 64), rhs, bi == 0, bi == len(blocks) - 1,
                             [cb, PT[jb]], [pd])
                    stage(8)
                    P.tt(tmpA.ap(0, 64, 0, [[128, 4], [1, 128]]), pd.ap(0, 64, 0, [[128, 4], [1, 128]]),
                         sinkx.ap(0, 64, l * 8 + g * 4, [[1, 4], [0, 128]]), OP.add, [pd, sinkx], [tmpA])
                    P.recip(tmpA.ap(0, 64, 0, [[1, 512]]), tmpA.ap(0, 64, 0, [[1, 512]]), [tmpA], [tmpA])
                    P.tt(oT.ap(0, 64, g * 4 * TB + n * 128, [[TB, 4], [1, 128]]),
                         po.ap(0, 64, 0, [[128, 4], [1, 128]]), tmpA.ap(0, 64, 0, [[128, 4], [1, 128]]),
                         OP.mult, [po, tmpA], [oT])
            for g in range(2):
                P.copy(st_k[l].t[:, g, :], B1.ap(0, 128, 2048 + g * 512 + 384, [[1, 128]]), [B1], [st_k[l]], eng="act")
            P.copy(st_v[l].t[:, :], vtm.t[:, 3, :], [vtm], [st_v[l]], eng="act")

        def retention(l):
            try:
                retention_(l)
            except _Stop:
                pass

        def retention_(l):
            import os as _os
            if "KSTOP" in _os.environ:
                P.memset(oT.ap(0, 64, 0, [[1, 8 * TB]]), 0.0, [oT])

            def rotary(pt, dst_ap, fac_col, m):
                P.copy(tmpD.t[:, :], pt.t[:, :], [pt], [tmpD], eng="act")
                p2, _ = psum()
                P.mm(p2.t[:, :], cba(CB_PERM, 128), tmpD.t[:, :], True, True, [cb, tmpD], [p2])
                import os as _os
                KR = _os.environ.get("KROT", "")
                if KR == "1":
                    P.copy(tmpA.t[:, :], p2.t[:, :], [p2], [tmpA])
                    P.copy(dst_ap, tmpA.ap(0, 128, 0, [[128, 4], [1, 128]]), [tmpA], [B1])
                    return
                if KR == "3":
                    P.tt(tmpA.t[:, :], pt.t[:, :], cf.ap(0, 128, 0, [[1, 512]]), OP.mult, [pt, cf], [tmpA])
                    P.tt(tmpB.t[:, :], p2.t[:, :], cf.ap(0, 128, 512, [[1, 512]]), OP.mult, [p2, cf], [tmpB])
                else:
                    P.copy(E3.t[:, :], pt.t[:, :], [pt], [E3], eng="act")
                    P.tt(tmpA.t[:, :], E3.t[:, :], rotc.ap(0, 128, 0, [[1, TB]]), OP.mult, [E3, rotc], [tmpA])
                    P.tt(tmpB.t[:, :], p2.t[:, :], rotc.ap(0, 128, TB, [[1, TB]]), OP.mult, [p2, rotc], [tmpB])
                P.tt(tmpA.t[:, :], tmpA.t[:, :], tmpB.t[:, :], OP.add, [tmpA, tmpB], [tmpA])
                if KR == "2":
                    P.copy(dst_ap, tmpA.ap(0, 128, 0, [[128, 4], [1, 128]]), [tmpA], [B1])
                    return
                P.tt(dst_ap, tmpA.ap(0, 128, 0, [[128, 4], [1, 128]]),
                     cf.ap(0, 128, fac_col + m * 128, [[0, 4], [1, 128]]), OP.mult, [tmpA, cf], [B1])

            w = wpiece(l, 2)
            proj_fm(w, 512, lambda m, pt: rotary(pt, B1.ap(0, 128, m * 512, [[128, 4], [1, 128]]), CF_XI, m))
            stage(10)
            w = wpiece(l, 3)
            proj_fm(w, 512, lambda m, pt: rotary(pt, B1.ap(0, 128, 2048 + m * 512, [[128, 4], [1, 128]]), CF_KF, m))
            stage(11)
            for n in range(4):
                _, ptb = psum()
                for m in range(4):
                    P.tr(ptb.ap(0, 128, m * 128, [[1, 128]]), B1.ap(0, 128, 2048 + m * 512 + n * 128, [[1, 128]]),
                         cba(CB_IDENT, 128), [B1, cb], [ptb], signal=(m == 3))
                P.copy(ktm.t[:, n, :], ptb.t[:, 0:512], [ptb], [ktm], eng="act")
            stage(12)
            w = wpiece(l, 4)
            for n in range(4):
                pt, _ = psum()
                for k in range(KD):
                    P.mm(pt.t[:, :], hT.t[:, k, n * 128:(n + 1) * 128], w.ap(0, 128, k * 512, [[1, 512]]),
                         k == 0, k == KD - 1, [w, hT], [pt])
                P.copy(rvtm.t[:, n, :], pt.t[:, :], [pt], [rvtm], eng="act")
            w = wpiece(l, 5)
            for n in range(4):
                pt, _ = psum()
                for k in range(KD):
                    P.mm(pt.t[:, :], hT.t[:, k, n * 128:(n + 1) * 128], w.ap(0, 128, k * 512, [[1, 512]]),
                         k == 0, k == KD - 1, [w, hT], [pt])
                P.act(rgtm.t[:, n, :], pt.t[:, :], AF.Silu, [pt], [rgtm])
            stage(13)
            for n in range(4):
                pa, _ = psum()
                pb, _ = psum()
                for h in range(8):
                    base = (h % 2) * 64
                    kap = B1.ap(base, 64, 2048 + (h // 2) * 512 + n * 128, [[1, 128]])
                    qap = B1.ap(base, 64, (h // 2) * 512 + n * 128, [[1, 128]])
                    pt = pa if h % 2 == 0 else pb
                    P.mm(pt.ap(0, 128, (h // 2) * 128, [[1, 128]]), kap, qap, True, True, [B1], [pt], signal=(h // 2 == 3))
                for par, pt in enumerate((pa, pb)):
                    P.act(sT.ap(0, 128, par * 128, [[256, 4], [1, 128]]), pt.ap(0, 128, 0, [[128, 4], [1, 128]]),
                          AF.Copy, [pt], [sT])
                P.tt(sT.ap(0, 128, 0, [[128, 8], [1, 128]]), sT.ap(0, 128, 0, [[128, 8], [1, 128]]),
                     cb.ap(0, 128, CB_MD, [[0, 8], [1, 128]]), OP.mult, [sT, cb], [sT])
                stage(14)
                po, _ = psum()
                for h in range(8):
                    o_ap = po.ap(0, 128, h * 64, [[1, 64]])
                    P.mm(o_ap, sT.ap(0, 128, h * 128, [[1, 128]]), rvtm.ap(0, 128, n * 512 + h * 64, [[1, 64]]),
                         True, False, [rvtm, sT], [po], signal=False)
                    P.mm(o_ap, B1.ap(0, 128, (h // 2) * 512 + n * 128, [[1, 128]]), st_Rb[l].ap(0, 128, h * 64, [[1, 64]]),
                         False, True, [st_Rb[l], B1], [po], signal=(h == 7))
                stage(15)
                pk0, _ = psum()
                pk1, _ = psum()
                for h in range(8):
                    base = (h % 2) * 64
                    pk = pk0 if h % 2 == 0 else pk1
                    P.mm(pk.ap(base, 64, (h // 2) * 64, [[1, 64]]), ktm.ap(0, 128, n * 512 + h * 64, [[1, 64]]),
                         rvtm.ap(0, 128, n * 512 + h * 64, [[1, 64]]), True, True, [ktm, rvtm], [pk], signal=(h >= 6))
                P.tt(st_R[l].ap(0, 64, 0, [[64, 4], [1, 64]]), st_R[l].ap(0, 64, 0, [[64, 4], [1, 64]]),
                     pk0.ap(0, 64, 0, [[64, 4], [1, 64]]), OP.add, [st_R[l], pk0], [st_R[l]])
                P.tt(st_R[l].ap(64, 64, 0, [[64, 4], [1, 64]]), st_R[l].ap(64, 64, 0, [[64, 4], [1, 64]]),
                     pk1.ap(64, 64, 0, [[64, 4], [1, 64]]), OP.add, [st_R[l], pk1], [st_R[l]])
                P.tt(st_R[l].t[:, :, :], st_R[l].t[:, :, :], cf.ap(0, 128, CF_GC, [[1, 4], [0, 64]]), OP.mult,
                     [st_R[l], cf], [st_R[l]])
                stage(16)
                P.act(tmpA.t[:, :], po.t[:, :], AF.Square, [po], [tmpA])
                P.red(sm.ap(0, 128, 0, [[1, 8]]), tmpA.ap(0, 128, 0, [[64, 8], [1, 64]]), OP.add, [tmpA], [sm])
                P.ts(sm.ap(0, 128, 0, [[1, 8]]), sm.ap(0, 128, 0, [[1, 8]]), 1.0 / 64, OP.mult, [sm], [sm], s2=1e-6, op1=OP.add)
                P.act(sm.ap(0, 128, 0, [[1, 8]]), sm.ap(0, 128, 0, [[1, 8]]), AF.Ln, [sm], [sm])
                P.act(sm.ap(0, 128, 0, [[1, 8]]), sm.ap(0, 128, 0, [[1, 8]]), AF.Exp, [sm], [sm], scale=-0.5)
                P.tt(tmpB.ap(0, 128, 0, [[64, 8], [1, 64]]), po.ap(0, 128, 0, [[64, 8], [1, 64]]),
                     sm.ap(0, 128, 0, [[1, 8], [0, 64]]), OP.mult, [po, sm], [tmpB])
                P.tt(octm.t[:, :], tmpB.t[:, :], rgtm.t[:, n, :], OP.mult, [tmpB, rgtm], [octm])
                _, pto = psum()
                for h in range(8):
                    P.tr(pto.ap(0, 64, h * 128, [[1, 128]]), octm.ap(0, 128, h * 64, [[1, 64]]), cba(CB_IDENT, 128),
                         [octm, cb], [pto], signal=(h == 7))
                P.copy(oT.ap(0, 64, n * 128, [[TB, 8], [1, 128]]), pto.ap(0, 64, 0, [[128, 8], [1, 128]]), [pto], [oT])
                P.copy(st_Rb[l].ap(0, 64, 0, [[128, 4], [1, 64]]), st_R[l].ap(0, 64, 0, [[64, 4], [1, 64]]),
                       [st_R[l]], [st_Rb[l]], eng="act")
                P.copy(st_Rb[l].ap(64, 64, 64, [[128, 4], [1, 64]]), st_R[l].ap(64, 64, 0, [[64, 4], [1, 64]]),
                       [st_R[l]], [st_Rb[l]], eng="act")

        def rwkv(l):
            ZR, ZK, ZV, KK, KA, RR = 0, 2048, 4096, 6144, 8192, 10240

            def shifted(pt, M, j, mucol, out_ap, outT):
                P.copy(rwsb.ap(0, M, 1, [[1, TB]]), pt.ap(0, M, 0, [[1, TB]]), [pt], [rwsb], eng="act")
                P.copy(rwsb.ap(0, M, 0, [[1, 1]]), st_sh[l].ap(0, M, j, [[1, 1]]), [st_sh[l]], [rwsb])
                P.copy(st_sh[l].ap(0, M, j, [[1, 1]]), rwsb.ap(0, M, TB, [[1, 1]]), [rwsb], [st_sh[l]])
                P.tt(tmpA.ap(0, M, 0, [[1, TB]]), rwsb.ap(0, M, 0, [[1, TB]]), rwsb.ap(0, M, 1, [[1, TB]]), OP.subtract,
                     [rwsb], [tmpA])
                P.stt(out_ap, tmpA.ap(0, M, 0, [[1, TB]]), vcol(l, mucol, 0, M), rwsb.ap(0, M, 1, [[1, TB]]),
                      OP.mult, OP.add, [tmpA, vecs, rwsb], [outT])

            for ti, zoff in enumerate((ZR, ZK, ZV)):
                w = wpiece(l, 6 + ti)
                proj_fm(w, 512, lambda m, pt, ti=ti, zoff=zoff: shifted(
                    pt, 128, ti * 4 + m, 16 + ti * 4 + m, BIG.ap(0, 128, zoff + m * 512, [[1, TB]]), BIG))
            for m in range(4):
                zk = BIG.ap(0, 128, ZK + m * 512, [[1, TB]])
                zr = BIG.ap(0, 128, ZR + m * 512, [[1, TB]])
                P.ts(BIG.ap(0, 128, KK + m * 512, [[1, TB]]), zk, vcol(l, 31 + m), OP.mult, [BIG, vecs], [BIG])
                P.ts(BIG.ap(0, 128, KA + m * 512, [[1, TB]]), zk, vcol(l, 35 + m), OP.mult, [BIG, vecs], [BIG])
                P.ts(BIG.ap(0, 128, RR + m * 512, [[1, TB]]), zr, vcol(l, 39 + m), OP.mult, [BIG, vecs], [BIG])
            w = wpiece(l, 9)
            for ji, (c0, M, dst, fn) in enumerate(((0, 64, wdx, AF.Tanh), (64, 64, adx, AF.Copy), (128, 128, gdx, AF.Sigmoid))):
                pt, _ = psum()
                for k in range(KD):
                    P.mm(pt.ap(0, M, 0, [[1, TB]]), w.ap(0, 128, k * 256 + c0, [[1, M]]), hT.t[:, k, :],
                         k == 0, k == KD - 1, [w, hT], [pt])
                shifted(pt, M, 12 + ji, 28 + ji, tmpB.ap(0, M, 0, [[1, TB]]), tmpB)
                for dup in range(2):
                    P.act(dst.ap(0, M, dup * 64, [[128, 8], [1, 64]]), tmpB.ap(0, M, 0, [[64, 8], [1, 64]]), fn, [tmpB], [dst])

            for ci in range(TB // 64):
                t0 = ci * 64
                pa_, _ = psum()
                P.mm(pa_.t[:, :], adx.ap(0, 64, 2 * t0, [[1, 128]]), lwb.ap(0, 64, 512, [[1, 512]]), True, False,
                     [adx, lwb], [pa_], signal=False)
                P.mm(pa_.t[:, :], cba(CB_ONES, 128, 0, 33), HL.ap(0, 33, 512, [[1, 512]]), False, True, [cb, HL], [pa_])
                P.act(a_tm.t[:, :], pa_.t[:, :], AF.Sigmoid, [pa_], [a_tm])
                pw_, _ = psum()
                P.mm(pw_.t[:, :], wdx.ap(0, 64, 2 * t0, [[1, 128]]), lwb.ap(0, 64, 0, [[1, 512]]), True, False,
                     [wdx, lwb], [pw_], signal=False)
                P.mm(pw_.t[:, :], cba(CB_ONES, 128, 0, 33), HL.ap(0, 33, 0, [[1, 512]]), False, True, [cb, HL], [pw_])
                P.act(sig.t[:, :], pw_.t[:, :], AF.Sigmoid, [pw_], [sig])
                pg_, _ = psum()
                P.mm(pg_.t[:, :], gdx.ap(0, 128, 2 * t0, [[1, 128]]), lwb.ap(0, 128, 1024, [[1, 512]]), True, True,
                     [gdx, lwb], [pg_])
                P.copy(g_tm.t[:, :], pg_.t[:, :], [pg_], [g_tm], eng="act")
                pc1, _ = psum()
                P.mm(pc1.t[:, :], cf.ap(0, 64, CF_TRI, [[1, 128]]), sig.ap(0, 64, 0, [[1, 512]]), True, True, [cf, sig], [pc1])
                P.act(E3.t[:, :], pc1.t[:, :], AF.Exp, [pc1], [E3])
                pc3, _ = psum()
                P.mm(pc3.t[:, :], cf.ap(0, 64, CF_TRI + 128, [[1, 128]]), sig.ap(0, 64, 0, [[1, 512]]), True, True,
                     [cf, sig], [pc3])
                P.act(E2.t[:, :], pc3.t[:, :], AF.Exp, [pc3], [E2])
                pgc, _ = psum()
                for h in range(8):
                    P.mm(pgc.ap(0, 64, h * 2, [[1, 2]]), sig.ap(0, 64, h * 64, [[1, 64]]), cf.ap(0, 64, CF_ONES, [[1, 2]]),
                         True, True, [sig, cf], [pgc], signal=(h == 7))
                P.act(sm.ap(0, 64, 16, [[1, 8]]), pgc.ap(0, 64, 0, [[2, 8]]), AF.Exp, [pgc], [sm])
                P.act(sm.ap(0, 64, 24, [[1, 8]]), pgc.ap(0, 64, 0, [[2, 8]]), AF.Exp, [pgc], [sm], scale=-1.0)
                tps = {}
                shared = None
                for name, off in (("kk", KK), ("k", ZK), ("ka", KA), ("r", ZR), ("rr", RR), ("v", ZV)):
                    if name == "k":
                        ptb = shared
                    else:
                        _, ptb = psum()
                    if name == "kk":
                        shared = ptb
                    p0 = 0 if name == "kk" else 64
                    for m in range(4):
                        P.tr(ptb.ap(p0, 64, m * 128, [[1, 128]]), BIG.ap(0, 128, off + m * 512 + t0, [[1, 64]]),
                             cba(CB_IDENT, 128), [BIG, cb], [ptb], signal=(m == 3))
                    tps[name] = ptb
                kkp = tps["kk"]
                P.act(tmpA.ap(0, 64, 0, [[1, 512]]), kkp.ap(0, 64, 0, [[1, 512]]), AF.Square, [kkp], [tmpA])
                P.red(sm.ap(0, 64, 0, [[1, 8]]), tmpA.ap(0, 64, 0, [[64, 8], [1, 64]]), OP.add, [tmpA], [sm])
                P.act(sm.ap(0, 64, 8, [[1, 8]]), sm.ap(0, 64, 0, [[1, 8]]), AF.Sqrt, [sm], [sm])
                P.ts(sm.ap(0, 64, 8, [[1, 8]]), sm.ap(0, 64, 8, [[1, 8]]), 1e-12, OP.max, [sm], [sm])
                P.recip(sm.ap(0, 64, 8, [[1, 8]]), sm.ap(0, 64, 8, [[1, 8]]), [sm], [sm])
                P.stt(nk.ap(0, 64, 0, [[64, 8], [1, 64]]), kkp.ap(0, 64, 0, [[64, 8], [1, 64]]), -1.0,
                      sm.ap(0, 64, 8, [[1, 8], [0, 64]]), OP.mult, OP.mult, [kkp, sm], [nk])
                P.tt(X3.ap(0, 64, 0, [[1, 512]]), nk.ap(0, 64, 0, [[1, 512]]), E3.ap(0, 64, 0, [[1, 512]]), OP.mult,
                     [nk, E3], [X3])
                P.stt(Q1.ap(0, 64, 0, [[1, 512]]), nk.ap(0, 64, 0, [[1, 512]]), -1.0, a_tm.ap(0, 64, 0, [[1, 512]]),
                      OP.mult, OP.mult, [nk, a_tm], [Q1])
                P.stt(tmpB.ap(64, 64, 0, [[1, 512]]), a_tm.ap(64, 64, 0, [[1, 512]]), 1.0, tps["ka"].ap(64, 64, 0, [[1, 512]]),
                      OP.subtract, OP.mult, [a_tm, tps["ka"]], [tmpB])
                P.tt(Q1.ap(64, 64, 0, [[1, 512]]), tmpB.ap(64, 64, 0, [[1, 512]]), tps["k"].ap(64, 64, 0, [[1, 512]]), OP.add,
                     [tmpB, tps["k"]], [Q1])
                P.tt(X3.ap(64, 64, 0, [[1, 512]]), tps["r"].ap(64, 64, 0, [[1, 512]]), E3.ap(64, 64, 0, [[1, 512]]), OP.mult,
                     [tps["r"], E3], [X3])
                P.copy(UV.ap(64, 64, 0, [[1, 512]]), tps["v"].ap(64, 64, 0, [[1, 512]]), [tps["v"]], [UV], eng="act")
                P.tt(tmpB.ap(64, 64, 0, [[1, 512]]), tps["rr"].ap(64, 64, 0, [[1, 512]]), Q1.ap(64, 64, 0, [[1, 512]]), OP.mult,
                     [tps["rr"], Q1], [tmpB])
                P.red(sm.ap(64, 64, 32, [[1, 8]]), tmpB.ap(64, 64, 0, [[64, 8], [1, 64]]), OP.add, [tmpB], [sm])
                P.tt(X2.t[:, :], Q1.t[:, :], E2.t[:, :], OP.mult, [Q1, E2], [X2])
                _, pt3 = psum()
                for h in range(8):
                    P.tr(pt3.ap(0, 64, h * 128, [[1, 128]]), X3.ap(0, 128, h * 64, [[1, 64]]), cba(CB_IDENT, 128),
                         [X3, cb], [pt3], signal=(h == 7))
                P.copy(X3T.ap(0, 64, 0, [[1, 1024]]), pt3.ap(0, 64, 0, [[1, 1024]]), [pt3], [X3T], eng="act")
                _, pt2 = psum()
                for h in range(8):
                    P.tr(pt2.ap(0, 64, h * 128, [[1, 128]]), X2.ap(0, 128, h * 64, [[1, 64]]), cba(CB_IDENT, 128),
                         [X2, cb], [pt2], signal=(h == 7))
                P.tt(X1T.ap(0, 64, 0, [[128, 8], [1, 128]]), pt2.ap(0, 64, 0, [[128, 8], [1, 128]]),
                     sm.ap(0, 64, 24, [[1, 8], [0, 128]]), OP.mult, [pt2, sm], [X1T])
                pS = [psum()[0], psum()[0]]
                for h in range(8):
                    pt = pS[h // 4]
                    P.mm(pt.ap(0, 128, (h % 4) * 128, [[1, 128]]), X1T.ap(0, 64, h * 128, [[1, 128]]),
                         X3T.ap(0, 64, h * 128, [[1, 128]]), True, True, [X1T, X3T], [pt], signal=(h % 4 == 3))
                for half in range(2):
                    P.tt(S_sb.ap(0, 128, half * 512, [[128, 4], [1, 128]]), pS[half].ap(0, 128, 0, [[128, 4], [1, 128]]),
                         cb.ap(0, 128, CB_M4, [[0, 4], [1, 128]]), OP.mult, [pS[half], cb], [S_sb])
                pA, _ = psum()
                for h in range(8):
                    P.mm(pA.ap(0, 64, h * 64, [[1, 64]]), X3T.ap(0, 64, h * 128, [[1, 64]]), X1T.ap(0, 64, h * 128, [[1, 64]]),
                         True, True, [X3T, X1T], [pA], signal=(h == 7))
                P.tt(Apow[0].ap(0, 64, 0, [[64, 8], [1, 64]]), pA.ap(0, 64, 0, [[64, 8], [1, 64]]),
                     cb.ap(0, 64, CB_MST, [[0, 8], [1, 64]]), OP.mult, [pA, cb], [Apow[0]])
                pX, _ = psum()
                pX2, _ = psum()
                for h in range(8):
                    P.mm(pX.ap(0, 64, h * 64, [[1, 64]]), X3T.ap(0, 64, h * 128, [[1, 64]]),
                         st_Hb[l].ap(0, 64, h * 64, [[1, 64]]), True, True, [X3T, st_Hb[l]], [pX], signal=(h == 7))
                for h in range(8):
                    P.mm(pX2.ap(0, 64, h * 64, [[1, 64]]), S_sb.ap(64, 64, h * 128, [[1, 64]]),
                         UV.ap(64, 64, h * 64, [[1, 64]]), True, True, [S_sb, UV], [pX2], signal=(h == 7))
                P.copy(Xf.ap(0, 64, 0, [[1, 512]]), pX.ap(0, 64, 0, [[1, 512]]), [pX], [Xf], eng="act")
                P.tt(Xf.ap(0, 64, 0, [[1, 512]]), Xf.ap(0, 64, 0, [[1, 512]]), pX2.ap(0, 64, 0, [[1, 512]]), OP.add,
                     [Xf, pX2], [Xf])
                P.copy(Xb.ap(0, 64, 0, [[1, 512]]), Xf.ap(0, 64, 0, [[1, 512]]), [Xf], [Xb], eng="act")
                for i in range(6):
                    if i == 0:
                        def Nap(h, c0=0, w=64):
                            return S_sb.ap(0, 64, h * 128 + c0, [[1, w]])
                        Nt = S_sb
                    else:
                        def Nap(h, c0=0, w=64, i=i):
                            return Npow[i % 2].ap(0, 64, h * 64 + c0, [[1, w]])
                        Nt = Npow[i % 2]
                    At = Apow[i % 2]
                    pY, _ = psum()
                    for h in range(8):
                        P.mm(pY.ap(0, 64, h * 64, [[1, 64]]), Nap(h), Xb.ap(0, 64, h * 64, [[1, 64]]), True, True,
                             [Nt, Xb], [pY], signal=(h == 7))
                    P.tt(Xf.ap(0, 64, 0, [[1, 512]]), Xf.ap(0, 64, 0, [[1, 512]]), pY.ap(0, 64, 0, [[1, 512]]), OP.add,
                         [Xf, pY], [Xf])
                    if i < 5:
                        P.copy(Xb.ap(0, 64, 0, [[1, 512]]), Xf.ap(0, 64, 0, [[1, 512]]), [Xf], [Xb], eng="act")
                        pN, _ = psum()
                        pA2, _ = psum()
                        for h in range(8):
                            P.mm(pN.ap(0, 64, h * 64, [[1, 64]]), At.ap(0, 64, h * 64, [[1, 64]]), Nap(h), True, True,
                                 [At, Nt], [pN], signal=(h == 7))
                        for h in range(8):
                            P.mm(pA2.ap(0, 64, h * 64, [[1, 64]]), Nap(h), At.ap(0, 64, h * 64, [[1, 64]]), True, True,
                                 [At, Nt], [pA2], signal=(h == 7))
                        P.copy(Npow[(i + 1) % 2].ap(0, 64, 0, [[1, 512]]), pN.ap(0, 64, 0, [[1, 512]]), [pN],
                               [Npow[(i + 1) % 2]], eng="act")
                        P.copy(Apow[(i + 1) % 2].ap(0, 64, 0, [[1, 512]]), pA2.ap(0, 64, 0, [[1, 512]]), [pA2],
                               [Apow[(i + 1) % 2]])
                    else:
                        P.copy(UV.ap(0, 64, 0, [[1, 512]]), Xf.ap(0, 64, 0, [[1, 512]]), [Xf], [UV], eng="act")
                pYo, _ = psum()
                for h in range(8):
                    o_ap = pYo.ap(64, 64, h * 64, [[1, 64]])
                    P.mm(o_ap, X3T.ap(0, 64, h * 128 + 64, [[1, 64]]), st_Hb[l].ap(0, 64, h * 64, [[1, 64]]), True, False,
                         [X3T, st_Hb[l]], [pYo], signal=False)
                    P.mm(o_ap, S_sb.ap(0, 128, h * 128 + 64, [[1, 64]]), UV.ap(0, 128, h * 64, [[1, 64]]), False, True,
                         [S_sb, UV], [pYo], signal=(h == 7))
                pH, _ = psum()
                for h in range(8):
                    P.mm(pH.ap(0, 64, h * 64, [[1, 64]]), X2.ap(0, 128, h * 64, [[1, 64]]), UV.ap(0, 128, h * 64, [[1, 64]]),
                         True, True, [X2, UV], [pH], signal=(h == 7))
                P.tt(st_H[l].ap(0, 64, 0, [[64, 8], [1, 64]]), st_H[l].ap(0, 64, 0, [[64, 8], [1, 64]]),
                     sm.ap(0, 64, 16, [[1, 8], [0, 64]]), OP.mult, [st_H[l], sm], [st_H[l]])
                P.tt(st_H[l].ap(0, 64, 0, [[1, 512]]), st_H[l].ap(0, 64, 0, [[1, 512]]), pH.ap(0, 64, 0, [[1, 512]]), OP.add,
                     [st_H[l], pH], [st_H[l]])
                P.copy(st_Hb[l].ap(0, 64, 0, [[1, 512]]), st_H[l].ap(0, 64, 0, [[1, 512]]), [st_H[l]], [st_Hb[l]], eng="act")
                R64 = (64, 64)
                P.copy(Ysb.ap(64, 64, 0, [[1, 512]]), pYo.ap(64, 64, 0, [[1, 512]]), [pYo], [Ysb], eng="act")
                P.red(sm.ap(64, 64, 40, [[1, 8]]), Ysb.ap(64, 64, 0, [[64, 8], [1, 64]]), OP.add, [Ysb], [sm])
                P.act(tmpA.ap(64, 64, 0, [[1, 512]]), Ysb.ap(64, 64, 0, [[1, 512]]), AF.Square, [Ysb], [tmpA])
                P.red(sm.ap(64, 64, 48, [[1, 8]]), tmpA.ap(64, 64, 0, [[64, 8], [1, 64]]), OP.add, [tmpA], [sm])
                P.ts(sm.ap(64, 64, 40, [[1, 8]]), sm.ap(64, 64, 40, [[1, 8]]), 1.0 / 64, OP.mult, [sm], [sm])
                P.tt(sm.ap(64, 64, 56, [[1, 8]]), sm.ap(64, 64, 40, [[1, 8]]), sm.ap(64, 64, 40, [[1, 8]]), OP.mult, [sm], [sm])
                P.stt(sm.ap(64, 64, 48, [[1, 8]]), sm.ap(64, 64, 48, [[1, 8]]), 1.0 / 64, sm.ap(64, 64, 56, [[1, 8]]),
                      OP.mult, OP.subtract, [sm], [sm])
                P.ts(sm.ap(64, 64, 48, [[1, 8]]), sm.ap(64, 64, 48, [[1, 8]]), 64e-5, OP.add, [sm], [sm])
                P.act(sm.ap(64, 64, 48, [[1, 8]]), sm.ap(64, 64, 48, [[1, 8]]), AF.Sqrt, [sm], [sm])
                P.recip(sm.ap(64, 64, 48, [[1, 8]]), sm.ap(64, 64, 48, [[1, 8]]), [sm], [sm])
                Y3 = Ysb.ap(64, 64, 0, [[64, 8], [1, 64]])
                P.tt(Y3, Y3, sm.ap(64, 64, 40, [[1, 8], [0, 64]]), OP.subtract, [Ysb, sm], [Ysb])
                P.tt(Y3, Y3, sm.ap(64, 64, 48, [[1, 8], [0, 64]]), OP.mult, [Ysb, sm], [Ysb])
                Y2 = Ysb.ap(64, 64, 0, [[1, 512]])
                P.tt(Y2, Y2, rowf.ap(64, 64, 0, [[1, 512]]), OP.mult, [Ysb, rowf], [Ysb])
                P.tt(Y2, Y2, rowf.ap(64, 64, 512, [[1, 512]]), OP.add, [Ysb, rowf], [Ysb])
                P.tt(tmpA.ap(64, 64, 0, [[64, 8], [1, 64]]), UV.ap(64, 64, 0, [[64, 8], [1, 64]]),
                     sm.ap(64, 64, 32, [[1, 8], [0, 64]]), OP.mult, [UV, sm], [tmpA])
                P.tt(Y2, Y2, tmpA.ap(64, 64, 0, [[1, 512]]), OP.add, [Ysb, tmpA], [Ysb])
                P.tt(obt.ap(64, 64, 0, [[1, 512]]), Y2, g_tm.ap(64, 64, 0, [[1, 512]]), OP.mult, [Ysb, g_tm], [obt])
                _, pto = psum()
                for h in range(8):
                    P.tr(pto.ap(0, 64, h * 64, [[1, 64]]), obt.ap(64, 64, h * 64, [[1, 64]]),
                         cb.ap(64, 64, CB_IDENT + 64, [[1, 64]]), [obt, cb], [pto], signal=(h == 7))
                P.copy(oT.ap(0, 64, t0, [[TB, 8], [1, 64]]), pto.ap(0, 64, 0, [[64, 8], [1, 64]]), [pto], [oT])

        for s in range(NSEQ):
            for tb in range(NTB):
                c0 = s * T + tb * TB
                first = (tb == 0)
                tbi[0] = tb
                for hh in range(2):
                    P.dma("pool", rotc.ap(0, 128, hh * TB, [[1, TB]]),
                          bass.AP(rot_d.tensor, hh * T + tb * TB, [[2 * T, 128], [1, TB]]), rotc)
                P.dma("sp", xT.t[:, :, :], bass.AP(xT_d.tensor, c0, [[NT, 128], [128 * NT, KD], [1, TB]]), xT)
                for l in range(L):
                    if first:
                        for stt_ in (st_R[l], st_Rb[l], st_H[l], st_Hb[l], st_sh[l]):
                            P.memset(stt_.ap(0, stt_.shape[0], 0, [[1, stt_.rs]]), 0.0, [stt_])
                    P.dma("sp", rowf.t[:, :], bass.AP(rows_d.tensor, l * 2048 + 1024, [[0, 128], [1, 1024]]), rowf)
                    P.dma("pool", lwb.t[:, :], lw_d[:, l * 1536:(l + 1) * 1536], lwb)
                    P.dma("sp", rv33.t[0:33, :], bass.AP(rows_d.tensor, l * 2048, [[0, 33], [1, 1024]]), rv33)
                    P.copy(rhi.t[0:33, :], rv33.t[0:33, :], [rv33], [rhi])
                    P.tt(rv33.t[0:33, :], rv33.t[0:33, :], rhi.t[0:33, :], OP.subtract, [rv33, rhi], [rv33])
                    P.copy(HL.t[0:1, :], rhi.t[0:1, :], [rhi], [HL])
                    P.copy(HL.t[32:33, :], rv33.t[32:33, :], [rv33], [HL])
                    rmsnorm(l, 0)
                    import os as _os
                    SK = _os.environ.get("KSKIP", "")
                    if "a" not in SK:
                        attention(l, first)
                        merge_branch(l, 0)
                    else:
                        P.memset(mixed.ap(0, 128, 0, [[1, KD * TB]]), 0.0, [mixed])
                        if "m" in SK:
                            P.memset(oT.ap(0, 64, 0, [[1, 8 * TB]]), 0.0, [oT])
                            merge_branch(l, 0)
                    if "c" not in SK:
                        retention(l)
                        merge_branch(l, 2)
                    if "b" not in SK:
                        rwkv(l)
                        merge_branch(l, 1)
                    for hf in range(2):
                        w = wpiece(l, 22 + hf)

                        def resid(m, pt, hf=hf):
                            mi = hf * 4 + m
                            P.tt(xT.t[:, mi, :], xT.t[:, mi, :], pt.t[:, :], OP.add, [xT, pt], [xT])
                        proj_fm(w, 512, resid, src=mixed)
                    rmsnorm(l, 8)
                    actT = BIG
                    for i in range(6):
                        ncol = 512 if i < 5 else 256
                        wg = wpiece(l, 24 + 2 * i)
                        wu = wpiece(l, 25 + 2 * i)
                        for m in range(ncol // 128):
                            pg, _ = psum()
                            pu, _ = psum()
                            for k in range(KD):
                                P.mm(pg.t[:, :], wg.ap(0, 128, k * ncol + m * 128, [[1, 128]]), hT.t[:, k, :],
                                     k == 0, k == KD - 1, [wg, hT], [pg])
                            for k in range(KD):
                                P.mm(pu.t[:, :], wu.ap(0, 128, k * ncol + m * 128, [[1, 128]]), hT.t[:, k, :],
                                     k == 0, k == KD - 1, [wu, hT], [pu])
                            P.act(tmpA.t[:, :], pg.t[:, :], AF.Silu, [pg], [tmpA])
                            fi = i * 4 + m
                            P.tt(actT.ap(0, 128, fi * 512, [[1, 512]]), pu.t[:, :], tmpA.t[:, :], OP.mult, [pu, tmpA], [actT])
                    for m in range(8):
                        w = wpiece(l, 36 + m)
                        pt, _ = psum()
                        for k in range(KF):
                            P.mm(pt.t[:, :], w.ap(0, 128, k * 128, [[1, 128]]), actT.ap(0, 128, k * 512, [[1, 512]]),
                                 k == 0, k == KF - 1, [w, actT], [pt])
                        P.tt(xT.t[:, m, :], xT.t[:, m, :], pt.t[:, :], OP.add, [xT, pt], [xT])
                P.dma("sp", bass.AP(yT_d.tensor, c0, [[NT, 128], [128 * NT, KD], [1, TB]]), xT.t[:, :, :], xT, load=False)
        with nc.Block() as block:
            P.finish(block, [xT, oT])
        P.stats = {e: len(P.ins[e]) for e in P.ENGS}
        nc._prog_stats = (P.stats, P.nwaits, P.ndsem)
        nc._tags = P.tags
        nc._P = P
    return nc


def prep_inputs(inp, x_cores, T, L):
    consts = make_consts(T)
    wpk = np.stack([pack_layer(inp, l) for l in range(L)])
    vecs = pack_vecs(inp, L).reshape(128, L * NVEC)
    lw, rows, sinks = pack_small(inp, L)
    shared = {"wpk": wpk, "vecs": np.ascontiguousarray(vecs), "lw": np.ascontiguousarray(lw.reshape(128, L * 1536)),
              "rows": np.ascontiguousarray(rows.reshape(L, 2048)), "sinks": np.ascontiguousarray(sinks.reshape(1, L * 8)),
              "cf": consts["cf"], "cb": consts["cb"], "rot": consts["rot"]}
    maps = []
    for xc in x_cores:
        xT = np.ascontiguousarray(xc.reshape(-1, D).T)
        m = dict(shared)
        m["xT"] = xT
        maps.append(m)
    return maps


def kernel(**inputs):
    inp = {k: np.asarray(v, dtype=np.float32) for k, v in inputs.items()}
    x = inp["x"]
    B, T, _ = x.shape
    L = inp["w_in"].shape[0]
    ncores = 8
    nseq = B // ncores
    nc = build(nseq, T, L)
    maps = prep_inputs(inp, [x[c * nseq:(c + 1) * nseq] for c in range(ncores)], T, L)
    res = run_bass_kernel_spmd(nc, maps, core_ids=list(range(ncores)))
    out = np.empty((B, T, D), np.float32)
    for c in range(ncores):
        yT = np.asarray(res.results[c]["yT"])
        out[c * nseq:(c + 1) * nseq] = yT.T.reshape(nseq, T, D)
    return out
```

```python
from contextlib import ExitStack
import numpy as np
import concourse.bass as bass
import concourse.mybir as mybir
from concourse.bass_utils import run_bass_kernel_spmd

F32 = mybir.dt.float32
BF16 = mybir.dt.bfloat16
AF = mybir.ActivationFunctionType
OP = mybir.AluOpType
AX = mybir.AxisListType

D = 1024
KD = 8
TB = 512
FF = 2816
KF = 22
NPIECE = 44
PW = 4096
NVEC = 48
LOG_DECAY_C = -float(np.exp(-0.5))


class Buf:
    def __init__(self, name):
        self.name = name
        self.w = None
        self.r = {}
        self.dsem = None
        self.dcnt = 0


class Tl:
    def __init__(self, t, shape, buf=None, name=""):
        self.t = t
        self.shape = list(shape)
        self.rs = int(np.prod(shape[1:]))
        self.buf = buf or Buf(name)

    def ap(self, p0, npart, off, dims):
        return bass.AP(self.t, p0 * self.rs + off, [[self.rs, npart]] + [list(d) for d in dims])

    def __getitem__(self, idx):
        return self.t[idx]


class Prog:
    ENGS = ["pe", "dve", "act", "pool", "sp"]

    def __init__(self, nc, es):
        self.nc = nc
        self.es = es
        self.sem = {e: es.enter_context(nc.semaphore("s_" + e)) for e in self.ENGS}
        self.cnt = {e: 0 for e in self.ENGS}
        self.ins = {e: [] for e in self.ENGS}
        self.seen = {e: {} for e in self.ENGS}
        self.semobj = dict(self.sem)
        self.ndsem = 0
        self.nwaits = 0
        self.tags = {}

    def _waits(self, eng, deps):
        need = {}
        for d in deps:
            if d is None:
                continue
            k, v = d
            if k == eng and eng == "pe":
                continue
            if v > need.get(k, 0):
                need[k] = v
        out = []
        for k, v in need.items():
            if self.seen[eng].get(k, 0) >= v:
                continue
            self.seen[eng][k] = v
            out.append((self.semobj[k], v))
        self.nwaits += len(out)
        return out

    def op(self, eng, fn, reads=(), writes=(), signal=True):
        deps = []
        for b in reads:
            deps.append(b.buf.w)
        for b in writes:
            deps.append(b.buf.w)
            deps.extend(b.buf.r.items())
        waits = self._waits(eng, deps)
        val = self.cnt[eng] + 1
        if signal:
            self.cnt[eng] = val
        import sys as _sys
        fr = _sys._getframe(1)
        while fr.f_code.co_name in ("op", "mm", "tr", "act", "tt", "ts", "stt", "copy", "red", "memset", "recip", "<lambda>"):
            fr = fr.f_back
        tag = "%s:%d" % (fr.f_code.co_name, fr.f_lineno)
        self.ins[eng].append((waits, fn, (self.sem[eng], 1) if signal else None, tag))
        for b in reads:
            if b.buf.r.get(eng, 0) < val:
                b.buf.r[eng] = val
        for b in writes:
            b.buf.w = (eng, val)
            b.buf.r = {}

    def _dsem(self, buf):
        if buf.dsem is None:
            buf.dsem = "d%d" % self.ndsem
            self.ndsem += 1
            self.semobj[buf.dsem] = self.es.enter_context(self.nc.semaphore(buf.dsem))
        return buf.dsem

    def dma(self, q, out, in_, tile, load=True):
        b = tile.buf
        k = self._dsem(b)
        deps = [b.w]
        if load:
            deps.extend(b.r.items())
        waits = self._waits(q, deps)
        b.dcnt += 16
        self.ins[q].append((waits, lambda e: e.dma_start(out=out, in_=in_), (self.semobj[k], 16), "dma"))
        if load:
            b.w = (k, b.dcnt)
            b.r = {}
        else:
            b.r[k] = b.dcnt

    def inherit(self, dsts, srcs):
        acc = {}
        for s_ in srcs:
            items = list(s_.buf.r.items())
            if s_.buf.w is not None:
                items.append(s_.buf.w)
            for k, v in items:
                if v > acc.get(k, 0):
                    acc[k] = v
        for d_ in dsts:
            d_.buf.w = None
            d_.buf.r = dict(acc)

    def finish(self, block, final_bufs):
        waits = []
        for b in final_bufs:
            if b.buf.dsem is not None:
                waits.append((self.semobj[b.buf.dsem], b.buf.dcnt))
        self.ins["sp"].append((waits, None, None, "end"))
        engmap = {"pe": block.tensor, "dve": block.vector, "act": block.scalar,
                  "pool": block.gpsimd, "sp": block.sync}
        for e in self.ENGS:
            lst = self.ins[e]

            def body(eng, lst=lst):
                for waits, fn, inc, tag in lst:
                    for s, v in waits:
                        eng.wait_ge(s, v)
                    if fn is None:
                        continue
                    i = fn(eng)
                    try:
                        self.tags[str(i.ins.name)] = tag
                    except Exception:
                        pass
                    if inc is not None:
                        i.then_inc(inc[0], inc[1])
            engmap[e](body)

    def mm(self, out, lhsT, rhs, start, stop, reads, writes, signal=None):
        if signal is None:
            signal = stop
        self.op("pe", lambda e: e.matmul(out, lhsT=lhsT, rhs=rhs, start=start, stop=stop),
                reads, writes, signal)

    def tr(self, out, in_, ident, reads, writes, signal=True):
        self.op("pe", lambda e: e.transpose(out, in_, ident), reads, writes, signal)

    def act(self, out, in_, func, reads, writes, scale=1.0, bias=None):
        if bias is None:
            self.op("act", lambda e: e.activation(out=out, in_=in_, func=func, scale=scale), reads, writes)
        else:
            self.op("act", lambda e: e.activation(out=out, in_=in_, func=func, scale=scale, bias=bias),
                    reads, writes)

    def tt(self, out, in0, in1, op, reads, writes, eng="dve"):
        self.op(eng, lambda e: e.tensor_tensor(out=out, in0=in0, in1=in1, op=op), reads, writes)

    def ts(self, out, in0, s1, op0, reads, writes, s2=None, op1=None, eng="dve"):
        if op1 is None:
            self.op(eng, lambda e: e.tensor_scalar(out=out, in0=in0, scalar1=s1, scalar2=None, op0=op0),
                    reads, writes)
        else:
            self.op(eng, lambda e: e.tensor_scalar(out=out, in0=in0, scalar1=s1, scalar2=s2, op0=op0, op1=op1),
                    reads, writes)

    def stt(self, out, in0, scalar, in1, op0, op1, reads, writes):
        self.op("dve", lambda e: e.scalar_tensor_tensor(out=out, in0=in0, scalar=scalar, in1=in1,
                                                        op0=op0, op1=op1), reads, writes)

    def copy(self, out, in_, reads, writes, eng="dve"):
        if eng == "act":
            self.op("act", lambda e: e.copy(out=out, in_=in_), reads, writes)
        else:
            self.op(eng, lambda e: e.tensor_copy(out=out, in_=in_), reads, writes)

    def red(self, out, in_, op, reads, writes):
        self.op("dve", lambda e: e.tensor_reduce(out=out, in_=in_, op=op, axis=AX.X), reads, writes)

    def memset(self, ap, val, writes, eng="dve"):
        self.op(eng, lambda e: e.memset(ap, val), [], writes)

    def recip(self, out, in_, reads, writes):
        self.op("dve", lambda e: e.reciprocal(out=out, in_=in_), reads, writes)


def _bf(a):
    import ml_dtypes
    return np.asarray(a, dtype=np.float32).astype(ml_dtypes.bfloat16)


def make_consts(T):
    c = {}
    idx = np.arange(128)
    ident = np.eye(128, dtype=np.float32)
    s = np.arange(64)[:, None]
    t = np.arange(64)[None, :]
    tri1 = np.concatenate([(s < t), (s <= t)], axis=1).astype(np.float32)
    tri3 = np.concatenate([(s > t), (s > t)], axis=1).astype(np.float32)
    tri = np.zeros((128, 256), np.float32)
    tri[:64, :128] = tri1 * LOG_DECAY_C
    tri[:64, 128:] = tri3 * LOG_DECAY_C
    half = 32
    inv_freq = 1.0 / (10000.0 ** (np.arange(half, dtype=np.float32) * 2.0 / 64))
    pos = np.arange(T, dtype=np.float32)
    ang = pos[None, :] * inv_freq[:, None]
    cosT = np.cos(ang)[idx % 32]
    sinT = np.sin(ang)[idx % 32] * np.where((idx % 64) < 32, -1.0, 1.0)[:, None]
    H = 8
    log_gamma = np.log1p(-np.power(2.0, -5.0 - np.arange(H, dtype=np.float64)))
    i = np.arange(128, dtype=np.float64)
    xi = np.exp(log_gamma[:, None] * (i[None, :] + 1.0))
    kf = (64 ** -0.5) * np.exp(-log_gamma[:, None] * (i[None, :] + 1.0))
    gc = np.exp(log_gamma * 128.0)
    XI = np.zeros((128, 4, 128), np.float32)
    KFt = np.zeros((128, 4, 128), np.float32)
    GC = np.zeros((128, 4), np.float32)
    for h in range(H):
        rows = slice((h % 2) * 64, (h % 2) * 64 + 64)
        XI[rows, h // 2, :] = xi[h][None, :]
        KFt[rows, h // 2, :] = kf[h][None, :]
        GC[rows, h // 2] = gc[h]
    ones = np.full((128, 64), LOG_DECAY_C, np.float32)
    c["cf"] = np.concatenate([ident, tri, XI.reshape(128, -1), KFt.reshape(128, -1), GC, ones], axis=1)
    c["rot"] = np.concatenate([cosT, sinT], axis=1).astype(np.float32)
    identb = np.eye(128, dtype=np.float32)
    blk64 = (idx[:, None] // 64 == idx[None, :] // 64).astype(np.float32) / 64.0
    onesD = np.full((128, 128), 1.0 / 1024.0, np.float32)
    mdiag = (idx[:, None] <= idx[None, :]).astype(np.float32)
    mprev = (idx[:, None] > idx[None, :]).astype(np.float32)
    perm = np.zeros((128, 128), np.float32)
    for p in range(128):
        q = p + 32 if (p % 64) < 32 else p - 32
        perm[q, p] = 1.0
    m4 = np.zeros((128, 128), np.float32)
    ss = np.arange(64)[:, None]
    tt = np.arange(64)[None, :]
    for w in range(2):
        m4[w * 64:(w + 1) * 64, 0:64] = (ss < tt)
        m4[w * 64:(w + 1) * 64, 64:128] = (ss <= tt)
    mst = np.zeros((128, 64), np.float32)
    mst[:64] = (np.arange(64)[:, None] > np.arange(64)[None, :])
    ones128 = np.ones((128, 128), np.float32)
    c["cb"] = _bf(np.concatenate([identb, blk64, onesD, mdiag, mprev, perm, m4, mst, ones128], axis=1))
    return c


CF_IDENT, CF_TRI, CF_XI, CF_KF, CF_GC, CF_ONES = 0, 128, 384, 896, 1408, 1412
CF_W = 1412 + 64
CB_IDENT, CB_BLK, CB_OND, CB_MD, CB_MP, CB_PERM, CB_M4, CB_MST, CB_ONES = 0, 128, 256, 384, 512, 640, 768, 896, 960
CB_W = 960 + 128


def _fm(W):
    K, N = W.shape
    kc = K // 128
    out = np.zeros((128, PW), np.float32)
    out[:, :kc * N] = W.reshape(kc, 128, N).transpose(1, 0, 2).reshape(128, kc * N)
    return out


def _hm(W, c0):
    out = np.zeros((128, PW), np.float32)
    out[:64] = W[:, c0:c0 + 512].reshape(8, 64, 512).transpose(1, 0, 2).reshape(64, 4096)
    return out


def pack_layer(inp, l):
    w_in = inp["w_in"][l]
    P = []
    P.append(_fm(w_in[:, 0:512]))
    akv = np.concatenate([w_in[:, 512:576], w_in[:, 512:576], w_in[:, 576:640], w_in[:, 576:640],
                          w_in[:, 640:768]], axis=1)
    P.append(_fm(akv))
    P.append(_fm(w_in[:, 2560:3072]))
    P.append(_fm(w_in[:, 3072:3584]))
    P.append(_fm(w_in[:, 3584:4096]))
    P.append(_fm(w_in[:, 4096:4608]))
    P.append(_fm(w_in[:, 768:1280]))
    P.append(_fm(w_in[:, 1280:1792]))
    P.append(_fm(w_in[:, 1792:2304]))
    P.append(_fm(w_in[:, 2304:2560]))
    for b, wo in enumerate([inp["w_attn_o"][l], inp["w_rwkv_o"][l], inp["w_ret_o"][l]]):
        for hf in range(2):
            P.append(_hm(wo, hf * 512))
            c0 = 4608 + b * 1024 + hf * 512
            P.append(_fm(w_in[:, c0:c0 + 512]))
    for hf in range(2):
        P.append(_fm(inp["w_out"][l][:, hf * 512:(hf + 1) * 512]))
    for i in range(6):
        c0 = i * 512
        c1 = min(c0 + 512, FF)
        P.append(_fm(inp["w_ffn_gate"][l][:, c0:c1]))
        P.append(_fm(inp["w_ffn_up"][l][:, c0:c1]))
    wd = inp["w_ffn_down"][l]
    for m in range(8):
        P.append(_fm(wd[:, m * 128:(m + 1) * 128]))
    assert len(P) == NPIECE
    return np.stack(P)


def pack_vecs(inp, L):
    v = np.zeros((128, L, NVEC), np.float32)
    idx = np.arange(128)

    def fm(a):
        return a.reshape(-1, 128).T

    for l in range(L):
        v[:, l, 0:8] = fm(inp["norm1_g"][l])
        v[:, l, 8:16] = fm(inp["norm2_g"][l])
        mu = inp["rwkv_shift_mu"][l]
        v[:, l, 16:28] = fm(mu[0:1536])
        v[:64, l, 28] = mu[1536:1600]
        v[:64, l, 29] = mu[1600:1664]
        v[:, l, 30] = mu[1664:1792]
        v[:, l, 31:35] = fm(inp["rwkv_k_k"][l])
        v[:, l, 35:39] = fm(inp["rwkv_k_a"][l])
        v[:, l, 39:43] = fm(inp["rwkv_r_k"][l].reshape(-1))
        v[:, l, 43] = inp["attn_q_norm_g"][l][idx % 64]
        v[:, l, 44] = inp["attn_k_norm_g"][l][idx % 64]
    return v


def pack_small(inp, L):
    lw = np.zeros((128, L, 3, 512), np.float32)
    rows = np.zeros((L, 4, 512), np.float32)
    for l in range(L):
        lw[:64, l, 0] = inp["rwkv_w2"][l]
        lw[:64, l, 1] = inp["rwkv_a2"][l]
        lw[:, l, 2] = inp["rwkv_g2"][l]
        rows[l, 0] = inp["rwkv_w0"][l]
        rows[l, 1] = inp["rwkv_a0"][l]
        rows[l, 2] = inp["rwkv_lnx_g"][l]
        rows[l, 3] = inp["rwkv_lnx_b"][l]
    sinks = np.asarray(inp["attn_sinks"], np.float32)[:L].reshape(L, 8)
    return lw, rows, sinks


def build(NSEQ, T, L, debug=False):
    nc = bass.Bass("TRN2", target_bir_lowering=False)
    NTB = T // TB
    NT = NSEQ * T
    xT_d = nc.dram_tensor("xT", [D, NT], F32, kind="ExternalInput").ap()
    wpk_d = nc.dram_tensor("wpk", [L, NPIECE, 128, PW], F32, kind="ExternalInput").ap()
    vec_d = nc.dram_tensor("vecs", [128, L * NVEC], F32, kind="ExternalInput").ap()
    lw_d = nc.dram_tensor("lw", [128, L * 1536], F32, kind="ExternalInput").ap()
    rows_d = nc.dram_tensor("rows", [L, 2048], F32, kind="ExternalInput").ap()
    sink_d = nc.dram_tensor("sinks", [1, L * 8], F32, kind="ExternalInput").ap()
    cf_d = nc.dram_tensor("cf", [128, CF_W], F32, kind="ExternalInput").ap()
    cb_d = nc.dram_tensor("cb", [128, CB_W], BF16, kind="ExternalInput").ap()
    rot_d = nc.dram_tensor("rot", [128, 2 * T], F32, kind="ExternalInput").ap()
    yT_d = nc.dram_tensor("yT", [D, NT], F32, kind="ExternalOutput").ap()
    dbg_d = None
    if debug:
        dbg_d = nc.dram_tensor("dbg", [3, 64, 8 * TB], BF16, kind="ExternalOutput").ap()

    es = ExitStack()
    with es:
        P = Prog(nc, es)

        def sb(name, shape, dt=F32):
            return Tl(es.enter_context(nc.sbuf_tensor("s_" + name, list(shape), dt)), shape, name=name)

        def view(base, name, shape, dt, col0_bytes):
            raise NotImplementedError

        rvtm = sb("rvtm", [128, 4, 512], BF16)
        cf = sb("cf", [128, CF_W])
        cb = sb("cb", [128, CB_W], BF16)
        rotc = sb("rotb", [128, 2 * TB], BF16)
        vecs = sb("vecs", [128, L * NVEC])
        lwb = sb("lwb", [128, 1536], BF16)
        rowf = sb("rowf", [128, 1024])
        rv33 = sb("rv33", [64, 1024])
        HL = sb("HL", [64, 1024], BF16)
        sinkx = sb("sinkx", [128, L * 8])
        eps6 = sb("eps6", [128, 1])
        P.dma("sp", cf.t[:, :], cf_d, cf)
        P.dma("sp", cb.t[:, :], cb_d, cb)
        P.dma("sp", vecs.t[:, :], vec_d, vecs)
        P.dma("sp", sinkx.t[:, :], bass.AP(sink_d.tensor, 0, [[0, 128], [1, L * 8]]), sinkx)
        P.act(sinkx.t[:, :], sinkx.t[:, :], AF.Exp, [sinkx], [sinkx])
        P.memset(eps6.t[:, :], 1e-6, [eps6])
        P.memset(HL.t[:, :], 0.0, [HL])

        def cba(col, w, p0=0, npart=128):
            return cb.ap(p0, npart, col, [[1, w]])

        def vcol(l, j, p0=0, npart=128):
            return vecs.ap(p0, npart, l * NVEC + j, [[1, 1]])

        xT = sb("xT", [128, KD, TB])
        hT = sb("hT", [128, KD, TB], BF16)
        NW = 3
        wring = [sb("w%d" % i, [128, PW], BF16) for i in range(NW)]
        ps = [Tl(es.enter_context(nc.psum_tensor("ps%d" % i, [128, 512], F32)), [128, 512], name="ps%d" % i)
              for i in range(8)]
        psb = [Tl(p.t.bitcast(BF16), [128, 1024], buf=p.buf) for p in ps]
        pctr = [0]

        def psum():
            i = pctr[0] % 8
            pctr[0] += 1
            return ps[i], psb[i]

        oT = sb("oT", [64, 8, TB], BF16)
        B1 = sb("B1", [128, 4096], BF16)
        BIG = sb("BIG", [128, 3 * 4096], BF16)
        mixed = sb("mixed", [128, KD, TB], BF16)
        tmpA = sb("tmpA", [128, TB])
        tmpB = sb("tmpB", [128, TB])
        tmpD = sb("tmpD", [128, TB], BF16)
        rgtm = sb("rgtm", [128, 4, 512], BF16)
        vtm = sb("vtm", [128, 4, 128], BF16)
        PT = [sb("PT0", [128, 1024], BF16), None]
        ktm = sb("ktm", [128, 4, 512], BF16)
        sT = sb("sT", [128, 1024], BF16)
        PT[1] = sT
        rhi = sT
        rwsb = sb("rwsb", [128, TB + 1])
        wdx = sb("wdx", [64, 2 * TB], BF16)
        adx = sb("adx", [64, 2 * TB], BF16)
        gdx = sb("gdx", [128, 2 * TB], BF16)
        a_tm = sb("a_tm", [128, 512])
        sig = sb("sig", [128, 512])
        g_tm = sb("g_tm", [128, 512])
        E3 = sb("E3", [128, 512])
        E2 = sb("E2", [128, 512])
        Q1 = sb("Q1", [128, 512])
        nk = sb("nk", [128, 512])
        Ysb = nk
        rstd = tmpB
        X3 = sb("X3", [128, 512], BF16)
        X2 = sb("X2", [128, 512], BF16)
        UV = sb("UV", [128, 512], BF16)
        obt = sb("obt", [128, 512], BF16)
        octm = obt
        X3T = sb("X3T", [64, 8, 128], BF16)
        X1T = sb("X1T", [64, 8, 128], BF16)
        S_sb = sb("S_sb", [128, 8, 128], BF16)
        Apow = [sb("Apow%d" % i, [64, 8, 64], BF16) for i in range(2)]
        Npow = [sb("Npow%d" % i, [64, 8, 64], BF16) for i in range(2)]
        Xf = sb("Xf", [64, 8, 64])
        Xb = sb("Xb", [64, 8, 64], BF16)
        sm = sb("sm", [128, 64])
        st_k = [sb("stk%d" % l, [128, 2, 128], BF16) for l in range(L)]
        st_v = [sb("stv%d" % l, [128, 128], BF16) for l in range(L)]
        st_R = [sb("stR%d" % l, [128, 4, 64]) for l in range(L)]
        st_Rb = [sb("stRb%d" % l, [128, 8, 64], BF16) for l in range(L)]
        st_H = [sb("stH%d" % l, [64, 8, 64]) for l in range(L)]
        st_Hb = [sb("stHb%d" % l, [64, 8, 64], BF16) for l in range(L)]
        st_sh = [sb("stsh%d" % l, [128, 16]) for l in range(L)]

        wq = {"i": 0}
        tbi = [0]

        def wpiece(l, j):
            tl = wring[wq["i"] % NW]
            wq["i"] += 1
            P.dma("pool", tl.ap(0, 128, 0, [[2048, 2], [1, 2048]]),
                  bass.AP(wpk_d.tensor, (l * NPIECE + j) * 128 * PW, [[PW, 128], [2048, 2], [1, 2048]]), tl)
            return tl

        def rmsnorm(l, gcol):
            sq = BIG
            P.act(sq.ap(0, 128, 0, [[1, KD * TB]]), xT.ap(0, 128, 0, [[1, KD * TB]]), AF.Square, [xT], [sq])
            pt, _ = psum()
            for k in range(KD):
                P.mm(pt.t[:, :], cba(CB_OND, 128), sq.ap(0, 128, k * TB, [[1, TB]]), k == 0, k == KD - 1, [cb, sq], [pt])
            P.act(rstd.t[:, :], pt.t[:, :], AF.Ln, [pt, eps6], [rstd], bias=eps6.t[:, 0:1])
            P.act(rstd.t[:, :], rstd.t[:, :], AF.Exp, [rstd], [rstd], scale=-0.5)
            for k in range(KD):
                P.stt(hT.t[:, k, :], xT.t[:, k, :], vcol(l, gcol + k), rstd.t[:, :], OP.mult, OP.mult,
                      [xT, vecs, rstd], [hT])

        def proj_fm(w, ncol, cb_fn, src=None, M=128):
            src = src or hT
            for m in range(ncol // M):
                pt, ptb = psum()
                for k in range(KD):
                    P.mm(pt.ap(0, M, 0, [[1, TB]]), w.ap(0, 128, k * ncol + m * M, [[1, M]]),
                         src.t[:, k, :], k == 0, k == KD - 1, [w, src], [pt])
                cb_fn(m, pt)

        def headnorm(pt, dst_ap, gcolap, dstT):
            P.act(tmpD.t[:, :], pt.t[:, :], AF.Square, [pt], [tmpD])
            p2, _ = psum()
            P.mm(p2.t[:, :], cba(CB_BLK, 128), tmpD.t[:, :], True, True, [cb, tmpD], [p2])
            P.act(tmpA.t[:, :], p2.t[:, :], AF.Ln, [p2, eps6], [tmpA], bias=eps6.t[:, 0:1])
            P.act(tmpA.t[:, :], tmpA.t[:, :], AF.Exp, [tmpA], [tmpA], scale=-0.5)
            P.stt(dst_ap, pt.t[:, :], gcolap, tmpA.t[:, :], OP.mult, OP.mult, [pt, vecs, tmpA], [dstT])

        def merge_branch(l, b):
            for hf in range(2):
                wo = wpiece(l, 10 + b * 4 + hf * 2)
                wg = wpiece(l, 11 + b * 4 + hf * 2)
                for m in range(4):
                    pg, _ = psum()
                    for k in range(KD):
                        P.mm(pg.t[:, :], wg.ap(0, 128, k * 512 + m * 128, [[1, 128]]), hT.t[:, k, :],
                             k == 0, k == KD - 1, [wg, hT], [pg])
                    po, _ = psum()
                    for h in range(8):
                        P.mm(po.t[:, :], wo.ap(0, 64, h * 512 + m * 128, [[1, 128]]), oT.t[:, h, :],
                             h == 0, h == 7, [wo, oT], [po])
                    P.act(tmpA.t[:, :], pg.t[:, :], AF.Sigmoid, [pg], [tmpA])
                    mi = hf * 4 + m
                    if b == 0:
                        P.tt(mixed.t[:, mi, :], po.t[:, :], tmpA.t[:, :], OP.mult, [po, tmpA], [mixed])
                    else:
                        P.tt(tmpB.t[:, :], po.t[:, :], tmpA.t[:, :], OP.mult, [po, tmpA], [tmpB])
                        P.tt(mixed.t[:, mi, :], mixed.t[:, mi, :], tmpB.t[:, :], OP.add, [mixed, tmpB], [mixed])
            if debug:
                P.dma("sp", dbg_d[b], oT.ap(0, 64, 0, [[1, 8 * TB]]), oT, load=False)

        class _Stop(Exception):
            pass

        def stage(i):
            import os as _os
            if int(_os.environ.get("KSTOP", "99")) < i:
                raise _Stop()

        def attention(l, first):
            try:
                attention_(l, first)
            except _Stop:
                pass

        def attention_(l, first):
            import os as _os
            if "KSTOP" in _os.environ:
                P.memset(oT.ap(0, 64, 0, [[1, 8 * TB]]), 0.0, [oT])
            w = wpiece(l, 0)
            proj_fm(w, 512, lambda m, pt: headnorm(pt, B1.ap(0, 128, m * 512, [[1, 512]]), vcol(l, 43), B1))
            stage(2)
            w = wpiece(l, 1)
            for m in range(2):
                pt, _ = psum()
                for k in range(KD):
                    P.mm(pt.t[:, :], w.ap(0, 128, k * 384 + m * 128, [[1, 128]]), hT.t[:, k, :],
                         k == 0, k == KD - 1, [w, hT], [pt])
                headnorm(pt, B1.ap(0, 128, 2048 + m * 512, [[1, 512]]), vcol(l, 44), B1)
            stage(3)
            for n in range(4):
                pt, _ = psum()
                for k in range(KD):
                    P.mm(pt.ap(0, 128, 0, [[1, 128]]), hT.t[:, k, n * 128:(n + 1) * 128],
                         w.ap(0, 128, k * 384 + 256, [[1, 128]]), k == 0, k == KD - 1, [w, hT], [pt])
                P.copy(vtm.t[:, n, :], pt.t[:, 0:128], [pt], [vtm], eng="act")
            stage(4)
            for n in range(4):
                blocks = []
                if not (first and n == 0):
                    blocks.append(0)
                blocks.append(1)
                pts = {}
                for jb in blocks:
                    pa, _ = psum()
                    pb, _ = psum()
                    for h in range(8):
                        g = h // 4
                        base = (h % 2) * 64
                        if jb == 1:
                            kap = B1.ap(base, 64, 2048 + g * 512 + n * 128, [[1, 128]])
                            kr = [B1]
                        elif n == 0:
                            kap = st_k[l].ap(base, 64, g * 128, [[1, 128]])
                            kr = [st_k[l]]
                        else:
                            kap = B1.ap(base, 64, 2048 + g * 512 + (n - 1) * 128, [[1, 128]])
                            kr = [B1]
                        qap = B1.ap(base, 64, (h // 2) * 512 + n * 128, [[1, 128]])
                        pt = pa if h % 2 == 0 else pb
                        P.mm(pt.ap(0, 128, (h // 2) * 128, [[1, 128]]), kap, qap, True, True,
                             kr + [B1], [pt], signal=(h // 2 == 3))
                    pts[jb] = (pa, pb)
                stage(5)
                for jb in blocks:
                    for par, pt in enumerate(pts[jb]):
                        P.act(PT[jb].ap(0, 128, par * 128, [[256, 4], [1, 128]]), pt.ap(0, 128, 0, [[128, 4], [1, 128]]),
                              AF.Exp, [pt], [PT[jb]], scale=0.125)
                    stage(6)
                    mcol = CB_MP if jb == 0 else CB_MD
                    P.tt(PT[jb].ap(0, 128, 0, [[128, 8], [1, 128]]), PT[jb].ap(0, 128, 0, [[128, 8], [1, 128]]),
                         cb.ap(0, 128, mcol, [[0, 8], [1, 128]]), OP.mult, [PT[jb], cb], [PT[jb]])
                stage(7)
                for g in range(2):
                    po, _ = psum()
                    pd, _ = psum()
                    for bi, jb in enumerate(blocks):
                        if jb == 1:
                            vap = vtm.ap(0, 128, n * 128 + g * 64, [[1, 64]])
                            vr = [vtm]
                        elif n == 0:
                            vap = st_v[l].ap(0, 128, g * 64, [[1, 64]])
                            vr = [st_v[l]]
                        else:
                            vap = vtm.ap(0, 128, (n - 1) * 128 + g * 64, [[1, 64]])
                            vr = [vtm]
                        rhs = PT[jb].ap(0, 128, g * 512, [[1, 512]])
                        P.mm(po.ap(0, 64, 0, [[1, 512]]), vap, rhs, bi == 0, bi == len(blocks) - 1, vr + [PT[jb]], [po])
                        P.mm(pd.ap(0, 64, 0, [[1, 512]]), cba(CB_ONES, 64), rhs, bi == 0, bi == len(blocks) - 1,
                             [cb, PT[jb]], [pd])
                    stage(8)
                    P.tt(tmpA.ap(0, 64, 0, [[128, 4], [1, 128]]), pd.ap(0, 64, 0, [[128, 4], [1, 128]]),
                         sinkx.ap(0, 64, l * 8 + g * 4, [[1, 4], [0, 128]]), OP.add, [pd, sinkx], [tmpA])
                    P.recip(tmpA.ap(0, 64, 0, [[1, 512]]), tmpA.ap(0, 64, 0, [[1, 512]]), [tmpA], [tmpA])
                    P.tt(oT.ap(0, 64, g * 4 * TB + n * 128, [[TB, 4], [1, 128]]),
                         po.ap(0, 64, 0, [[128, 4], [1, 128]]), tmpA.ap(0, 64, 0, [[128, 4], [1, 128]]),
                         OP.mult, [po, tmpA], [oT])
            for g in range(2):
                P.copy(st_k[l].t[:, g, :], B1.ap(0, 128, 2048 + g * 512 + 384, [[1, 128]]), [B1], [st_k[l]], eng="act")
            P.copy(st_v[l].t[:, :], vtm.t[:, 3, :], [vtm], [st_v[l]], eng="act")

        def retention(l):
            try:
                retention_(l)
            except _Stop:
                pass

        def retention_(l):
            import os as _os
            if "KSTOP" in _os.environ:
                P.memset(oT.ap(0, 64, 0, [[1, 8 * TB]]), 0.0, [oT])

            def rotary(pt, dst_ap, fac_col, m):
                P.copy(tmpD.t[:, :], pt.t[:, :], [pt], [tmpD], eng="act")
                p2, _ = psum()
                P.mm(p2.t[:, :], cba(CB_PERM, 128), tmpD.t[:, :], True, True, [cb, tmpD], [p2])
                import os as _os
                KR = _os.environ.get("KROT", "")
                if KR == "1":
                    P.copy(tmpA.t[:, :], p2.t[:, :], [p2], [tmpA])
                    P.copy(dst_ap, tmpA.ap(0, 128, 0, [[128, 4], [1, 128]]), [tmpA], [B1])
                    return
                if KR == "3":
                    P.tt(tmpA.t[:, :], pt.t[:, :], cf.ap(0, 128, 0, [[1, 512]]), OP.mult, [pt, cf], [tmpA])
                    P.tt(tmpB.t[:, :], p2.t[:, :], cf.ap(0, 128, 512, [[1, 512]]), OP.mult, [p2, cf], [tmpB])
                else:
                    P.copy(E3.t[:, :], pt.t[:, :], [pt], [E3], eng="act")
                    P.tt(tmpA.t[:, :], E3.t[:, :], rotc.ap(0, 128, 0, [[1, TB]]), OP.mult, [E3, rotc], [tmpA])
                    P.tt(tmpB.t[:, :], p2.t[:, :], rotc.ap(0, 128, TB, [[1, TB]]), OP.mult, [p2, rotc], [tmpB])
                P.tt(tmpA.t[:, :], tmpA.t[:, :], tmpB.t[:, :], OP.add, [tmpA, tmpB], [tmpA])
                if KR == "2":
                    P.copy(dst_ap, tmpA.ap(0, 128, 0, [[128, 4], [1, 128]]), [tmpA], [B1])
                    return
                P.tt(dst_ap, tmpA.ap(0, 128, 0, [[128, 4], [1, 128]]),
                     cf.ap(0, 128, fac_col + m * 128, [[0, 4], [1, 128]]), OP.mult, [tmpA, cf], [B1])

            w = wpiece(l, 2)
            proj_fm(w, 512, lambda m, pt: rotary(pt, B1.ap(0, 128, m * 512, [[128, 4], [1, 128]]), CF_XI, m))
            stage(10)
            w = wpiece(l, 3)
            proj_fm(w, 512, lambda m, pt: rotary(pt, B1.ap(0, 128, 2048 + m * 512, [[128, 4], [1, 128]]), CF_KF, m))
            stage(11)
            for n in range(4):
                _, ptb = psum()
                for m in range(4):
                    P.tr(ptb.ap(0, 128, m * 128, [[1, 128]]), B1.ap(0, 128, 2048 + m * 512 + n * 128, [[1, 128]]),
                         cba(CB_IDENT, 128), [B1, cb], [ptb], signal=(m == 3))
                P.copy(ktm.t[:, n, :], ptb.t[:, 0:512], [ptb], [ktm], eng="act")
            stage(12)
            w = wpiece(l, 4)
            for n in range(4):
                pt, _ = psum()
                for k in range(KD):
                    P.mm(pt.t[:, :], hT.t[:, k, n * 128:(n + 1) * 128], w.ap(0, 128, k * 512, [[1, 512]]),
                         k == 0, k == KD - 1, [w, hT], [pt])
                P.copy(rvtm.t[:, n, :], pt.t[:, :], [pt], [rvtm], eng="act")
            w = wpiece(l, 5)
            for n in range(4):
                pt, _ = psum()
                for k in range(KD):
                    P.mm(pt.t[:, :], hT.t[:, k, n * 128:(n + 1) * 128], w.ap(0, 128, k * 512, [[1, 512]]),
                         k == 0, k == KD - 1, [w, hT], [pt])
                P.act(rgtm.t[:, n, :], pt.t[:, :], AF.Silu, [pt], [rgtm])
            stage(13)
            for n in range(4):
                pa, _ = psum()
                pb, _ = psum()
                for h in range(8):
                    base = (h % 2) * 64
                    kap = B1.ap(base, 64, 2048 + (h // 2) * 512 + n * 128, [[1, 128]])
                    qap = B1.ap(base, 64, (h // 2) * 512 + n * 128, [[1, 128]])
                    pt = pa if h % 2 == 0 else pb
                    P.mm(pt.ap(0, 128, (h // 2) * 128, [[1, 128]]), kap, qap, True, True, [B1], [pt], signal=(h // 2 == 3))
                for par, pt in enumerate((pa, pb)):
                    P.act(sT.ap(0, 128, par * 128, [[256, 4], [1, 128]]), pt.ap(0, 128, 0, [[128, 4], [1, 128]]),
                          AF.Copy, [pt], [sT])
                P.tt(sT.ap(0, 128, 0, [[128, 8], [1, 128]]), sT.ap(0, 128, 0, [[128, 8], [1, 128]]),
                     cb.ap(0, 128, CB_MD, [[0, 8], [1, 128]]), OP.mult, [sT, cb], [sT])
                stage(14)
                po, _ = psum()
                for h in range(8):
                    o_ap = po.ap(0, 128, h * 64, [[1, 64]])
                    P.mm(o_ap, sT.ap(0, 128, h * 128, [[1, 128]]), rvtm.ap(0, 128, n * 512 + h * 64, [[1, 64]]),
                         True, False, [rvtm, sT], [po], signal=False)
                    P.mm(o_ap, B1.ap(0, 128, (h // 2) * 512 + n * 128, [[1, 128]]), st_Rb[l].ap(0, 128, h * 64, [[1, 64]]),
                         False, True, [st_Rb[l], B1], [po], signal=(h == 7))
                stage(15)
                pk0, _ = psum()
                pk1, _ = psum()
                for h in range(8):
                    base = (h % 2) * 64
                    pk = pk0 if h % 2 == 0 else pk1
                    P.mm(pk.ap(base, 64, (h // 2) * 64, [[1, 64]]), ktm.ap(0, 128, n * 512 + h * 64, [[1, 64]]),
                         rvtm.ap(0, 128, n * 512 + h * 64, [[1, 64]]), True, True, [ktm, rvtm], [pk], signal=(h >= 6))
                P.tt(st_R[l].ap(0, 64, 0, [[64, 4], [1, 64]]), st_R[l].ap(0, 64, 0, [[64, 4], [1, 64]]),
                     pk0.ap(0, 64, 0, [[64, 4], [1, 64]]), OP.add, [st_R[l], pk0], [st_R[l]])
                P.tt(st_R[l].ap(64, 64, 0, [[64, 4], [1, 64]]), st_R[l].ap(64, 64, 0, [[64, 4], [1, 64]]),
                     pk1.ap(64, 64, 0, [[64, 4], [1, 64]]), OP.add, [st_R[l], pk1], [st_R[l]])
                P.tt(st_R[l].t[:, :, :], st_R[l].t[:, :, :], cf.ap(0, 128, CF_GC, [[1, 4], [0, 64]]), OP.mult,
                     [st_R[l], cf], [st_R[l]])
                stage(16)
                P.act(tmpA.t[:, :], po.t[:, :], AF.Square, [po], [tmpA])
                P.red(sm.ap(0, 128, 0, [[1, 8]]), tmpA.ap(0, 128, 0, [[64, 8], [1, 64]]), OP.add, [tmpA], [sm])
                P.ts(sm.ap(0, 128, 0, [[1, 8]]), sm.ap(0, 128, 0, [[1, 8]]), 1.0 / 64, OP.mult, [sm], [sm], s2=1e-6, op1=OP.add)
                P.act(sm.ap(0, 128, 0, [[1, 8]]), sm.ap(0, 128, 0, [[1, 8]]), AF.Ln, [sm], [sm])
                P.act(sm.ap(0, 128, 0, [[1, 8]]), sm.ap(0, 128, 0, [[1, 8]]), AF.Exp, [sm], [sm], scale=-0.5)
                P.tt(tmpB.ap(0, 128, 0, [[64, 8], [1, 64]]), po.ap(0, 128, 0, [[64, 8], [1, 64]]),
                     sm.ap(0, 128, 0, [[1, 8], [0, 64]]), OP.mult, [po, sm], [tmpB])
                P.tt(octm.t[:, :], tmpB.t[:, :], rgtm.t[:, n, :], OP.mult, [tmpB, rgtm], [octm])
                _, pto = psum()
                for h in range(8):
                    P.tr(pto.ap(0, 64, h * 128, [[1, 128]]), octm.ap(0, 128, h * 64, [[1, 64]]), cba(CB_IDENT, 128),
                         [octm, cb], [pto], signal=(h == 7))
                P.copy(oT.ap(0, 64, n * 128, [[TB, 8], [1, 128]]), pto.ap(0, 64, 0, [[128, 8], [1, 128]]), [pto], [oT])
                P.copy(st_Rb[l].ap(0, 64, 0, [[128, 4], [1, 64]]), st_R[l].ap(0, 64, 0, [[64, 4], [1, 64]]),
                       [st_R[l]], [st_Rb[l]], eng="act")
                P.copy(st_Rb[l].ap(64, 64, 64, [[128, 4], [1, 64]]), st_R[l].ap(64, 64, 0, [[64, 4], [1, 64]]),
                       [st_R[l]], [st_Rb[l]], eng="act")

        def rwkv(l):
            ZR, ZK, ZV, KK, KA, RR = 0, 2048, 4096, 6144, 8192, 10240

            def shifted(pt, M, j, mucol, out_ap, outT):
                P.copy(rwsb.ap(0, M, 1, [[1, TB]]), pt.ap(0, M, 0, [[1, TB]]), [pt], [rwsb], eng="act")
                P.copy(rwsb.ap(0, M, 0, [[1, 1]]), st_sh[l].ap(0, M, j, [[1, 1]]), [st_sh[l]], [rwsb])
                P.copy(st_sh[l].ap(0, M, j, [[1, 1]]), rwsb.ap(0, M, TB, [[1, 1]]), [rwsb], [st_sh[l]])
                P.tt(tmpA.ap(0, M, 0, [[1, TB]]), rwsb.ap(0, M, 0, [[1, TB]]), rwsb.ap(0, M, 1, [[1, TB]]), OP.subtract,
                     [rwsb], [tmpA])
                P.stt(out_ap, tmpA.ap(0, M, 0, [[1, TB]]), vcol(l, mucol, 0, M), rwsb.ap(0, M, 1, [[1, TB]]),
                      OP.mult, OP.add, [tmpA, vecs, rwsb], [outT])

            for ti, zoff in enumerate((ZR, ZK, ZV)):
                w = wpiece(l, 6 + ti)
                proj_fm(w, 512, lambda m, pt, ti=ti, zoff=zoff: shifted(
                    pt, 128, ti * 4 + m, 16 + ti * 4 + m, BIG.ap(0, 128, zoff + m * 512, [[1, TB]]), BIG))
            for m in range(4):
                zk = BIG.ap(0, 128, ZK + m * 512, [[1, TB]])
                zr = BIG.ap(0, 128, ZR + m * 512, [[1, TB]])
                P.ts(BIG.ap(0, 128, KK + m * 512, [[1, TB]]), zk, vcol(l, 31 + m), OP.mult, [BIG, vecs], [BIG])
                P.ts(BIG.ap(0, 128, KA + m * 512, [[1, TB]]), zk, vcol(l, 35 + m), OP.mult, [BIG, vecs], [BIG])
                P.ts(BIG.ap(0, 128, RR + m * 512, [[1, TB]]), zr, vcol(l, 39 + m), OP.mult, [BIG, vecs], [BIG])
            w = wpiece(l, 9)
            for ji, (c0, M, dst, fn) in enumerate(((0, 64, wdx, AF.Tanh), (64, 64, adx, AF.Copy), (128, 128, gdx, AF.Sigmoid))):
                pt, _ = psum()
                for k in range(KD):
                    P.mm(pt.ap(0, M, 0, [[1, TB]]), w.ap(0, 128, k * 256 + c0, [[1, M]]), hT.t[:, k, :],
                         k == 0, k == KD - 1, [w, hT], [pt])
                shifted(pt, M, 12 + ji, 28 + ji, tmpB.ap(0, M, 0, [[1, TB]]), tmpB)
                for dup in range(2):
                    P.act(dst.ap(0, M, dup * 64, [[128, 8], [1, 64]]), tmpB.ap(0, M, 0, [[64, 8], [1, 64]]), fn, [tmpB], [dst])

            for ci in range(TB // 64):
                t0 = ci * 64
                pa_, _ = psum()
                P.mm(pa_.t[:, :], adx.ap(0, 64, 2 * t0, [[1, 128]]), lwb.ap(0, 64, 512, [[1, 512]]), True, False,
                     [adx, lwb], [pa_], signal=False)
                P.mm(pa_.t[:, :], cba(CB_ONES, 128, 0, 33), HL.ap(0, 33, 512, [[1, 512]]), False, True, [cb, HL], [pa_])
                P.act(a_tm.t[:, :], pa_.t[:, :], AF.Sigmoid, [pa_], [a_tm])
                pw_, _ = psum()
                P.mm(pw_.t[:, :], wdx.ap(0, 64, 2 * t0, [[1, 128]]), lwb.ap(0, 64, 0, [[1, 512]]), True, False,
                     [wdx, lwb], [pw_], signal=False)
                P.mm(pw_.t[:, :], cba(CB_ONES, 128, 0, 33), HL.ap(0, 33, 0, [[1, 512]]), False, True, [cb, HL], [pw_])
                P.act(sig.t[:, :], pw_.t[:, :], AF.Sigmoid, [pw_], [sig])
                pg_, _ = psum()
                P.mm(pg_.t[:, :], gdx.ap(0, 128, 2 * t0, [[1, 128]]), lwb.ap(0, 128, 1024, [[1, 512]]), True, True,
                     [gdx, lwb], [pg_])
                P.copy(g_tm.t[:, :], pg_.t[:, :], [pg_], [g_tm], eng="act")
                pc1, _ = psum()
                P.mm(pc1.t[:, :], cf.ap(0, 64, CF_TRI, [[1, 128]]), sig.ap(0, 64, 0, [[1, 512]]), True, True, [cf, sig], [pc1])
                P.act(E3.t[:, :], pc1.t[:, :], AF.Exp, [pc1], [E3])
                pc3, _ = psum()
                P.mm(pc3.t[:, :], cf.ap(0, 64, CF_TRI + 128, [[1, 128]]), sig.ap(0, 64, 0, [[1, 512]]), True, True,
                     [cf, sig], [pc3])
                P.act(E2.t[:, :], pc3.t[:, :], AF.Exp, [pc3], [E2])
                pgc, _ = psum()
                for h in range(8):
                    P.mm(pgc.ap(0, 64, h * 2, [[1, 2]]), sig.ap(0, 64, h * 64, [[1, 64]]), cf.ap(0, 64, CF_ONES, [[1, 2]]),
                         True, True, [sig, cf], [pgc], signal=(h == 7))
                P.act(sm.ap(0, 64, 16, [[1, 8]]), pgc.ap(0, 64, 0, [[2, 8]]), AF.Exp, [pgc], [sm])
                P.act(sm.ap(0, 64, 24, [[1, 8]]), pgc.ap(0, 64, 0, [[2, 8]]), AF.Exp, [pgc], [sm], scale=-1.0)
                tps = {}
                shared = None
                for name, off in (("kk", KK), ("k", ZK), ("ka", KA), ("r", ZR), ("rr", RR), ("v", ZV)):
                    if name == "k":
                        ptb = shared
                    else:
                        _, ptb = psum()
                    if name == "kk":
                        shared = ptb
                    p0 = 0 if name == "kk" else 64
                    for m in range(4):
                        P.tr(ptb.ap(p0, 64, m * 128, [[1, 128]]), BIG.ap(0, 128, off + m * 512 + t0, [[1, 64]]),
                             cba(CB_IDENT, 128), [BIG, cb], [ptb], signal=(m == 3))
                    tps[name] = ptb
                kkp = tps["kk"]
                P.act(tmpA.ap(0, 64, 0, [[1, 512]]), kkp.ap(0, 64, 0, [[1, 512]]), AF.Square, [kkp], [tmpA])
                P.red(sm.ap(0, 64, 0, [[1, 8]]), tmpA.ap(0, 64, 0, [[64, 8], [1, 64]]), OP.add, [tmpA], [sm])
                P.act(sm.ap(0, 64, 8, [[1, 8]]), sm.ap(0, 64, 0, [[1, 8]]), AF.Sqrt, [sm], [sm])
                P.ts(sm.ap(0, 64, 8, [[1, 8]]), sm.ap(0, 64, 8, [[1, 8]]), 1e-12, OP.max, [sm], [sm])
                P.recip(sm.ap(0, 64, 8, [[1, 8]]), sm.ap(0, 64, 8, [[1, 8]]), [sm], [sm])
                P.stt(nk.ap(0, 64, 0, [[64, 8], [1, 64]]), kkp.ap(0, 64, 0, [[64, 8], [1, 64]]), -1.0,
                      sm.ap(0, 64, 8, [[1, 8], [0, 64]]), OP.mult, OP.mult, [kkp, sm], [nk])
                P.tt(X3.ap(0, 64, 0, [[1, 512]]), nk.ap(0, 64, 0, [[1, 512]]), E3.ap(0, 64, 0, [[1, 512]]), OP.mult,
                     [nk, E3], [X3])
                P.stt(Q1.ap(0, 64, 0, [[1, 512]]), nk.ap(0, 64, 0, [[1, 512]]), -1.0, a_tm.ap(0, 64, 0, [[1, 512]]),
                      OP.mult, OP.mult, [nk, a_tm], [Q1])
                P.stt(tmpB.ap(64, 64, 0, [[1, 512]]), a_tm.ap(64, 64, 0, [[1, 512]]), 1.0, tps["ka"].ap(64, 64, 0, [[1, 512]]),
                      OP.subtract, OP.mult, [a_tm, tps["ka"]], [tmpB])
                P.tt(Q1.ap(64, 64, 0, [[1, 512]]), tmpB.ap(64, 64, 0, [[1, 512]]), tps["k"].ap(64, 64, 0, [[1, 512]]), OP.add,
                     [tmpB, tps["k"]], [Q1])
                P.tt(X3.ap(64, 64, 0, [[1, 512]]), tps["r"].ap(64, 64, 0, [[1, 512]]), E3.ap(64, 64, 0, [[1, 512]]), OP.mult,
                     [tps["r"], E3], [X3])
                P.copy(UV.ap(64, 64, 0, [[1, 512]]), tps["v"].ap(64, 64, 0, [[1, 512]]), [tps["v"]], [UV], eng="act")
                P.tt(tmpB.ap(64, 64, 0, [[1, 512]]), tps["rr"].ap(64, 64, 0, [[1, 512]]), Q1.ap(64, 64, 0, [[1, 512]]), OP.mult,
                     [tps["rr"], Q1], [tmpB])
                P.red(sm.ap(64, 64, 32, [[1, 8]]), tmpB.ap(64, 64, 0, [[64, 8], [1, 64]]), OP.add, [tmpB], [sm])
                P.tt(X2.t[:, :], Q1.t[:, :], E2.t[:, :], OP.mult, [Q1, E2], [X2])
                _, pt3 = psum()
                for h in range(8):
                    P.tr(pt3.ap(0, 64, h * 128, [[1, 128]]), X3.ap(0, 128, h * 64, [[1, 64]]), cba(CB_IDENT, 128),
                         [X3, cb], [pt3], signal=(h == 7))
                P.copy(X3T.ap(0, 64, 0, [[1, 1024]]), pt3.ap(0, 64, 0, [[1, 1024]]), [pt3], [X3T], eng="act")
                _, pt2 = psum()
                for h in range(8):
                    P.tr(pt2.ap(0, 64, h * 128, [[1, 128]]), X2.ap(0, 128, h * 64, [[1, 64]]), cba(CB_IDENT, 128),
                         [X2, cb], [pt2], signal=(h == 7))
                P.tt(X1T.ap(0, 64, 0, [[128, 8], [1, 128]]), pt2.ap(0, 64, 0, [[128, 8], [1, 128]]),
                     sm.ap(0, 64, 24, [[1, 8], [0, 128]]), OP.mult, [pt2, sm], [X1T])
                pS = [psum()[0], psum()[0]]
                for h in range(8):
                    pt = pS[h // 4]
                    P.mm(pt.ap(0, 128, (h % 4) * 128, [[1, 128]]), X1T.ap(0, 64, h * 128, [[1, 128]]),
                         X3T.ap(0, 64, h * 128, [[1, 128]]), True, True, [X1T, X3T], [pt], signal=(h % 4 == 3))
                for half in range(2):
                    P.tt(S_sb.ap(0, 128, half * 512, [[128, 4], [1, 128]]), pS[half].ap(0, 128, 0, [[128, 4], [1, 128]]),
                         cb.ap(0, 128, CB_M4, [[0, 4], [1, 128]]), OP.mult, [pS[half], cb], [S_sb])
                pA, _ = psum()
                for h in range(8):
                    P.mm(pA.ap(0, 64, h * 64, [[1, 64]]), X3T.ap(0, 64, h * 128, [[1, 64]]), X1T.ap(0, 64, h * 128, [[1, 64]]),
                         True, True, [X3T, X1T], [pA], signal=(h == 7))
                P.tt(Apow[0].ap(0, 64, 0, [[64, 8], [1, 64]]), pA.ap(0, 64, 0, [[64, 8], [1, 64]]),
                     cb.ap(0, 64, CB_MST, [[0, 8], [1, 64]]), OP.mult, [pA, cb], [Apow[0]])
                pX, _ = psum()
                pX2, _ = psum()
                for h in range(8):
                    P.mm(pX.ap(0, 64, h * 64, [[1, 64]]), X3T.ap(0, 64, h * 128, [[1, 64]]),
                         st_Hb[l].ap(0, 64, h * 64, [[1, 64]]), True, True, [X3T, st_Hb[l]], [pX], signal=(h == 7))
                for h in range(8):
                    P.mm(pX2.ap(0, 64, h * 64, [[1, 64]]), S_sb.ap(64, 64, h * 128, [[1, 64]]),
                         UV.ap(64, 64, h * 64, [[1, 64]]), True, True, [S_sb, UV], [pX2], signal=(h == 7))
                P.copy(Xf.ap(0, 64, 0, [[1, 512]]), pX.ap(0, 64, 0, [[1, 512]]), [pX], [Xf], eng="act")
                P.tt(Xb.ap(0, 64, 0, [[1, 512]]), Xf.ap(0, 64, 0, [[1, 512]]), pX2.ap(0, 64, 0, [[1, 512]]), OP.add,
                     [Xf, pX2], [Xb])
                P.tt(Xf.ap(0, 64, 0, [[1, 512]]), Xf.ap(0, 64, 0, [[1, 512]]), pX2.ap(0, 64, 0, [[1, 512]]), OP.add,
                     [Xf, pX2], [Xf])
                for i in range(6):
                    if i == 0:
                        def Nap(h, c0=0, w=64):
                            return S_sb.ap(0, 64, h * 128 + c0, [[1, w]])
                        Nt = S_sb
                    else:
                        def Nap(h, c0=0, w=64, i=i):
                            return Npow[i % 2].ap(0, 64, h * 64 + c0, [[1, w]])
                        Nt = Npow[i % 2]
                    At = Apow[i % 2]
                    pY, _ = psum()
                    for h in range(8):
                        P.mm(pY.ap(0, 64, h * 64, [[1, 64]]), Nap(h), Xb.ap(0, 64, h * 64, [[1, 64]]), True, True,
                             [Nt, Xb], [pY], signal=(h == 7))
                    if i < 5:
                        P.tt(Xb.ap(0, 64, 0, [[1, 512]]), Xf.ap(0, 64, 0, [[1, 512]]), pY.ap(0, 64, 0, [[1, 512]]), OP.add,
                             [Xf, pY], [Xb])
                        P.tt(Xf.ap(0, 64, 0, [[1, 512]]), Xf.ap(0, 64, 0, [[1, 512]]), pY.ap(0, 64, 0, [[1, 512]]), OP.add,
                             [Xf, pY], [Xf])
                        pN, _ = psum()
                        pA2, _ = psum()
                        for h in range(8):
                            P.mm(pN.ap(0, 64, h * 64, [[1, 64]]), At.ap(0, 64, h * 64, [[1, 64]]), Nap(h), True, True,
                                 [At, Nt], [pN], signal=(h == 7))
                        for h in range(8):
                            P.mm(pA2.ap(0, 64, h * 64, [[1, 64]]), Nap(h), At.ap(0, 64, h * 64, [[1, 64]]), True, True,
                                 [At, Nt], [pA2], signal=(h == 7))
                        P.copy(Npow[(i + 1) % 2].ap(0, 64, 0, [[1, 512]]), pN.ap(0, 64, 0, [[1, 512]]), [pN],
                               [Npow[(i + 1) % 2]], eng="act")
                        P.copy(Apow[(i + 1) % 2].ap(0, 64, 0, [[1, 512]]), pA2.ap(0, 64, 0, [[1, 512]]), [pA2],
                               [Apow[(i + 1) % 2]])
                    else:
                        P.tt(UV.ap(0, 64, 0, [[1, 512]]), Xf.ap(0, 64, 0, [[1, 512]]), pY.ap(0, 64, 0, [[1, 512]]), OP.add,
                             [Xf, pY], [UV])
                pYo, _ = psum()
                for h in range(8):
                    o_ap = pYo.ap(64, 64, h * 64, [[1, 64]])
                    P.mm(o_ap, X3T.ap(0, 64, h * 128 + 64, [[1, 64]]), st_Hb[l].ap(0, 64, h * 64, [[1, 64]]), True, False,
                         [X3T, st_Hb[l]], [pYo], signal=False)
                    P.mm(o_ap, S_sb.ap(0, 128, h * 128 + 64, [[1, 64]]), UV.ap(0, 128, h * 64, [[1, 64]]), False, True,
                         [S_sb, UV], [pYo], signal=(h == 7))
                pH, _ = psum()
                for h in range(8):
                    P.mm(pH.ap(0, 64, h * 64, [[1, 64]]), X2.ap(0, 128, h * 64, [[1, 64]]), UV.ap(0, 128, h * 64, [[1, 64]]),
                         True, True, [X2, UV], [pH], signal=(h == 7))
                P.tt(st_H[l].ap(0, 64, 0, [[64, 8], [1, 64]]), st_H[l].ap(0, 64, 0, [[64, 8], [1, 64]]),
                     sm.ap(0, 64, 16, [[1, 8], [0, 64]]), OP.mult, [st_H[l], sm], [st_H[l]])
                P.tt(st_H[l].ap(0, 64, 0, [[1, 512]]), st_H[l].ap(0, 64, 0, [[1, 512]]), pH.ap(0, 64, 0, [[1, 512]]), OP.add,
                     [st_H[l], pH], [st_H[l]])
                P.copy(st_Hb[l].ap(0, 64, 0, [[1, 512]]), st_H[l].ap(0, 64, 0, [[1, 512]]), [st_H[l]], [st_Hb[l]], eng="act")
                R64 = (64, 64)
                P.copy(Ysb.ap(64, 64, 0, [[1, 512]]), pYo.ap(64, 64, 0, [[1, 512]]), [pYo], [Ysb], eng="act")
                P.red(sm.ap(64, 64, 40, [[1, 8]]), Ysb.ap(64, 64, 0, [[64, 8], [1, 64]]), OP.add, [Ysb], [sm])
                P.act(tmpA.ap(64, 64, 0, [[1, 512]]), Ysb.ap(64, 64, 0, [[1, 512]]), AF.Square, [Ysb], [tmpA])
                P.red(sm.ap(64, 64, 48, [[1, 8]]), tmpA.ap(64, 64, 0, [[64, 8], [1, 64]]), OP.add, [tmpA], [sm])
                P.ts(sm.ap(64, 64, 40, [[1, 8]]), sm.ap(64, 64, 40, [[1, 8]]), 1.0 / 64, OP.mult, [sm], [sm])
                P.tt(sm.ap(64, 64, 56, [[1, 8]]), sm.ap(64, 64, 40, [[1, 8]]), sm.ap(64, 64, 40, [[1, 8]]), OP.mult, [sm], [sm])
                P.stt(sm.ap(64, 64, 48, [[1, 8]]), sm.ap(64, 64, 48, [[1, 8]]), 1.0 / 64, sm.ap(64, 64, 56, [[1, 8]]),
                      OP.mult, OP.subtract, [sm], [sm])
                P.ts(sm.ap(64, 64, 48, [[1, 8]]), sm.ap(64, 64, 48, [[1, 8]]), 64e-5, OP.add, [sm], [sm])
                P.act(sm.ap(64, 64, 48, [[1, 8]]), sm.ap(64, 64, 48, [[1, 8]]), AF.Sqrt, [sm], [sm])
                P.recip(sm.ap(64, 64, 48, [[1, 8]]), sm.ap(64, 64, 48, [[1, 8]]), [sm], [sm])
                Y3 = Ysb.ap(64, 64, 0, [[64, 8], [1, 64]])
                P.tt(Y3, Y3, sm.ap(64, 64, 40, [[1, 8], [0, 64]]), OP.subtract, [Ysb, sm], [Ysb])
                P.tt(Y3, Y3, sm.ap(64, 64, 48, [[1, 8], [0, 64]]), OP.mult, [Ysb, sm], [Ysb])
                Y2 = Ysb.ap(64, 64, 0, [[1, 512]])
                P.tt(Y2, Y2, rowf.ap(64, 64, 0, [[1, 512]]), OP.mult, [Ysb, rowf], [Ysb])
                P.tt(Y2, Y2, rowf.ap(64, 64, 512, [[1, 512]]), OP.add, [Ysb, rowf], [Ysb])
                P.tt(tmpA.ap(64, 64, 0, [[64, 8], [1, 64]]), UV.ap(64, 64, 0, [[64, 8], [1, 64]]),
                     sm.ap(64, 64, 32, [[1, 8], [0, 64]]), OP.mult, [UV, sm], [tmpA])
                P.tt(Y2, Y2, tmpA.ap(64, 64, 0, [[1, 512]]), OP.add, [Ysb, tmpA], [Ysb])
                P.tt(obt.ap(64, 64, 0, [[1, 512]]), Y2, g_tm.ap(64, 64, 0, [[1, 512]]), OP.mult, [Ysb, g_tm], [obt])
                _, pto = psum()
                for h in range(8):
                    P.tr(pto.ap(0, 64, h * 64, [[1, 64]]), obt.ap(64, 64, h * 64, [[1, 64]]),
                         cb.ap(64, 64, CB_IDENT + 64, [[1, 64]]), [obt, cb], [pto], signal=(h == 7))
                P.copy(oT.ap(0, 64, t0, [[TB, 8], [1, 64]]), pto.ap(0, 64, 0, [[64, 8], [1, 64]]), [pto], [oT])

        for s in range(NSEQ):
            for tb in range(NTB):
                c0 = s * T + tb * TB
                first = (tb == 0)
                tbi[0] = tb
                for hh in range(2):
                    P.dma("pool", rotc.ap(0, 128, hh * TB, [[1, TB]]),
                          bass.AP(rot_d.tensor, hh * T + tb * TB, [[2 * T, 128], [1, TB]]), rotc)
                P.dma("sp", xT.t[:, :, :], bass.AP(xT_d.tensor, c0, [[NT, 128], [128 * NT, KD], [1, TB]]), xT)
                for l in range(L):
                    if first:
                        for stt_ in (st_R[l], st_Rb[l], st_H[l], st_Hb[l], st_sh[l]):
                            P.memset(stt_.ap(0, stt_.shape[0], 0, [[1, stt_.rs]]), 0.0, [stt_])
                    P.dma("sp", rowf.t[:, :], bass.AP(rows_d.tensor, l * 2048 + 1024, [[0, 128], [1, 1024]]), rowf)
                    P.dma("pool", lwb.t[:, :], lw_d[:, l * 1536:(l + 1) * 1536], lwb)
                    P.dma("sp", rv33.t[0:33, :], bass.AP(rows_d.tensor, l * 2048, [[0, 33], [1, 1024]]), rv33)
                    P.copy(rhi.t[0:33, :], rv33.t[0:33, :], [rv33], [rhi])
                    P.tt(rv33.t[0:33, :], rv33.t[0:33, :], rhi.t[0:33, :], OP.subtract, [rv33, rhi], [rv33])
                    P.copy(HL.t[0:1, :], rhi.t[0:1, :], [rhi], [HL])
                    P.copy(HL.t[32:33, :], rv33.t[32:33, :], [rv33], [HL])
                    rmsnorm(l, 0)
                    import os as _os
                    SK = _os.environ.get("KSKIP", "")
                    if "a" not in SK:
                        attention(l, first)
                        merge_branch(l, 0)
                    else:
                        P.memset(mixed.ap(0, 128, 0, [[1, KD * TB]]), 0.0, [mixed])
                        if "m" in SK:
                            P.memset(oT.ap(0, 64, 0, [[1, 8 * TB]]), 0.0, [oT])
                            merge_branch(l, 0)
                    if "c" not in SK:
                        retention(l)
                        merge_branch(l, 2)
                    if "b" not in SK:
                        rwkv(l)
                        merge_branch(l, 1)
                    for hf in range(2):
                        w = wpiece(l, 22 + hf)

                        def resid(m, pt, hf=hf):
                            mi = hf * 4 + m
                            P.tt(xT.t[:, mi, :], xT.t[:, mi, :], pt.t[:, :], OP.add, [xT, pt], [xT])
                        proj_fm(w, 512, resid, src=mixed)
                    rmsnorm(l, 8)
                    actT = BIG
                    for i in range(6):
                        ncol = 512 if i < 5 else 256
                        wg = wpiece(l, 24 + 2 * i)
                        wu = wpiece(l, 25 + 2 * i)
                        for m in range(ncol // 128):
                            pg, _ = psum()
                            pu, _ = psum()
                            for k in range(KD):
                                P.mm(pg.t[:, :], wg.ap(0, 128, k * ncol + m * 128, [[1, 128]]), hT.t[:, k, :],
                                     k == 0, k == KD - 1, [wg, hT], [pg])
                            for k in range(KD):
                                P.mm(pu.t[:, :], wu.ap(0, 128, k * ncol + m * 128, [[1, 128]]), hT.t[:, k, :],
                                     k == 0, k == KD - 1, [wu, hT], [pu])
                            P.act(tmpA.t[:, :], pg.t[:, :], AF.Silu, [pg], [tmpA])
                            fi = i * 4 + m
                            P.tt(actT.ap(0, 128, fi * 512, [[1, 512]]), pu.t[:, :], tmpA.t[:, :], OP.mult, [pu, tmpA], [actT])
                    for m in range(8):
                        w = wpiece(l, 36 + m)
                        pt, _ = psum()
                        for k in range(KF):
                            P.mm(pt.t[:, :], w.ap(0, 128, k * 128, [[1, 128]]), actT.ap(0, 128, k * 512, [[1, 512]]),
                                 k == 0, k == KF - 1, [w, actT], [pt])
                        P.tt(xT.t[:, m, :], xT.t[:, m, :], pt.t[:, :], OP.add, [xT, pt], [xT])
                P.dma("sp", bass.AP(yT_d.tensor, c0, [[NT, 128], [128 * NT, KD], [1, TB]]), xT.t[:, :, :], xT, load=False)
        with nc.Block() as block:
            P.finish(block, [xT, oT])
        P.stats = {e: len(P.ins[e]) for e in P.ENGS}
        nc._prog_stats = (P.stats, P.nwaits, P.ndsem)
        nc._tags = P.tags
        nc._P = P
    return nc


def prep_inputs(inp, x_cores, T, L):
    consts = make_consts(T)
    wpk = np.stack([pack_layer(inp, l) for l in range(L)])
    vecs = pack_vecs(inp, L).reshape(128, L * NVEC)
    lw, rows, sinks = pack_small(inp, L)
    shared = {"wpk": wpk, "vecs": np.ascontiguousarray(vecs), "lw": np.ascontiguousarray(lw.reshape(128, L * 1536)),
              "rows": np.ascontiguousarray(rows.reshape(L, 2048)), "sinks": np.ascontiguousarray(sinks.reshape(1, L * 8)),
              "cf": consts["cf"], "cb": consts["cb"], "rot": consts["rot"]}
    maps = []
    for xc in x_cores:
        xT = np.ascontiguousarray(xc.reshape(-1, D).T)
        m = dict(shared)
        m["xT"] = xT
        maps.append(m)
    return maps


def kernel(**inputs):
    inp = {k: np.asarray(v, dtype=np.float32) for k, v in inputs.items()}
    x = inp["x"]
    B, T, _ = x.shape
    L = inp["w_in"].shape[0]
    ncores = 8
    nseq = B // ncores
    nc = build(nseq, T, L)
    maps = prep_inputs(inp, [x[c * nseq:(c + 1) * nseq] for c in range(ncores)], T, L)
    res = run_bass_kernel_spmd(nc, maps, core_ids=list(range(ncores)))
    out = np.empty((B, T, D), np.float32)
    for c in range(ncores):
        yT = np.asarray(res.results[c]["yT"])
        out[c * nseq:(c + 1) * nseq] = yT.T.reshape(nseq, T, D)
    return out
```

```python
from contextlib import ExitStack
import numpy as np
import concourse.bass as bass
import concourse.mybir as mybir
from concourse.bass_utils import run_bass_kernel_spmd

F32 = mybir.dt.float32
BF16 = mybir.dt.bfloat16
AF = mybir.ActivationFunctionType
OP = mybir.AluOpType
AX = mybir.AxisListType

D = 1024
KD = 8
TB = 512
FF = 2816
KF = 22
NPIECE = 44
PW = 4096
NVEC = 48
LOG_DECAY_C = -float(np.exp(-0.5))


class Buf:
    def __init__(self, name):
        self.name = name
        self.w = None
        self.r = {}
        self.dsem = None
        self.dcnt = 0


class Tl:
    def __init__(self, t, shape, buf=None, name=""):
        self.t = t
        self.shape = list(shape)
        self.rs = int(np.prod(shape[1:]))
        self.buf = buf or Buf(name)

    def ap(self, p0, npart, off, dims):
        return bass.AP(self.t, p0 * self.rs + off, [[self.rs, npart]] + [list(d) for d in dims])

    def __getitem__(self, idx):
        return self.t[idx]


class Prog:
    ENGS = ["pe", "dve", "act", "pool", "sp"]

    def __init__(self, nc, es):
        self.nc = nc
        self.es = es
        self.sem = {e: es.enter_context(nc.semaphore("s_" + e)) for e in self.ENGS}
        self.cnt = {e: 0 for e in self.ENGS}
        self.ins = {e: [] for e in self.ENGS}
        self.seen = {e: {} for e in self.ENGS}
        self.semobj = dict(self.sem)
        self.ndsem = 0
        self.nwaits = 0
        self.tags = {}

    def _waits(self, eng, deps):
        need = {}
        for d in deps:
            if d is None:
                continue
            k, v = d
            if k == eng and eng == "pe":
                continue
            if v > need.get(k, 0):
                need[k] = v
        out = []
        for k, v in need.items():
            if self.seen[eng].get(k, 0) >= v:
                continue
            self.seen[eng][k] = v
            out.append((self.semobj[k], v))
        self.nwaits += len(out)
        return out

    def op(self, eng, fn, reads=(), writes=(), signal=True):
        deps = []
        for b in reads:
            deps.append(b.buf.w)
        for b in writes:
            deps.append(b.buf.w)
            deps.extend(b.buf.r.items())
        waits = self._waits(eng, deps)
        val = self.cnt[eng] + 1
        if signal:
            self.cnt[eng] = val
        import sys as _sys
        fr = _sys._getframe(1)
        while fr.f_code.co_name in ("op", "mm", "tr", "act", "tt", "ts", "stt", "copy", "red", "memset", "recip", "<lambda>"):
            fr = fr.f_back
        tag = "%s:%d" % (fr.f_code.co_name, fr.f_lineno)
        self.ins[eng].append((waits, fn, (self.sem[eng], 1) if signal else None, tag))
        for b in reads:
            if b.buf.r.get(eng, 0) < val:
                b.buf.r[eng] = val
        for b in writes:
            b.buf.w = (eng, val)
            b.buf.r = {}

    def _dsem(self, buf):
        if buf.dsem is None:
            buf.dsem = "d%d" % self.ndsem
            self.ndsem += 1
            self.semobj[buf.dsem] = self.es.enter_context(self.nc.semaphore(buf.dsem))
        return buf.dsem

    def dma(self, q, out, in_, tile, load=True):
        b = tile.buf
        k = self._dsem(b)
        deps = [b.w]
        if load:
            deps.extend(b.r.items())
        waits = self._waits(q, deps)
        b.dcnt += 16
        self.ins[q].append((waits, lambda e: e.dma_start(out=out, in_=in_), (self.semobj[k], 16), "dma"))
        if load:
            b.w = (k, b.dcnt)
            b.r = {}
        else:
            b.r[k] = b.dcnt

    def inherit(self, dsts, srcs):
        acc = {}
        for s_ in srcs:
            items = list(s_.buf.r.items())
            if s_.buf.w is not None:
                items.append(s_.buf.w)
            for k, v in items:
                if v > acc.get(k, 0):
                    acc[k] = v
        for d_ in dsts:
            d_.buf.w = None
            d_.buf.r = dict(acc)

    def finish(self, block, final_bufs):
        waits = []
        for b in final_bufs:
            if b.buf.dsem is not None:
                waits.append((self.semobj[b.buf.dsem], b.buf.dcnt))
        self.ins["sp"].append((waits, None, None, "end"))
        engmap = {"pe": block.tensor, "dve": block.vector, "act": block.scalar,
                  "pool": block.gpsimd, "sp": block.sync}
        for e in self.ENGS:
            lst = self.ins[e]

            def body(eng, lst=lst):
                for waits, fn, inc, tag in lst:
                    for s, v in waits:
                        eng.wait_ge(s, v)
                    if fn is None:
                        continue
                    i = fn(eng)
                    try:
                        self.tags[str(i.ins.name)] = tag
                    except Exception:
                        pass
                    if inc is not None:
                        i.then_inc(inc[0], inc[1])
            engmap[e](body)

    def mm(self, out, lhsT, rhs, start, stop, reads, writes, signal=None):
        if signal is None:
            signal = stop
        self.op("pe", lambda e: e.matmul(out, lhsT=lhsT, rhs=rhs, start=start, stop=stop),
                reads, writes, signal)

    def tr(self, out, in_, ident, reads, writes, signal=True):
        self.op("pe", lambda e: e.transpose(out, in_, ident), reads, writes, signal)

    def act(self, out, in_, func, reads, writes, scale=1.0, bias=None):
        if bias is None:
            self.op("act", lambda e: e.activation(out=out, in_=in_, func=func, scale=scale), reads, writes)
        else:
            self.op("act", lambda e: e.activation(out=out, in_=in_, func=func, scale=scale, bias=bias),
                    reads, writes)

    def tt(self, out, in0, in1, op, reads, writes, eng="dve"):
        self.op(eng, lambda e: e.tensor_tensor(out=out, in0=in0, in1=in1, op=op), reads, writes)

    def ts(self, out, in0, s1, op0, reads, writes, s2=None, op1=None, eng="dve"):
        if op1 is None:
            self.op(eng, lambda e: e.tensor_scalar(out=out, in0=in0, scalar1=s1, scalar2=None, op0=op0),
                    reads, writes)
        else:
            self.op(eng, lambda e: e.tensor_scalar(out=out, in0=in0, scalar1=s1, scalar2=s2, op0=op0, op1=op1),
                    reads, writes)

    def stt(self, out, in0, scalar, in1, op0, op1, reads, writes):
        self.op("dve", lambda e: e.scalar_tensor_tensor(out=out, in0=in0, scalar=scalar, in1=in1,
                                                        op0=op0, op1=op1), reads, writes)

    def copy(self, out, in_, reads, writes, eng="dve"):
        if eng == "act":
            self.op("act", lambda e: e.copy(out=out, in_=in_), reads, writes)
        else:
            self.op(eng, lambda e: e.tensor_copy(out=out, in_=in_), reads, writes)

    def red(self, out, in_, op, reads, writes):
        self.op("dve", lambda e: e.tensor_reduce(out=out, in_=in_, op=op, axis=AX.X), reads, writes)

    def memset(self, ap, val, writes, eng="dve"):
        self.op(eng, lambda e: e.memset(ap, val), [], writes)

    def recip(self, out, in_, reads, writes):
        self.op("dve", lambda e: e.reciprocal(out=out, in_=in_), reads, writes)


def _bf(a):
    import ml_dtypes
    return np.asarray(a, dtype=np.float32).astype(ml_dtypes.bfloat16)


def make_consts(T):
    c = {}
    idx = np.arange(128)
    ident = np.eye(128, dtype=np.float32)
    s = np.arange(64)[:, None]
    t = np.arange(64)[None, :]
    tri1 = np.concatenate([(s < t), (s <= t)], axis=1).astype(np.float32)
    tri3 = np.concatenate([(s > t), (s > t)], axis=1).astype(np.float32)
    tri = np.zeros((128, 256), np.float32)
    tri[:64, :128] = tri1 * LOG_DECAY_C
    tri[:64, 128:] = tri3 * LOG_DECAY_C
    half = 32
    inv_freq = 1.0 / (10000.0 ** (np.arange(half, dtype=np.float32) * 2.0 / 64))
    pos = np.arange(T, dtype=np.float32)
    ang = pos[None, :] * inv_freq[:, None]
    cosT = np.cos(ang)[idx % 32]
    sinT = np.sin(ang)[idx % 32] * np.where((idx % 64) < 32, -1.0, 1.0)[:, None]
    H = 8
    log_gamma = np.log1p(-np.power(2.0, -5.0 - np.arange(H, dtype=np.float64)))
    i = np.arange(128, dtype=np.float64)
    xi = np.exp(log_gamma[:, None] * (i[None, :] + 1.0))
    kf = (64 ** -0.5) * np.exp(-log_gamma[:, None] * (i[None, :] + 1.0))
    gc = np.exp(log_gamma * 128.0)
    XI = np.zeros((128, 4, 128), np.float32)
    KFt = np.zeros((128, 4, 128), np.float32)
    GC = np.zeros((128, 4), np.float32)
    for h in range(H):
        rows = slice((h % 2) * 64, (h % 2) * 64 + 64)
        XI[rows, h // 2, :] = xi[h][None, :]
        KFt[rows, h // 2, :] = kf[h][None, :]
        GC[rows, h // 2] = gc[h]
    ones = np.full((128, 64), LOG_DECAY_C, np.float32)
    c["cf"] = np.concatenate([ident, tri, XI.reshape(128, -1), KFt.reshape(128, -1), GC, ones], axis=1)
    c["rot"] = np.concatenate([cosT, sinT], axis=1).astype(np.float32)
    identb = np.eye(128, dtype=np.float32)
    blk64 = (idx[:, None] // 64 == idx[None, :] // 64).astype(np.float32) / 64.0
    onesD = np.full((128, 128), 1.0 / 1024.0, np.float32)
    mdiag = (idx[:, None] <= idx[None, :]).astype(np.float32)
    mprev = (idx[:, None] > idx[None, :]).astype(np.float32)
    perm = np.zeros((128, 128), np.float32)
    for p in range(128):
        q = p + 32 if (p % 64) < 32 else p - 32
        perm[q, p] = 1.0
    m4 = np.zeros((128, 128), np.float32)
    ss = np.arange(64)[:, None]
    tt = np.arange(64)[None, :]
    for w in range(2):
        m4[w * 64:(w + 1) * 64, 0:64] = (ss < tt)
        m4[w * 64:(w + 1) * 64, 64:128] = (ss <= tt)
    mst = np.zeros((128, 64), np.float32)
    mst[:64] = (np.arange(64)[:, None] > np.arange(64)[None, :])
    ones128 = np.ones((128, 128), np.float32)
    c["cb"] = _bf(np.concatenate([identb, blk64, onesD, mdiag, mprev, perm, m4, mst, ones128], axis=1))
    return c


CF_IDENT, CF_TRI, CF_XI, CF_KF, CF_GC, CF_ONES = 0, 128, 384, 896, 1408, 1412
CF_W = 1412 + 64
CB_IDENT, CB_BLK, CB_OND, CB_MD, CB_MP, CB_PERM, CB_M4, CB_MST, CB_ONES = 0, 128, 256, 384, 512, 640, 768, 896, 960
CB_W = 960 + 128


def _fm(W):
    K, N = W.shape
    kc = K // 128
    out = np.zeros((128, PW), np.float32)
    out[:, :kc * N] = W.reshape(kc, 128, N).transpose(1, 0, 2).reshape(128, kc * N)
    return out


def _hm(W, c0):
    out = np.zeros((128, PW), np.float32)
    out[:64] = W[:, c0:c0 + 512].reshape(8, 64, 512).transpose(1, 0, 2).reshape(64, 4096)
    return out


def pack_layer(inp, l):
    w_in = inp["w_in"][l]
    P = []
    P.append(_fm(w_in[:, 0:512]))
    akv = np.concatenate([w_in[:, 512:576], w_in[:, 512:576], w_in[:, 576:640], w_in[:, 576:640],
                          w_in[:, 640:768]], axis=1)
    P.append(_fm(akv))
    P.append(_fm(w_in[:, 2560:3072]))
    P.append(_fm(w_in[:, 3072:3584]))
    P.append(_fm(w_in[:, 3584:4096]))
    P.append(_fm(w_in[:, 4096:4608]))
    P.append(_fm(w_in[:, 768:1280]))
    P.append(_fm(w_in[:, 1280:1792]))
    P.append(_fm(w_in[:, 1792:2304]))
    P.append(_fm(w_in[:, 2304:2560]))
    for b, wo in enumerate([inp["w_attn_o"][l], inp["w_rwkv_o"][l], inp["w_ret_o"][l]]):
        for hf in range(2):
            P.append(_hm(wo, hf * 512))
            c0 = 4608 + b * 1024 + hf * 512
            P.append(_fm(w_in[:, c0:c0 + 512]))
    for hf in range(2):
        P.append(_fm(inp["w_out"][l][:, hf * 512:(hf + 1) * 512]))
    for i in range(6):
        c0 = i * 512
        c1 = min(c0 + 512, FF)
        P.append(_fm(inp["w_ffn_gate"][l][:, c0:c1]))
        P.append(_fm(inp["w_ffn_up"][l][:, c0:c1]))
    wd = inp["w_ffn_down"][l]
    for m in range(8):
        P.append(_fm(wd[:, m * 128:(m + 1) * 128]))
    assert len(P) == NPIECE
    return np.stack(P)


def pack_vecs(inp, L):
    v = np.zeros((128, L, NVEC), np.float32)
    idx = np.arange(128)

    def fm(a):
        return a.reshape(-1, 128).T

    for l in range(L):
        v[:, l, 0:8] = fm(inp["norm1_g"][l])
        v[:, l, 8:16] = fm(inp["norm2_g"][l])
        mu = inp["rwkv_shift_mu"][l]
        v[:, l, 16:28] = fm(mu[0:1536])
        v[:64, l, 28] = mu[1536:1600]
        v[:64, l, 29] = mu[1600:1664]
        v[:, l, 30] = mu[1664:1792]
        v[:, l, 31:35] = fm(inp["rwkv_k_k"][l])
        v[:, l, 35:39] = fm(inp["rwkv_k_a"][l])
        v[:, l, 39:43] = fm(inp["rwkv_r_k"][l].reshape(-1))
        v[:, l, 43] = inp["attn_q_norm_g"][l][idx % 64]
        v[:, l, 44] = inp["attn_k_norm_g"][l][idx % 64]
    return v


def pack_small(inp, L):
    lw = np.zeros((128, L, 3, 512), np.float32)
    rows = np.zeros((L, 4, 512), np.float32)
    for l in range(L):
        lw[:64, l, 0] = inp["rwkv_w2"][l]
        lw[:64, l, 1] = inp["rwkv_a2"][l]
        lw[:, l, 2] = inp["rwkv_g2"][l]
        rows[l, 0] = inp["rwkv_w0"][l]
        rows[l, 1] = inp["rwkv_a0"][l]
        rows[l, 2] = inp["rwkv_lnx_g"][l]
        rows[l, 3] = inp["rwkv_lnx_b"][l]
    sinks = np.asarray(inp["attn_sinks"], np.float32)[:L].reshape(L, 8)
    return lw, rows, sinks


def build(NSEQ, T, L, debug=False):
    nc = bass.Bass("TRN2", target_bir_lowering=False)
    NTB = T // TB
    NT = NSEQ * T
    xT_d = nc.dram_tensor("xT", [D, NT], F32, kind="ExternalInput").ap()
    wpk_d = nc.dram_tensor("wpk", [L, NPIECE, 128, PW], F32, kind="ExternalInput").ap()
    vec_d = nc.dram_tensor("vecs", [128, L * NVEC], F32, kind="ExternalInput").ap()
    lw_d = nc.dram_tensor("lw", [128, L * 1536], F32, kind="ExternalInput").ap()
    rows_d = nc.dram_tensor("rows", [L, 2048], F32, kind="ExternalInput").ap()
    sink_d = nc.dram_tensor("sinks", [1, L * 8], F32, kind="ExternalInput").ap()
    cf_d = nc.dram_tensor("cf", [128, CF_W], F32, kind="ExternalInput").ap()
    cb_d = nc.dram_tensor("cb", [128, CB_W], BF16, kind="ExternalInput").ap()
    rot_d = nc.dram_tensor("rot", [128, 2 * T], F32, kind="ExternalInput").ap()
    yT_d = nc.dram_tensor("yT", [D, NT], F32, kind="ExternalOutput").ap()
    dbg_d = None
    if debug:
        dbg_d = nc.dram_tensor("dbg", [3, 64, 8 * TB], BF16, kind="ExternalOutput").ap()

    es = ExitStack()
    with es:
        P = Prog(nc, es)

        def sb(name, shape, dt=F32):
            return Tl(es.enter_context(nc.sbuf_tensor("s_" + name, list(shape), dt)), shape, name=name)

        def view(base, name, shape, dt, col0_bytes):
            raise NotImplementedError

        rvtm = sb("rvtm", [128, 4, 512], BF16)
        cf = sb("cf", [128, CF_W])
        cb = sb("cb", [128, CB_W], BF16)
        rotc = sb("rotb", [128, 2 * TB], BF16)
        vecs = sb("vecs", [128, L * NVEC])
        lwb = sb("lwb", [128, 1536], BF16)
        rowf = sb("rowf", [128, 1024])
        rv33 = sb("rv33", [64, 1024])
        HL = sb("HL", [64, 1024], BF16)
        sinkx = sb("sinkx", [128, L * 8])
        eps6 = sb("eps6", [128, 1])
        P.dma("sp", cf.t[:, :], cf_d, cf)
        P.dma("sp", cb.t[:, :], cb_d, cb)
        P.dma("sp", vecs.t[:, :], vec_d, vecs)
        P.dma("sp", sinkx.t[:, :], bass.AP(sink_d.tensor, 0, [[0, 128], [1, L * 8]]), sinkx)
        P.act(sinkx.t[:, :], sinkx.t[:, :], AF.Exp, [sinkx], [sinkx])
        P.memset(eps6.t[:, :], 1e-6, [eps6])
        P.memset(HL.t[:, :], 0.0, [HL])

        def cba(col, w, p0=0, npart=128):
            return cb.ap(p0, npart, col, [[1, w]])

        def vcol(l, j, p0=0, npart=128):
            return vecs.ap(p0, npart, l * NVEC + j, [[1, 1]])

        xT = sb("xT", [128, KD, TB])
        hT = sb("hT", [128, KD, TB], BF16)
        NW = 3
        wring = [sb("w%d" % i, [128, PW], BF16) for i in range(NW)]
        ps = [Tl(es.enter_context(nc.psum_tensor("ps%d" % i, [128, 512], F32)), [128, 512], name="ps%d" % i)
              for i in range(8)]
        psb = [Tl(p.t.bitcast(BF16), [128, 1024], buf=p.buf) for p in ps]
        pctr = [0]

        def psum():
            i = pctr[0] % 8
            pctr[0] += 1
            return ps[i], psb[i]

        oT = sb("oT", [64, 8, TB], BF16)
        B1 = sb("B1", [128, 4096], BF16)
        BIG = sb("BIG", [128, 3 * 4096], BF16)
        mixed = sb("mixed", [128, KD, TB], BF16)
        tmpA = sb("tmpA", [128, TB])
        tmpB = sb("tmpB", [128, TB])
        tmpD = sb("tmpD", [128, TB], BF16)
        rgtm = sb("rgtm", [128, 4, 512], BF16)
        vtm = sb("vtm", [128, 4, 128], BF16)
        PT = [sb("PT0", [128, 1024], BF16), None]
        ktm = sb("ktm", [128, 4, 512], BF16)
        sT = sb("sT", [128, 1024], BF16)
        PT[1] = sT
        rhi = sT
        rwsb = sb("rwsb", [128, TB + 1])
        wdx = sb("wdx", [64, 2 * TB], BF16)
        adx = sb("adx", [64, 2 * TB], BF16)
        gdx = sb("gdx", [128, 2 * TB], BF16)
        a_tm = sb("a_tm", [128, 512])
        sig = sb("sig", [128, 512])
        g_tm = sb("g_tm", [128, 512])
        E3 = sb("E3", [128, 512])
        E2 = sb("E2", [128, 512])
        Q1 = sb("Q1", [128, 512])
        nk = sb("nk", [128, 512])
        Ysb = nk
        rstd = tmpB
        X3 = sb("X3", [128, 512], BF16)
        X2 = sb("X2", [128, 512], BF16)
        UV = sb("UV", [128, 512], BF16)
        obt = sb("obt", [128, 512], BF16)
        octm = obt
        X3T = sb("X3T", [64, 8, 128], BF16)
        X1T = sb("X1T", [64, 8, 128], BF16)
        S_sb = sb("S_sb", [128, 8, 128], BF16)
        Apow = [sb("Apow%d" % i, [64, 8, 64], BF16) for i in range(2)]
        Npow = [sb("Npow%d" % i, [64, 8, 64], BF16) for i in range(2)]
        Xf = sb("Xf", [64, 8, 64])
        Xb = sb("Xb", [64, 8, 64], BF16)
        sm = sb("sm", [128, 64])
        st_k = [sb("stk%d" % l, [128, 2, 128], BF16) for l in range(L)]
        st_v = [sb("stv%d" % l, [128, 128], BF16) for l in range(L)]
        st_R = [sb("stR%d" % l, [128, 4, 64]) for l in range(L)]
        st_Rb = [sb("stRb%d" % l, [128, 8, 64], BF16) for l in range(L)]
        st_H = [sb("stH%d" % l, [64, 8, 64]) for l in range(L)]
        st_Hb = [sb("stHb%d" % l, [64, 8, 64], BF16) for l in range(L)]
        st_sh = [sb("stsh%d" % l, [128, 16]) for l in range(L)]

        wq = {"i": 0}
        tbi = [0]

        def wpiece(l, j):
            tl = wring[wq["i"] % NW]
            wq["i"] += 1
            P.dma("pool", tl.ap(0, 128, 0, [[2048, 2], [1, 2048]]),
                  bass.AP(wpk_d.tensor, (l * NPIECE + j) * 128 * PW, [[PW, 128], [2048, 2], [1, 2048]]), tl)
            return tl

        def rmsnorm(l, gcol):
            sq = BIG
            P.act(sq.ap(0, 128, 0, [[1, KD * TB]]), xT.ap(0, 128, 0, [[1, KD * TB]]), AF.Square, [xT], [sq])
            pt, _ = psum()
            for k in range(KD):
                P.mm(pt.t[:, :], cba(CB_OND, 128), sq.ap(0, 128, k * TB, [[1, TB]]), k == 0, k == KD - 1, [cb, sq], [pt])
            P.act(rstd.t[:, :], pt.t[:, :], AF.Ln, [pt, eps6], [rstd], bias=eps6.t[:, 0:1])
            P.act(rstd.t[:, :], rstd.t[:, :], AF.Exp, [rstd], [rstd], scale=-0.5)
            for k in range(KD):
                P.stt(hT.t[:, k, :], xT.t[:, k, :], vcol(l, gcol + k), rstd.t[:, :], OP.mult, OP.mult,
                      [xT, vecs, rstd], [hT])

        def proj_fm(w, ncol, cb_fn, src=None, M=128):
            src = src or hT
            for m in range(ncol // M):
                pt, ptb = psum()
                for k in range(KD):
                    P.mm(pt.ap(0, M, 0, [[1, TB]]), w.ap(0, 128, k * ncol + m * M, [[1, M]]),
                         src.t[:, k, :], k == 0, k == KD - 1, [w, src], [pt])
                cb_fn(m, pt)

        def headnorm(pt, dst_ap, gcolap, dstT):
            P.act(tmpD.t[:, :], pt.t[:, :], AF.Square, [pt], [tmpD])
            p2, _ = psum()
            P.mm(p2.t[:, :], cba(CB_BLK, 128), tmpD.t[:, :], True, True, [cb, tmpD], [p2])
            P.act(tmpA.t[:, :], p2.t[:, :], AF.Ln, [p2, eps6], [tmpA], bias=eps6.t[:, 0:1])
            P.act(tmpA.t[:, :], tmpA.t[:, :], AF.Exp, [tmpA], [tmpA], scale=-0.5)
            P.stt(dst_ap, pt.t[:, :], gcolap, tmpA.t[:, :], OP.mult, OP.mult, [pt, vecs, tmpA], [dstT])

        def merge_branch(l, b):
            for hf in range(2):
                wo = wpiece(l, 10 + b * 4 + hf * 2)
                wg = wpiece(l, 11 + b * 4 + hf * 2)
                for m in range(4):
                    pg, _ = psum()
                    for k in range(KD):
                        P.mm(pg.t[:, :], wg.ap(0, 128, k * 512 + m * 128, [[1, 128]]), hT.t[:, k, :],
                             k == 0, k == KD - 1, [wg, hT], [pg])
                    po, _ = psum()
                    for h in range(8):
                        P.mm(po.t[:, :], wo.ap(0, 64, h * 512 + m * 128, [[1, 128]]), oT.t[:, h, :],
                             h == 0, h == 7, [wo, oT], [po])
                    P.act(tmpA.t[:, :], pg.t[:, :], AF.Sigmoid, [pg], [tmpA])
                    mi = hf * 4 + m
                    if b == 0:
                        P.tt(mixed.t[:, mi, :], po.t[:, :], tmpA.t[:, :], OP.mult, [po, tmpA], [mixed])
                    else:
                        P.tt(tmpB.t[:, :], po.t[:, :], tmpA.t[:, :], OP.mult, [po, tmpA], [tmpB])
                        P.tt(mixed.t[:, mi, :], mixed.t[:, mi, :], tmpB.t[:, :], OP.add, [mixed, tmpB], [mixed])
            if debug:
                P.dma("sp", dbg_d[b], oT.ap(0, 64, 0, [[1, 8 * TB]]), oT, load=False)

        class _Stop(Exception):
            pass

        def stage(i):
            import os as _os
            if int(_os.environ.get("KSTOP", "99")) < i:
                raise _Stop()

        def attention(l, first):
            try:
                attention_(l, first)
            except _Stop:
                pass

        def attention_(l, first):
            import os as _os
            if "KSTOP" in _os.environ:
                P.memset(oT.ap(0, 64, 0, [[1, 8 * TB]]), 0.0, [oT])
            w = wpiece(l, 0)
            proj_fm(w, 512, lambda m, pt: headnorm(pt, B1.ap(0, 128, m * 512, [[1, 512]]), vcol(l, 43), B1))
            stage(2)
            w = wpiece(l, 1)
            for m in range(2):
                pt, _ = psum()
                for k in range(KD):
                    P.mm(pt.t[:, :], w.ap(0, 128, k * 384 + m * 128, [[1, 128]]), hT.t[:, k, :],
                         k == 0, k == KD - 1, [w, hT], [pt])
                headnorm(pt, B1.ap(0, 128, 2048 + m * 512, [[1, 512]]), vcol(l, 44), B1)
            stage(3)
            for n in range(4):
                pt, _ = psum()
                for k in range(KD):
                    P.mm(pt.ap(0, 128, 0, [[1, 128]]), hT.t[:, k, n * 128:(n + 1) * 128],
                         w.ap(0, 128, k * 384 + 256, [[1, 128]]), k == 0, k == KD - 1, [w, hT], [pt])
                P.copy(vtm.t[:, n, :], pt.t[:, 0:128], [pt], [vtm], eng="act")
            stage(4)
            for n in range(4):
                blocks = []
                if not (first and n == 0):
                    blocks.append(0)
                blocks.append(1)
                pts = {}
                for jb in blocks:
                    pa, _ = psum()
                    pb, _ = psum()
                    for h in range(8):
                        g = h // 4
                        base = (h % 2) * 64
                        if jb == 1:
                            kap = B1.ap(base, 64, 2048 + g * 512 + n * 128, [[1, 128]])
                            kr = [B1]
                        elif n == 0:
                            kap = st_k[l].ap(base, 64, g * 128, [[1, 128]])
                            kr = [st_k[l]]
                        else:
                            kap = B1.ap(base, 64, 2048 + g * 512 + (n - 1) * 128, [[1, 128]])
                            kr = [B1]
                        qap = B1.ap(base, 64, (h // 2) * 512 + n * 128, [[1, 128]])
                        pt = pa if h % 2 == 0 else pb
                        P.mm(pt.ap(0, 128, (h // 2) * 128, [[1, 128]]), kap, qap, True, True,
                             kr + [B1], [pt], signal=(h // 2 == 3))
                    pts[jb] = (pa, pb)
                stage(5)
                for jb in blocks:
                    for par, pt in enumerate(pts[jb]):
                        P.act(PT[jb].ap(0, 128, par * 128, [[256, 4], [1, 128]]), pt.ap(0, 128, 0, [[128, 4], [1, 128]]),
                              AF.Exp, [pt], [PT[jb]], scale=0.125)
                    stage(6)
                    mcol = CB_MP if jb == 0 else CB_MD
                    P.tt(PT[jb].ap(0, 128, 0, [[128, 8], [1, 128]]), PT[jb].ap(0, 128, 0, [[128, 8], [1, 128]]),
                         cb.ap(0, 128, mcol, [[0, 8], [1, 128]]), OP.mult, [PT[jb], cb], [PT[jb]])
                stage(7)
                for g in range(2):
                    po, _ = psum()
                    pd, _ = psum()
                    for bi, jb in enumerate(blocks):
                        if jb == 1:
                            vap = vtm.ap(0, 128, n * 128 + g * 64, [[1, 64]])
                            vr = [vtm]
                        elif n == 0:
                            vap = st_v[l].ap(0, 128, g * 64, [[1, 64]])
                            vr = [st_v[l]]
                        else:
                            vap = vtm.ap(0, 128, (n - 1) * 128 + g * 64, [[1, 64]])
                            vr = [vtm]
                        rhs = PT[jb].ap(0, 128, g * 512, [[1, 512]])
                        P.mm(po.ap(0, 64, 0, [[1, 512]]), vap, rhs, bi == 0, bi == len(blocks) - 1, vr + [PT[jb]], [po])
                        P.mm(pd.ap(0, 64, 0, [[1, 512]]), cba(CB_ONES, 64), rhs, bi == 0, bi == len(blocks) - 1,
                             [cb, PT[jb]], [pd])
                    stage(8)
                    P.tt(tmpA.ap(0, 64, 0, [[128, 4], [1, 128]]), pd.ap(0, 64, 0, [[128, 4], [1, 128]]),
                         sinkx.ap(0, 64, l * 8 + g * 4, [[1, 4], [0, 128]]), OP.add, [pd, sinkx], [tmpA])
                    P.act(tmpA.ap(0, 64, 0, [[1, 512]]), tmpA.ap(0, 64, 0, [[1, 512]]), AF.Ln, [tmpA], [tmpA])
                    P.act(tmpA.ap(0, 64, 0, [[1, 512]]), tmpA.ap(0, 64, 0, [[1, 512]]), AF.Exp, [tmpA], [tmpA], scale=-1.0)
                    P.tt(oT.ap(0, 64, g * 4 * TB + n * 128, [[TB, 4], [1, 128]]),
                         po.ap(0, 64, 0, [[128, 4], [1, 128]]), tmpA.ap(0, 64, 0, [[128, 4], [1, 128]]),
                         OP.mult, [po, tmpA], [oT])
            for g in range(2):
                P.copy(st_k[l].t[:, g, :], B1.ap(0, 128, 2048 + g * 512 + 384, [[1, 128]]), [B1], [st_k[l]], eng="act")
            P.copy(st_v[l].t[:, :], vtm.t[:, 3, :], [vtm], [st_v[l]], eng="act")

        def retention(l):
            try:
                retention_(l)
            except _Stop:
                pass

        def retention_(l):
            import os as _os
            if "KSTOP" in _os.environ:
                P.memset(oT.ap(0, 64, 0, [[1, 8 * TB]]), 0.0, [oT])

            def rotary(pt, dst_ap, fac_col, m):
                P.copy(tmpD.t[:, :], pt.t[:, :], [pt], [tmpD], eng="act")
                p2, _ = psum()
                P.mm(p2.t[:, :], cba(CB_PERM, 128), tmpD.t[:, :], True, True, [cb, tmpD], [p2])
                import os as _os
                KR = _os.environ.get("KROT", "")
                if KR == "1":
                    P.copy(tmpA.t[:, :], p2.t[:, :], [p2], [tmpA])
                    P.copy(dst_ap, tmpA.ap(0, 128, 0, [[128, 4], [1, 128]]), [tmpA], [B1])
                    return
                if KR == "3":
                    P.tt(tmpA.t[:, :], pt.t[:, :], cf.ap(0, 128, 0, [[1, 512]]), OP.mult, [pt, cf], [tmpA])
                    P.tt(tmpB.t[:, :], p2.t[:, :], cf.ap(0, 128, 512, [[1, 512]]), OP.mult, [p2, cf], [tmpB])
                else:
                    P.copy(E3.t[:, :], pt.t[:, :], [pt], [E3], eng="act")
                    P.tt(tmpA.t[:, :], E3.t[:, :], rotc.ap(0, 128, 0, [[1, TB]]), OP.mult, [E3, rotc], [tmpA])
                    P.tt(tmpB.t[:, :], p2.t[:, :], rotc.ap(0, 128, TB, [[1, TB]]), OP.mult, [p2, rotc], [tmpB])
                P.tt(tmpA.t[:, :], tmpA.t[:, :], tmpB.t[:, :], OP.add, [tmpA, tmpB], [tmpA])
                if KR == "2":
                    P.copy(dst_ap, tmpA.ap(0, 128, 0, [[128, 4], [1, 128]]), [tmpA], [B1])
                    return
                P.tt(dst_ap, tmpA.ap(0, 128, 0, [[128, 4], [1, 128]]),
                     cf.ap(0, 128, fac_col + m * 128, [[0, 4], [1, 128]]), OP.mult, [tmpA, cf], [B1])

            w = wpiece(l, 2)
            proj_fm(w, 512, lambda m, pt: rotary(pt, B1.ap(0, 128, m * 512, [[128, 4], [1, 128]]), CF_XI, m))
            stage(10)
            w = wpiece(l, 3)
            proj_fm(w, 512, lambda m, pt: rotary(pt, B1.ap(0, 128, 2048 + m * 512, [[128, 4], [1, 128]]), CF_KF, m))
            stage(11)
            for n in range(4):
                _, ptb = psum()
                for m in range(4):
                    P.tr(ptb.ap(0, 128, m * 128, [[1, 128]]), B1.ap(0, 128, 2048 + m * 512 + n * 128, [[1, 128]]),
                         cba(CB_IDENT, 128), [B1, cb], [ptb], signal=(m == 3))
                P.copy(ktm.t[:, n, :], ptb.t[:, 0:512], [ptb], [ktm], eng="act")
            stage(12)
            w = wpiece(l, 4)
            for n in range(4):
                pt, _ = psum()
                for k in range(KD):
                    P.mm(pt.t[:, :], hT.t[:, k, n * 128:(n + 1) * 128], w.ap(0, 128, k * 512, [[1, 512]]),
                         k == 0, k == KD - 1, [w, hT], [pt])
                P.copy(rvtm.t[:, n, :], pt.t[:, :], [pt], [rvtm], eng="act")
            w = wpiece(l, 5)
            for n in range(4):
                pt, _ = psum()
                for k in range(KD):
                    P.mm(pt.t[:, :], hT.t[:, k, n * 128:(n + 1) * 128], w.ap(0, 128, k * 512, [[1, 512]]),
                         k == 0, k == KD - 1, [w, hT], [pt])
                P.act(rgtm.t[:, n, :], pt.t[:, :], AF.Silu, [pt], [rgtm])
            stage(13)
            for n in range(4):
                pa, _ = psum()
                pb, _ = psum()
                for h in range(8):
                    base = (h % 2) * 64
                    kap = B1.ap(base, 64, 2048 + (h // 2) * 512 + n * 128, [[1, 128]])
                    qap = B1.ap(base, 64, (h // 2) * 512 + n * 128, [[1, 128]])
                    pt = pa if h % 2 == 0 else pb
                    P.mm(pt.ap(0, 128, (h // 2) * 128, [[1, 128]]), kap, qap, True, True, [B1], [pt], signal=(h // 2 == 3))
                for par, pt in enumerate((pa, pb)):
                    P.act(sT.ap(0, 128, par * 128, [[256, 4], [1, 128]]), pt.ap(0, 128, 0, [[128, 4], [1, 128]]),
                          AF.Copy, [pt], [sT])
                P.tt(sT.ap(0, 128, 0, [[128, 8], [1, 128]]), sT.ap(0, 128, 0, [[128, 8], [1, 128]]),
                     cb.ap(0, 128, CB_MD, [[0, 8], [1, 128]]), OP.mult, [sT, cb], [sT])
                stage(14)
                po, _ = psum()
                for h in range(8):
                    o_ap = po.ap(0, 128, h * 64, [[1, 64]])
                    P.mm(o_ap, sT.ap(0, 128, h * 128, [[1, 128]]), rvtm.ap(0, 128, n * 512 + h * 64, [[1, 64]]),
                         True, False, [rvtm, sT], [po], signal=False)
                    P.mm(o_ap, B1.ap(0, 128, (h // 2) * 512 + n * 128, [[1, 128]]), st_Rb[l].ap(0, 128, h * 64, [[1, 64]]),
                         False, True, [st_Rb[l], B1], [po], signal=(h == 7))
                stage(15)
                pk0, _ = psum()
                pk1, _ = psum()
                for h in range(8):
                    base = (h % 2) * 64
                    pk = pk0 if h % 2 == 0 else pk1
                    P.mm(pk.ap(base, 64, (h // 2) * 64, [[1, 64]]), ktm.ap(0, 128, n * 512 + h * 64, [[1, 64]]),
                         rvtm.ap(0, 128, n * 512 + h * 64, [[1, 64]]), True, True, [ktm, rvtm], [pk], signal=(h >= 6))
                P.tt(st_R[l].ap(0, 64, 0, [[64, 4], [1, 64]]), st_R[l].ap(0, 64, 0, [[64, 4], [1, 64]]),
                     pk0.ap(0, 64, 0, [[64, 4], [1, 64]]), OP.add, [st_R[l], pk0], [st_R[l]])
                P.tt(st_R[l].ap(64, 64, 0, [[64, 4], [1, 64]]), st_R[l].ap(64, 64, 0, [[64, 4], [1, 64]]),
                     pk1.ap(64, 64, 0, [[64, 4], [1, 64]]), OP.add, [st_R[l], pk1], [st_R[l]])
                P.tt(st_R[l].t[:, :, :], st_R[l].t[:, :, :], cf.ap(0, 128, CF_GC, [[1, 4], [0, 64]]), OP.mult,
                     [st_R[l], cf], [st_R[l]])
                stage(16)
                P.act(tmpA.t[:, :], po.t[:, :], AF.Square, [po], [tmpA])
                P.red(sm.ap(0, 128, 0, [[1, 8]]), tmpA.ap(0, 128, 0, [[64, 8], [1, 64]]), OP.add, [tmpA], [sm])
                P.ts(sm.ap(0, 128, 0, [[1, 8]]), sm.ap(0, 128, 0, [[1, 8]]), 1.0 / 64, OP.mult, [sm], [sm], s2=1e-6, op1=OP.add)
                P.act(sm.ap(0, 128, 0, [[1, 8]]), sm.ap(0, 128, 0, [[1, 8]]), AF.Ln, [sm], [sm])
                P.act(sm.ap(0, 128, 0, [[1, 8]]), sm.ap(0, 128, 0, [[1, 8]]), AF.Exp, [sm], [sm], scale=-0.5)
                P.tt(tmpB.ap(0, 128, 0, [[64, 8], [1, 64]]), po.ap(0, 128, 0, [[64, 8], [1, 64]]),
                     sm.ap(0, 128, 0, [[1, 8], [0, 64]]), OP.mult, [po, sm], [tmpB])
                P.tt(octm.t[:, :], tmpB.t[:, :], rgtm.t[:, n, :], OP.mult, [tmpB, rgtm], [octm])
                _, pto = psum()
                for h in range(8):
                    P.tr(pto.ap(0, 64, h * 128, [[1, 128]]), octm.ap(0, 128, h * 64, [[1, 64]]), cba(CB_IDENT, 128),
                         [octm, cb], [pto], signal=(h == 7))
                P.copy(oT.ap(0, 64, n * 128, [[TB, 8], [1, 128]]), pto.ap(0, 64, 0, [[128, 8], [1, 128]]), [pto], [oT])
                P.copy(st_Rb[l].ap(0, 64, 0, [[128, 4], [1, 64]]), st_R[l].ap(0, 64, 0, [[64, 4], [1, 64]]),
                       [st_R[l]], [st_Rb[l]], eng="act")
                P.copy(st_Rb[l].ap(64, 64, 64, [[128, 4], [1, 64]]), st_R[l].ap(64, 64, 0, [[64, 4], [1, 64]]),
                       [st_R[l]], [st_Rb[l]], eng="act")

        def rwkv(l):
            ZR, ZK, ZV, KK, KA, RR = 0, 2048, 4096, 6144, 8192, 10240

            def shifted(pt, M, j, mucol, out_ap, outT):
                P.copy(rwsb.ap(0, M, 1, [[1, TB]]), pt.ap(0, M, 0, [[1, TB]]), [pt], [rwsb], eng="act")
                P.copy(rwsb.ap(0, M, 0, [[1, 1]]), st_sh[l].ap(0, M, j, [[1, 1]]), [st_sh[l]], [rwsb])
                P.copy(st_sh[l].ap(0, M, j, [[1, 1]]), rwsb.ap(0, M, TB, [[1, 1]]), [rwsb], [st_sh[l]])
                P.tt(tmpA.ap(0, M, 0, [[1, TB]]), rwsb.ap(0, M, 0, [[1, TB]]), rwsb.ap(0, M, 1, [[1, TB]]), OP.subtract,
                     [rwsb], [tmpA])
                P.stt(out_ap, tmpA.ap(0, M, 0, [[1, TB]]), vcol(l, mucol, 0, M), rwsb.ap(0, M, 1, [[1, TB]]),
                      OP.mult, OP.add, [tmpA, vecs, rwsb], [outT])

            for ti, zoff in enumerate((ZR, ZK, ZV)):
                w = wpiece(l, 6 + ti)
                proj_fm(w, 512, lambda m, pt, ti=ti, zoff=zoff: shifted(
                    pt, 128, ti * 4 + m, 16 + ti * 4 + m, BIG.ap(0, 128, zoff + m * 512, [[1, TB]]), BIG))
            for m in range(4):
                zk = BIG.ap(0, 128, ZK + m * 512, [[1, TB]])
                zr = BIG.ap(0, 128, ZR + m * 512, [[1, TB]])
                P.ts(BIG.ap(0, 128, KK + m * 512, [[1, TB]]), zk, vcol(l, 31 + m), OP.mult, [BIG, vecs], [BIG])
                P.ts(BIG.ap(0, 128, KA + m * 512, [[1, TB]]), zk, vcol(l, 35 + m), OP.mult, [BIG, vecs], [BIG])
                P.ts(BIG.ap(0, 128, RR + m * 512, [[1, TB]]), zr, vcol(l, 39 + m), OP.mult, [BIG, vecs], [BIG])
            w = wpiece(l, 9)
            for ji, (c0, M, dst, fn) in enumerate(((0, 64, wdx, AF.Tanh), (64, 64, adx, AF.Copy), (128, 128, gdx, AF.Sigmoid))):
                pt, _ = psum()
                for k in range(KD):
                    P.mm(pt.ap(0, M, 0, [[1, TB]]), w.ap(0, 128, k * 256 + c0, [[1, M]]), hT.t[:, k, :],
                         k == 0, k == KD - 1, [w, hT], [pt])
                shifted(pt, M, 12 + ji, 28 + ji, tmpB.ap(0, M, 0, [[1, TB]]), tmpB)
                for dup in range(2):
                    P.act(dst.ap(0, M, dup * 64, [[128, 8], [1, 64]]), tmpB.ap(0, M, 0, [[64, 8], [1, 64]]), fn, [tmpB], [dst])

            for ci in range(TB // 64):
                t0 = ci * 64
                pa_, _ = psum()
                P.mm(pa_.t[:, :], adx.ap(0, 64, 2 * t0, [[1, 128]]), lwb.ap(0, 64, 512, [[1, 512]]), True, False,
                     [adx, lwb], [pa_], signal=False)
                P.mm(pa_.t[:, :], cba(CB_ONES, 128, 0, 33), HL.ap(0, 33, 512, [[1, 512]]), False, True, [cb, HL], [pa_])
                P.act(a_tm.t[:, :], pa_.t[:, :], AF.Sigmoid, [pa_], [a_tm])
                pw_, _ = psum()
                P.mm(pw_.t[:, :], wdx.ap(0, 64, 2 * t0, [[1, 128]]), lwb.ap(0, 64, 0, [[1, 512]]), True, False,
                     [wdx, lwb], [pw_], signal=False)
                P.mm(pw_.t[:, :], cba(CB_ONES, 128, 0, 33), HL.ap(0, 33, 0, [[1, 512]]), False, True, [cb, HL], [pw_])
                P.act(sig.t[:, :], pw_.t[:, :], AF.Sigmoid, [pw_], [sig])
                pg_, _ = psum()
                P.mm(pg_.t[:, :], gdx.ap(0, 128, 2 * t0, [[1, 128]]), lwb.ap(0, 128, 1024, [[1, 512]]), True, True,
                     [gdx, lwb], [pg_])
                P.copy(g_tm.t[:, :], pg_.t[:, :], [pg_], [g_tm], eng="act")
                pc1, _ = psum()
                P.mm(pc1.t[:, :], cf.ap(0, 64, CF_TRI, [[1, 128]]), sig.ap(0, 64, 0, [[1, 512]]), True, True, [cf, sig], [pc1])
                P.act(E3.t[:, :], pc1.t[:, :], AF.Exp, [pc1], [E3])
                pc3, _ = psum()
                P.mm(pc3.t[:, :], cf.ap(0, 64, CF_TRI + 128, [[1, 128]]), sig.ap(0, 64, 0, [[1, 512]]), True, True,
                     [cf, sig], [pc3])
                P.act(E2.t[:, :], pc3.t[:, :], AF.Exp, [pc3], [E2])
                pgc, _ = psum()
                for h in range(8):
                    P.mm(pgc.ap(0, 64, h * 2, [[1, 2]]), sig.ap(0, 64, h * 64, [[1, 64]]), cf.ap(0, 64, CF_ONES, [[1, 2]]),
                         True, True, [sig, cf], [pgc], signal=(h == 7))
                P.act(sm.ap(0, 64, 16, [[1, 8]]), pgc.ap(0, 64, 0, [[2, 8]]), AF.Exp, [pgc], [sm])
                P.act(sm.ap(0, 64, 24, [[1, 8]]), pgc.ap(0, 64, 0, [[2, 8]]), AF.Exp, [pgc], [sm], scale=-1.0)
                tps = {}
                shared = None
                for name, off in (("kk", KK), ("k", ZK), ("ka", KA), ("r", ZR), ("rr", RR), ("v", ZV)):
                    if name == "k":
                        ptb = shared
                    else:
                        _, ptb = psum()
                    if name == "kk":
                        shared = ptb
                    p0 = 0 if name == "kk" else 64
                    for m in range(4):
                        P.tr(ptb.ap(p0, 64, m * 128, [[1, 128]]), BIG.ap(0, 128, off + m * 512 + t0, [[1, 64]]),
                             cba(CB_IDENT, 128), [BIG, cb], [ptb], signal=(m == 3))
                    tps[name] = ptb
                kkp = tps["kk"]
                P.act(tmpA.ap(0, 64, 0, [[1, 512]]), kkp.ap(0, 64, 0, [[1, 512]]), AF.Square, [kkp], [tmpA])
                P.red(sm.ap(0, 64, 0, [[1, 8]]), tmpA.ap(0, 64, 0, [[64, 8], [1, 64]]), OP.add, [tmpA], [sm])
                P.act(sm.ap(0, 64, 8, [[1, 8]]), sm.ap(0, 64, 0, [[1, 8]]), AF.Sqrt, [sm], [sm])
                P.ts(sm.ap(0, 64, 8, [[1, 8]]), sm.ap(0, 64, 8, [[1, 8]]), 1e-12, OP.max, [sm], [sm])
                P.recip(sm.ap(0, 64, 8, [[1, 8]]), sm.ap(0, 64, 8, [[1, 8]]), [sm], [sm])
                P.stt(nk.ap(0, 64, 0, [[64, 8], [1, 64]]), kkp.ap(0, 64, 0, [[64, 8], [1, 64]]), -1.0,
                      sm.ap(0, 64, 8, [[1, 8], [0, 64]]), OP.mult, OP.mult, [kkp, sm], [nk])
                P.tt(X3.ap(0, 64, 0, [[1, 512]]), nk.ap(0, 64, 0, [[1, 512]]), E3.ap(0, 64, 0, [[1, 512]]), OP.mult,
                     [nk, E3], [X3])
                P.stt(Q1.ap(0, 64, 0, [[1, 512]]), nk.ap(0, 64, 0, [[1, 512]]), -1.0, a_tm.ap(0, 64, 0, [[1, 512]]),
                      OP.mult, OP.mult, [nk, a_tm], [Q1])
                P.stt(tmpB.ap(64, 64, 0, [[1, 512]]), a_tm.ap(64, 64, 0, [[1, 512]]), 1.0, tps["ka"].ap(64, 64, 0, [[1, 512]]),
                      OP.subtract, OP.mult, [a_tm, tps["ka"]], [tmpB])
                P.tt(Q1.ap(64, 64, 0, [[1, 512]]), tmpB.ap(64, 64, 0, [[1, 512]]), tps["k"].ap(64, 64, 0, [[1, 512]]), OP.add,
                     [tmpB, tps["k"]], [Q1])
                P.tt(X3.ap(64, 64, 0, [[1, 512]]), tps["r"].ap(64, 64, 0, [[1, 512]]), E3.ap(64, 64, 0, [[1, 512]]), OP.mult,
                     [tps["r"], E3], [X3])
                P.copy(UV.ap(64, 64, 0, [[1, 512]]), tps["v"].ap(64, 64, 0, [[1, 512]]), [tps["v"]], [UV], eng="act")
                P.tt(tmpB.ap(64, 64, 0, [[1, 512]]), tps["rr"].ap(64, 64, 0, [[1, 512]]), Q1.ap(64, 64, 0, [[1, 512]]), OP.mult,
                     [tps["rr"], Q1], [tmpB])
                P.red(sm.ap(64, 64, 32, [[1, 8]]), tmpB.ap(64, 64, 0, [[64, 8], [1, 64]]), OP.add, [tmpB], [sm])
                P.tt(X2.t[:, :], Q1.t[:, :], E2.t[:, :], OP.mult, [Q1, E2], [X2])
                _, pt3 = psum()
                for h in range(8):
                    P.tr(pt3.ap(0, 64, h * 128, [[1, 128]]), X3.ap(0, 128, h * 64, [[1, 64]]), cba(CB_IDENT, 128),
                         [X3, cb], [pt3], signal=(h == 7))
                P.copy(X3T.ap(0, 64, 0, [[1, 1024]]), pt3.ap(0, 64, 0, [[1, 1024]]), [pt3], [X3T], eng="act")
                _, pt2 = psum()
                for h in range(8):
                    P.tr(pt2.ap(0, 64, h * 128, [[1, 128]]), X2.ap(0, 128, h * 64, [[1, 64]]), cba(CB_IDENT, 128),
                         [X2, cb], [pt2], signal=(h == 7))
                P.tt(X1T.ap(0, 64, 0, [[128, 8], [1, 128]]), pt2.ap(0, 64, 0, [[128, 8], [1, 128]]),
                     sm.ap(0, 64, 24, [[1, 8], [0, 128]]), OP.mult, [pt2, sm], [X1T])
                pS = [psum()[0], psum()[0]]
                for h in range(8):
                    pt = pS[h // 4]
                    P.mm(pt.ap(0, 128, (h % 4) * 128, [[1, 128]]), X1T.ap(0, 64, h * 128, [[1, 128]]),
                         X3T.ap(0, 64, h * 128, [[1, 128]]), True, True, [X1T, X3T], [pt], signal=(h % 4 == 3))
                for half in range(2):
                    P.tt(S_sb.ap(0, 128, half * 512, [[128, 4], [1, 128]]), pS[half].ap(0, 128, 0, [[128, 4], [1, 128]]),
                         cb.ap(0, 128, CB_M4, [[0, 4], [1, 128]]), OP.mult, [pS[half], cb], [S_sb])
                pA, _ = psum()
                for h in range(8):
                    P.mm(pA.ap(0, 64, h * 64, [[1, 64]]), X3T.ap(0, 64, h * 128, [[1, 64]]), X1T.ap(0, 64, h * 128, [[1, 64]]),
                         True, True, [X3T, X1T], [pA], signal=(h == 7))
                P.tt(Apow[0].ap(0, 64, 0, [[64, 8], [1, 64]]), pA.ap(0, 64, 0, [[64, 8], [1, 64]]),
                     cb.ap(0, 64, CB_MST, [[0, 8], [1, 64]]), OP.mult, [pA, cb], [Apow[0]])
                pX, _ = psum()
                pX2, _ = psum()
                for h in range(8):
                    P.mm(pX.ap(0, 64, h * 64, [[1, 64]]), X3T.ap(0, 64, h * 128, [[1, 64]]),
                         st_Hb[l].ap(0, 64, h * 64, [[1, 64]]), True, True, [X3T, st_Hb[l]], [pX], signal=(h == 7))
                for h in range(8):
                    P.mm(pX2.ap(0, 64, h * 64, [[1, 64]]), S_sb.ap(64, 64, h * 128, [[1, 64]]),
                         UV.ap(64, 64, h * 64, [[1, 64]]), True, True, [S_sb, UV], [pX2], signal=(h == 7))
                P.copy(Xf.ap(0, 64, 0, [[1, 512]]), pX.ap(0, 64, 0, [[1, 512]]), [pX], [Xf], eng="act")
                P.tt(Xb.ap(0, 64, 0, [[1, 512]]), Xf.ap(0, 64, 0, [[1, 512]]), pX2.ap(0, 64, 0, [[1, 512]]), OP.add,
                     [Xf, pX2], [Xb])
                P.tt(Xf.ap(0, 64, 0, [[1, 512]]), Xf.ap(0, 64, 0, [[1, 512]]), pX2.ap(0, 64, 0, [[1, 512]]), OP.add,
                     [Xf, pX2], [Xf])
                for i in range(6):
                    if i == 0:
                        def Nap(h, c0=0, w=64):
                            return S_sb.ap(0, 64, h * 128 + c0, [[1, w]])
                        Nt = S_sb
                    else:
                        def Nap(h, c0=0, w=64, i=i):
                            return Npow[i % 2].ap(0, 64, h * 64 + c0, [[1, w]])
                        Nt = Npow[i % 2]
                    At = Apow[i % 2]
                    pY, _ = psum()
                    for h in range(8):
                        P.mm(pY.ap(0, 64, h * 64, [[1, 64]]), Nap(h), Xb.ap(0, 64, h * 64, [[1, 64]]), True, True,
                             [Nt, Xb], [pY], signal=(h == 7))
                    if i < 5:
                        P.tt(Xb.ap(0, 64, 0, [[1, 512]]), Xf.ap(0, 64, 0, [[1, 512]]), pY.ap(0, 64, 0, [[1, 512]]), OP.add,
                             [Xf, pY], [Xb])
                        P.tt(Xf.ap(0, 64, 0, [[1, 512]]), Xf.ap(0, 64, 0, [[1, 512]]), pY.ap(0, 64, 0, [[1, 512]]), OP.add,
                             [Xf, pY], [Xf])
                        pN, _ = psum()
                        pA2, _ = psum()
                        for h in range(8):
                            P.mm(pN.ap(0, 64, h * 64, [[1, 64]]), At.ap(0, 64, h * 64, [[1, 64]]), Nap(h), True, True,
                                 [At, Nt], [pN], signal=(h == 7))
                        for h in range(8):
                            P.mm(pA2.ap(0, 64, h * 64, [[1, 64]]), Nap(h), At.ap(0, 64, h * 64, [[1, 64]]), True, True,
                                 [At, Nt], [pA2], signal=(h == 7))
                        P.copy(Npow[(i + 1) % 2].ap(0, 64, 0, [[1, 512]]), pN.ap(0, 64, 0, [[1, 512]]), [pN],
                               [Npow[(i + 1) % 2]], eng="act")
                        P.copy(Apow[(i + 1) % 2].ap(0, 64, 0, [[1, 512]]), pA2.ap(0, 64, 0, [[1, 512]]), [pA2],
                               [Apow[(i + 1) % 2]])
                    else:
                        P.tt(UV.ap(0, 64, 0, [[1, 512]]), Xf.ap(0, 64, 0, [[1, 512]]), pY.ap(0, 64, 0, [[1, 512]]), OP.add,
                             [Xf, pY], [UV])
                pYo, _ = psum()
                for h in range(8):
                    o_ap = pYo.ap(64, 64, h * 64, [[1, 64]])
                    P.mm(o_ap, X3T.ap(0, 64, h * 128 + 64, [[1, 64]]), st_Hb[l].ap(0, 64, h * 64, [[1, 64]]), True, False,
                         [X3T, st_Hb[l]], [pYo], signal=False)
                    P.mm(o_ap, S_sb.ap(0, 128, h * 128 + 64, [[1, 64]]), UV.ap(0, 128, h * 64, [[1, 64]]), False, True,
                         [S_sb, UV], [pYo], signal=(h == 7))
                pH, _ = psum()
                for h in range(8):
                    P.mm(pH.ap(0, 64, h * 64, [[1, 64]]), X2.ap(0, 128, h * 64, [[1, 64]]), UV.ap(0, 128, h * 64, [[1, 64]]),
                         True, True, [X2, UV], [pH], signal=(h == 7))
                P.tt(st_H[l].ap(0, 64, 0, [[64, 8], [1, 64]]), st_H[l].ap(0, 64, 0, [[64, 8], [1, 64]]),
                     sm.ap(0, 64, 16, [[1, 8], [0, 64]]), OP.mult, [st_H[l], sm], [st_H[l]])
                P.tt(st_H[l].ap(0, 64, 0, [[1, 512]]), st_H[l].ap(0, 64, 0, [[1, 512]]), pH.ap(0, 64, 0, [[1, 512]]), OP.add,
                     [st_H[l], pH], [st_H[l]])
                P.copy(st_Hb[l].ap(0, 64, 0, [[1, 512]]), st_H[l].ap(0, 64, 0, [[1, 512]]), [st_H[l]], [st_Hb[l]], eng="act")
                R64 = (64, 64)
                P.copy(Ysb.ap(64, 64, 0, [[1, 512]]), pYo.ap(64, 64, 0, [[1, 512]]), [pYo], [Ysb], eng="act")
                P.red(sm.ap(64, 64, 40, [[1, 8]]), Ysb.ap(64, 64, 0, [[64, 8], [1, 64]]), OP.add, [Ysb], [sm])
                P.act(tmpA.ap(64, 64, 0, [[1, 512]]), Ysb.ap(64, 64, 0, [[1, 512]]), AF.Square, [Ysb], [tmpA])
                P.red(sm.ap(64, 64, 48, [[1, 8]]), tmpA.ap(64, 64, 0, [[64, 8], [1, 64]]), OP.add, [tmpA], [sm])
                P.ts(sm.ap(64, 64, 40, [[1, 8]]), sm.ap(64, 64, 40, [[1, 8]]), 1.0 / 64, OP.mult, [sm], [sm])
                P.tt(sm.ap(64, 64, 56, [[1, 8]]), sm.ap(64, 64, 40, [[1, 8]]), sm.ap(64, 64, 40, [[1, 8]]), OP.mult, [sm], [sm])
                P.stt(sm.ap(64, 64, 48, [[1, 8]]), sm.ap(64, 64, 48, [[1, 8]]), 1.0 / 64, sm.ap(64, 64, 56, [[1, 8]]),
                      OP.mult, OP.subtract, [sm], [sm])
                P.ts(sm.ap(64, 64, 48, [[1, 8]]), sm.ap(64, 64, 48, [[1, 8]]), 64e-5, OP.add, [sm], [sm])
                P.act(sm.ap(64, 64, 48, [[1, 8]]), sm.ap(64, 64, 48, [[1, 8]]), AF.Sqrt, [sm], [sm])
                P.recip(sm.ap(64, 64, 48, [[1, 8]]), sm.ap(64, 64, 48, [[1, 8]]), [sm], [sm])
                Y3 = Ysb.ap(64, 64, 0, [[64, 8], [1, 64]])
                P.tt(Y3, Y3, sm.ap(64, 64, 40, [[1, 8], [0, 64]]), OP.subtract, [Ysb, sm], [Ysb])
                P.tt(Y3, Y3, sm.ap(64, 64, 48, [[1, 8], [0, 64]]), OP.mult, [Ysb, sm], [Ysb])
                Y2 = Ysb.ap(64, 64, 0, [[1, 512]])
                P.tt(Y2, Y2, rowf.ap(64, 64, 0, [[1, 512]]), OP.mult, [Ysb, rowf], [Ysb])
                P.tt(Y2, Y2, rowf.ap(64, 64, 512, [[1, 512]]), OP.add, [Ysb, rowf], [Ysb])
                P.tt(tmpA.ap(64, 64, 0, [[64, 8], [1, 64]]), UV.ap(64, 64, 0, [[64, 8], [1, 64]]),
                     sm.ap(64, 64, 32, [[1, 8], [0, 64]]), OP.mult, [UV, sm], [tmpA])
                P.tt(Y2, Y2, tmpA.ap(64, 64, 0, [[1, 512]]), OP.add, [Ysb, tmpA], [Ysb])
                P.tt(obt.ap(64, 64, 0, [[1, 512]]), Y2, g_tm.ap(64, 64, 0, [[1, 512]]), OP.mult, [Ysb, g_tm], [obt])
                _, pto = psum()
                for h in range(8):
                    P.tr(pto.ap(0, 64, h * 64, [[1, 64]]), obt.ap(64, 64, h * 64, [[1, 64]]),
                         cb.ap(64, 64, CB_IDENT + 64, [[1, 64]]), [obt, cb], [pto], signal=(h == 7))
                P.copy(oT.ap(0, 64, t0, [[TB, 8], [1, 64]]), pto.ap(0, 64, 0, [[64, 8], [1, 64]]), [pto], [oT])

        for s in range(NSEQ):
            for tb in range(NTB):
                c0 = s * T + tb * TB
                first = (tb == 0)
                tbi[0] = tb
                for hh in range(2):
                    P.dma("pool", rotc.ap(0, 128, hh * TB, [[1, TB]]),
                          bass.AP(rot_d.tensor, hh * T + tb * TB, [[2 * T, 128], [1, TB]]), rotc)
                P.dma("sp", xT.t[:, :, :], bass.AP(xT_d.tensor, c0, [[NT, 128], [128 * NT, KD], [1, TB]]), xT)
                for l in range(L):
                    if first:
                        for stt_ in (st_R[l], st_Rb[l], st_H[l], st_Hb[l], st_sh[l]):
                            P.memset(stt_.ap(0, stt_.shape[0], 0, [[1, stt_.rs]]), 0.0, [stt_])
                    P.dma("sp", rowf.t[:, :], bass.AP(rows_d.tensor, l * 2048 + 1024, [[0, 128], [1, 1024]]), rowf)
                    P.dma("pool", lwb.t[:, :], lw_d[:, l * 1536:(l + 1) * 1536], lwb)
                    P.dma("sp", rv33.t[0:33, :], bass.AP(rows_d.tensor, l * 2048, [[0, 33], [1, 1024]]), rv33)
                    P.copy(rhi.t[0:33, :], rv33.t[0:33, :], [rv33], [rhi])
                    P.tt(rv33.t[0:33, :], rv33.t[0:33, :], rhi.t[0:33, :], OP.subtract, [rv33, rhi], [rv33])
                    P.copy(HL.t[0:1, :], rhi.t[0:1, :], [rhi], [HL])
                    P.copy(HL.t[32:33, :], rv33.t[32:33, :], [rv33], [HL])
                    rmsnorm(l, 0)
                    import os as _os
                    SK = _os.environ.get("KSKIP", "")
                    if "a" not in SK:
                        attention(l, first)
                        merge_branch(l, 0)
                    else:
                        P.memset(mixed.ap(0, 128, 0, [[1, KD * TB]]), 0.0, [mixed])
                        if "m" in SK:
                            P.memset(oT.ap(0, 64, 0, [[1, 8 * TB]]), 0.0, [oT])
                            merge_branch(l, 0)
                    if "c" not in SK:
                        retention(l)
                        merge_branch(l, 2)
                    if "b" not in SK:
                        rwkv(l)
                        merge_branch(l, 1)
                    for hf in range(2):
                        w = wpiece(l, 22 + hf)

                        def resid(m, pt, hf=hf):
                            mi = hf * 4 + m
                            P.tt(xT.t[:, mi, :], xT.t[:, mi, :], pt.t[:, :], OP.add, [xT, pt], [xT])
                        proj_fm(w, 512, resid, src=mixed)
                    rmsnorm(l, 8)
                    actT = BIG
                    for i in range(6):
                        ncol = 512 if i < 5 else 256
                        wg = wpiece(l, 24 + 2 * i)
                        wu = wpiece(l, 25 + 2 * i)
                        for m in range(ncol // 128):
                            pg, _ = psum()
                            pu, _ = psum()
                            for k in range(KD):
                                P.mm(pg.t[:, :], wg.ap(0, 128, k * ncol + m * 128, [[1, 128]]), hT.t[:, k, :],
                                     k == 0, k == KD - 1, [wg, hT], [pg])
                            for k in range(KD):
                                P.mm(pu.t[:, :], wu.ap(0, 128, k * ncol + m * 128, [[1, 128]]), hT.t[:, k, :],
                                     k == 0, k == KD - 1, [wu, hT], [pu])
                            P.act(tmpA.t[:, :], pg.t[:, :], AF.Silu, [pg], [tmpA])
                            fi = i * 4 + m
                            P.tt(actT.ap(0, 128, fi * 512, [[1, 512]]), pu.t[:, :], tmpA.t[:, :], OP.mult, [pu, tmpA], [actT])
                    for m in range(8):
                        w = wpiece(l, 36 + m)
                        pt, _ = psum()
                        for k in range(KF):
                            P.mm(pt.t[:, :], w.ap(0, 128, k * 128, [[1, 128]]), actT.ap(0, 128, k * 512, [[1, 512]]),
                                 k == 0, k == KF - 1, [w, actT], [pt])
                        P.tt(xT.t[:, m, :], xT.t[:, m, :], pt.t[:, :], OP.add, [xT, pt], [xT])
                P.dma("sp", bass.AP(yT_d.tensor, c0, [[NT, 128], [128 * NT, KD], [1, TB]]), xT.t[:, :, :], xT, load=False)
        with nc.Block() as block:
            P.finish(block, [xT, oT])
        P.stats = {e: len(P.ins[e]) for e in P.ENGS}
        nc._prog_stats = (P.stats, P.nwaits, P.ndsem)
        nc._tags = P.tags
        nc._P = P
    return nc


def prep_inputs(inp, x_cores, T, L):
    consts = make_consts(T)
    wpk = np.stack([pack_layer(inp, l) for l in range(L)])
    vecs = pack_vecs(inp, L).reshape(128, L * NVEC)
    lw, rows, sinks = pack_small(inp, L)
    shared = {"wpk": wpk, "vecs": np.ascontiguousarray(vecs), "lw": np.ascontiguousarray(lw.reshape(128, L * 1536)),
              "rows": np.ascontiguousarray(rows.reshape(L, 2048)), "sinks": np.ascontiguousarray(sinks.reshape(1, L * 8)),
              "cf": consts["cf"], "cb": consts["cb"], "rot": consts["rot"]}
    maps = []
    for xc in x_cores:
        xT = np.ascontiguousarray(xc.reshape(-1, D).T)
        m = dict(shared)
        m["xT"] = xT
        maps.append(m)
    return maps


def kernel(**inputs):
    inp = {k: np.asarray(v, dtype=np.float32) for k, v in inputs.items()}
    x = inp["x"]
    B, T, _ = x.shape
    L = inp["w_in"].shape[0]
    ncores = 8
    nseq = B // ncores
    nc = build(nseq, T, L)
    maps = prep_inputs(inp, [x[c * nseq:(c + 1) * nseq] for c in range(ncores)], T, L)
    res = run_bass_kernel_spmd(nc, maps, core_ids=list(range(ncores)))
    out = np.empty((B, T, D), np.float32)
    for c in range(ncores):
        yT = np.asarray(res.results[c]["yT"])
        out[c * nseq:(c + 1) * nseq] = yT.T.reshape(nseq, T, D)
    return out
```

```python
from contextlib import ExitStack
import numpy as np
import concourse.bass as bass
import concourse.mybir as mybir
from concourse.bass_utils import run_bass_kernel_spmd

F32 = mybir.dt.float32
BF16 = mybir.dt.bfloat16
AF = mybir.ActivationFunctionType
OP = mybir.AluOpType
AX = mybir.AxisListType

D = 1024
KD = 8
TB = 512
FF = 2816
KF = 22
NPIECE = 44
PW = 4096
NVEC = 48
LOG_DECAY_C = -float(np.exp(-0.5))


class Buf:
    def __init__(self, name):
        self.name = name
        self.w = None
        self.r = {}
        self.dsem = None
        self.dcnt = 0


class Tl:
    def __init__(self, t, shape, buf=None, name=""):
        self.t = t
        self.shape = list(shape)
        self.rs = int(np.prod(shape[1:]))
        self.buf = buf or Buf(name)

    def ap(self, p0, npart, off, dims):
        return bass.AP(self.t, p0 * self.rs + off, [[self.rs, npart]] + [list(d) for d in dims])

    def __getitem__(self, idx):
        return self.t[idx]


class Prog:
    ENGS = ["pe", "dve", "act", "pool", "sp"]

    def __init__(self, nc, es):
        self.nc = nc
        self.es = es
        self.sem = {e: es.enter_context(nc.semaphore("s_" + e)) for e in self.ENGS}
        self.cnt = {e: 0 for e in self.ENGS}
        self.ins = {e: [] for e in self.ENGS}
        self.seen = {e: {} for e in self.ENGS}
        self.semobj = dict(self.sem)
        self.ndsem = 0
        self.nwaits = 0
        self.tags = {}

    def _waits(self, eng, deps):
        need = {}
        for d in deps:
            if d is None:
                continue
            k, v = d
            if k == eng and eng == "pe":
                continue
            if v > need.get(k, 0):
                need[k] = v
        out = []
        for k, v in need.items():
            if self.seen[eng].get(k, 0) >= v:
                continue
            self.seen[eng][k] = v
            out.append((self.semobj[k], v))
        self.nwaits += len(out)
        return out

    def op(self, eng, fn, reads=(), writes=(), signal=True):
        deps = []
        for b in reads:
            deps.append(b.buf.w)
        for b in writes:
            deps.append(b.buf.w)
            deps.extend(b.buf.r.items())
        waits = self._waits(eng, deps)
        val = self.cnt[eng] + 1
        if signal:
            self.cnt[eng] = val
        import sys as _sys
        fr = _sys._getframe(1)
        while fr.f_code.co_name in ("op", "mm", "tr", "act", "tt", "ts", "stt", "copy", "red", "memset", "recip", "<lambda>"):
            fr = fr.f_back
        tag = "%s:%d" % (fr.f_code.co_name, fr.f_lineno)
        self.ins[eng].append((waits, fn, (self.sem[eng], 1) if signal else None, tag))
        for b in reads:
            if b.buf.r.get(eng, 0) < val:
                b.buf.r[eng] = val
        for b in writes:
            b.buf.w = (eng, val)
            b.buf.r = {}

    def _dsem(self, buf):
        if buf.dsem is None:
            buf.dsem = "d%d" % self.ndsem
            self.ndsem += 1
            self.semobj[buf.dsem] = self.es.enter_context(self.nc.semaphore(buf.dsem))
        return buf.dsem

    def dma(self, q, out, in_, tile, load=True):
        b = tile.buf
        k = self._dsem(b)
        deps = [b.w]
        if load:
            deps.extend(b.r.items())
        waits = self._waits(q, deps)
        b.dcnt += 16
        self.ins[q].append((waits, lambda e: e.dma_start(out=out, in_=in_), (self.semobj[k], 16), "dma"))
        if load:
            b.w = (k, b.dcnt)
            b.r = {}
        else:
            b.r[k] = b.dcnt

    def inherit(self, dsts, srcs):
        acc = {}
        for s_ in srcs:
            items = list(s_.buf.r.items())
            if s_.buf.w is not None:
                items.append(s_.buf.w)
            for k, v in items:
                if v > acc.get(k, 0):
                    acc[k] = v
        for d_ in dsts:
            d_.buf.w = None
            d_.buf.r = dict(acc)

    def finish(self, block, final_bufs):
        waits = []
        for b in final_bufs:
            if b.buf.dsem is not None:
                waits.append((self.semobj[b.buf.dsem], b.buf.dcnt))
        self.ins["sp"].append((waits, None, None, "end"))
        engmap = {"pe": block.tensor, "dve": block.vector, "act": block.scalar,
                  "pool": block.gpsimd, "sp": block.sync}
        for e in self.ENGS:
            lst = self.ins[e]

            def body(eng, lst=lst):
                for waits, fn, inc, tag in lst:
                    for s, v in waits:
                        eng.wait_ge(s, v)
                    if fn is None:
                        continue
                    i = fn(eng)
                    try:
                        self.tags[str(i.ins.name)] = tag
                    except Exception:
                        pass
                    if inc is not None:
                        i.then_inc(inc[0], inc[1])
            engmap[e](body)

    def mm(self, out, lhsT, rhs, start, stop, reads, writes, signal=None):
        if signal is None:
            signal = stop
        self.op("pe", lambda e: e.matmul(out, lhsT=lhsT, rhs=rhs, start=start, stop=stop),
                reads, writes, signal)

    def tr(self, out, in_, ident, reads, writes, signal=True):
        self.op("pe", lambda e: e.transpose(out, in_, ident), reads, writes, signal)

    def act(self, out, in_, func, reads, writes, scale=1.0, bias=None):
        if bias is None:
            self.op("act", lambda e: e.activation(out=out, in_=in_, func=func, scale=scale), reads, writes)
        else:
            self.op("act", lambda e: e.activation(out=out, in_=in_, func=func, scale=scale, bias=bias),
                    reads, writes)

    def tt(self, out, in0, in1, op, reads, writes, eng="dve"):
        self.op(eng, lambda e: e.tensor_tensor(out=out, in0=in0, in1=in1, op=op), reads, writes)

    def ts(self, out, in0, s1, op0, reads, writes, s2=None, op1=None, eng="dve"):
        if op1 is None:
            self.op(eng, lambda e: e.tensor_scalar(out=out, in0=in0, scalar1=s1, scalar2=None, op0=op0),
                    reads, writes)
        else:
            self.op(eng, lambda e: e.tensor_scalar(out=out, in0=in0, scalar1=s1, scalar2=s2, op0=op0, op1=op1),
                    reads, writes)

    def stt(self, out, in0, scalar, in1, op0, op1, reads, writes):
        self.op("dve", lambda e: e.scalar_tensor_tensor(out=out, in0=in0, scalar=scalar, in1=in1,
                                                        op0=op0, op1=op1), reads, writes)

    def copy(self, out, in_, reads, writes, eng="dve"):
        if eng == "act":
            self.op("act", lambda e: e.copy(out=out, in_=in_), reads, writes)
        else:
            self.op(eng, lambda e: e.tensor_copy(out=out, in_=in_), reads, writes)

    def red(self, out, in_, op, reads, writes):
        self.op("dve", lambda e: e.tensor_reduce(out=out, in_=in_, op=op, axis=AX.X), reads, writes)

    def memset(self, ap, val, writes, eng="dve"):
        self.op(eng, lambda e: e.memset(ap, val), [], writes)

    def recip(self, out, in_, reads, writes):
        self.op("dve", lambda e: e.reciprocal(out=out, in_=in_), reads, writes)


def _bf(a):
    import ml_dtypes
    return np.asarray(a, dtype=np.float32).astype(ml_dtypes.bfloat16)


def make_consts(T):
    c = {}
    idx = np.arange(128)
    ident = np.eye(128, dtype=np.float32)
    s = np.arange(64)[:, None]
    t = np.arange(64)[None, :]
    tri1 = np.concatenate([(s < t), (s <= t)], axis=1).astype(np.float32)
    tri3 = np.concatenate([(s > t), (s > t)], axis=1).astype(np.float32)
    tri = np.zeros((128, 256), np.float32)
    tri[:64, :128] = tri1 * LOG_DECAY_C
    tri[:64, 128:] = tri3 * LOG_DECAY_C
    half = 32
    inv_freq = 1.0 / (10000.0 ** (np.arange(half, dtype=np.float32) * 2.0 / 64))
    pos = np.arange(T, dtype=np.float32)
    ang = pos[None, :] * inv_freq[:, None]
    cosT = np.cos(ang)[idx % 32]
    sinT = np.sin(ang)[idx % 32] * np.where((idx % 64) < 32, -1.0, 1.0)[:, None]
    H = 8
    log_gamma = np.log1p(-np.power(2.0, -5.0 - np.arange(H, dtype=np.float64)))
    i = np.arange(128, dtype=np.float64)
    xi = np.exp(log_gamma[:, None] * (i[None, :] + 1.0))
    kf = (64 ** -0.5) * np.exp(-log_gamma[:, None] * (i[None, :] + 1.0))
    gc = np.exp(log_gamma * 128.0)
    XI = np.zeros((128, 4, 128), np.float32)
    KFt = np.zeros((128, 4, 128), np.float32)
    GC = np.zeros((128, 4), np.float32)
    for h in range(H):
        rows = slice((h % 2) * 64, (h % 2) * 64 + 64)
        XI[rows, h // 2, :] = xi[h][None, :]
        KFt[rows, h // 2, :] = kf[h][None, :]
        GC[rows, h // 2] = gc[h]
    ones = np.full((128, 64), LOG_DECAY_C, np.float32)
    c["cf"] = np.concatenate([ident, tri, XI.reshape(128, -1), KFt.reshape(128, -1), GC, ones], axis=1)
    c["rot"] = np.concatenate([cosT, sinT], axis=1).astype(np.float32)
    identb = np.eye(128, dtype=np.float32)
    blk64 = (idx[:, None] // 64 == idx[None, :] // 64).astype(np.float32) / 64.0
    onesD = np.full((128, 128), 1.0 / 1024.0, np.float32)
    mdiag = (idx[:, None] <= idx[None, :]).astype(np.float32)
    mprev = (idx[:, None] > idx[None, :]).astype(np.float32)
    perm = np.zeros((128, 128), np.float32)
    for p in range(128):
        q = p + 32 if (p % 64) < 32 else p - 32
        perm[q, p] = 1.0
    m4 = np.zeros((128, 128), np.float32)
    ss = np.arange(64)[:, None]
    tt = np.arange(64)[None, :]
    for w in range(2):
        m4[w * 64:(w + 1) * 64, 0:64] = (ss < tt)
        m4[w * 64:(w + 1) * 64, 64:128] = (ss <= tt)
    mst = np.zeros((128, 64), np.float32)
    mst[:64] = (np.arange(64)[:, None] > np.arange(64)[None, :])
    ones128 = np.ones((128, 128), np.float32)
    c["cb"] = _bf(np.concatenate([identb, blk64, onesD, mdiag, mprev, perm, m4, mst, ones128], axis=1))
    return c


CF_IDENT, CF_TRI, CF_XI, CF_KF, CF_GC, CF_ONES = 0, 128, 384, 896, 1408, 1412
CF_W = 1412 + 64
CB_IDENT, CB_BLK, CB_OND, CB_MD, CB_MP, CB_PERM, CB_M4, CB_MST, CB_ONES = 0, 128, 256, 384, 512, 640, 768, 896, 960
CB_W = 960 + 128


def _fm(W):
    K, N = W.shape
    kc = K // 128
    out = np.zeros((128, PW), np.float32)
    out[:, :kc * N] = W.reshape(kc, 128, N).transpose(1, 0, 2).reshape(128, kc * N)
    return out


def _hm(W, c0):
    out = np.zeros((128, PW), np.float32)
    out[:64] = W[:, c0:c0 + 512].reshape(8, 64, 512).transpose(1, 0, 2).reshape(64, 4096)
    return out


def pack_layer(inp, l):
    w_in = inp["w_in"][l]
    P = []
    P.append(_fm(w_in[:, 0:512]))
    akv = np.concatenate([w_in[:, 512:576], w_in[:, 512:576], w_in[:, 576:640], w_in[:, 576:640],
                          w_in[:, 640:768]], axis=1)
    P.append(_fm(akv))
    P.append(_fm(w_in[:, 2560:3072]))
    P.append(_fm(w_in[:, 3072:3584]))
    P.append(_fm(w_in[:, 3584:4096]))
    P.append(_fm(w_in[:, 4096:4608]))
    P.append(_fm(w_in[:, 768:1280]))
    P.append(_fm(w_in[:, 1280:1792]))
    P.append(_fm(w_in[:, 1792:2304]))
    P.append(_fm(w_in[:, 2304:2560]))
    for b, wo in enumerate([inp["w_attn_o"][l], inp["w_rwkv_o"][l], inp["w_ret_o"][l]]):
        for hf in range(2):
            P.append(_hm(wo, hf * 512))
            c0 = 4608 + b * 1024 + hf * 512
            P.append(_fm(w_in[:, c0:c0 + 512]))
    for hf in range(2):
        P.append(_fm(inp["w_out"][l][:, hf * 512:(hf + 1) * 512]))
    for i in range(6):
        c0 = i * 512
        c1 = min(c0 + 512, FF)
        P.append(_fm(inp["w_ffn_gate"][l][:, c0:c1]))
        P.append(_fm(inp["w_ffn_up"][l][:, c0:c1]))
    wd = inp["w_ffn_down"][l]
    for m in range(8):
        P.append(_fm(wd[:, m * 128:(m + 1) * 128]))
    assert len(P) == NPIECE
    return np.stack(P)


def pack_vecs(inp, L):
    v = np.zeros((128, L, NVEC), np.float32)
    idx = np.arange(128)

    def fm(a):
        return a.reshape(-1, 128).T

    for l in range(L):
        v[:, l, 0:8] = fm(inp["norm1_g"][l])
        v[:, l, 8:16] = fm(inp["norm2_g"][l])
        mu = inp["rwkv_shift_mu"][l]
        v[:, l, 16:28] = fm(mu[0:1536])
        v[:64, l, 28] = mu[1536:1600]
        v[:64, l, 29] = mu[1600:1664]
        v[:, l, 30] = mu[1664:1792]
        v[:, l, 31:35] = fm(inp["rwkv_k_k"][l])
        v[:, l, 35:39] = fm(inp["rwkv_k_a"][l])
        v[:, l, 39:43] = fm(inp["rwkv_r_k"][l].reshape(-1))
        v[:, l, 43] = inp["attn_q_norm_g"][l][idx % 64]
        v[:, l, 44] = inp["attn_k_norm_g"][l][idx % 64]
    return v


def pack_small(inp, L):
    lw = np.zeros((128, L, 3, 512), np.float32)
    rows = np.zeros((L, 4, 512), np.float32)
    for l in range(L):
        lw[:64, l, 0] = inp["rwkv_w2"][l]
        lw[:64, l, 1] = inp["rwkv_a2"][l]
        lw[:, l, 2] = inp["rwkv_g2"][l]
        rows[l, 0] = inp["rwkv_w0"][l]
        rows[l, 1] = inp["rwkv_a0"][l]
        rows[l, 2] = inp["rwkv_lnx_g"][l]
        rows[l, 3] = inp["rwkv_lnx_b"][l]
    sinks = np.asarray(inp["attn_sinks"], np.float32)[:L].reshape(L, 8)
    return lw, rows, sinks


def build(NSEQ, T, L, debug=False):
    nc = bass.Bass("TRN2", target_bir_lowering=False)
    NTB = T // TB
    NT = NSEQ * T
    xT_d = nc.dram_tensor("xT", [D, NT], F32, kind="ExternalInput").ap()
    wpk_d = nc.dram_tensor("wpk", [L, NPIECE, 128, PW], F32, kind="ExternalInput").ap()
    vec_d = nc.dram_tensor("vecs", [128, L * NVEC], F32, kind="ExternalInput").ap()
    lw_d = nc.dram_tensor("lw", [128, L * 1536], F32, kind="ExternalInput").ap()
    rows_d = nc.dram_tensor("rows", [L, 2048], F32, kind="ExternalInput").ap()
    sink_d = nc.dram_tensor("sinks", [1, L * 8], F32, kind="ExternalInput").ap()
    cf_d = nc.dram_tensor("cf", [128, CF_W], F32, kind="ExternalInput").ap()
    cb_d = nc.dram_tensor("cb", [128, CB_W], BF16, kind="ExternalInput").ap()
    rot_d = nc.dram_tensor("rot", [128, 2 * T], F32, kind="ExternalInput").ap()
    yT_d = nc.dram_tensor("yT", [D, NT], F32, kind="ExternalOutput").ap()
    dbg_d = None
    if debug:
        dbg_d = nc.dram_tensor("dbg", [3, 64, 8 * TB], BF16, kind="ExternalOutput").ap()

    es = ExitStack()
    with es:
        P = Prog(nc, es)

        def sb(name, shape, dt=F32):
            return Tl(es.enter_context(nc.sbuf_tensor("s_" + name, list(shape), dt)), shape, name=name)

        def view(base, name, shape, dt, col0_bytes):
            raise NotImplementedError

        rvtm = sb("rvtm", [128, 4, 512], BF16)
        cf = sb("cf", [128, CF_W])
        cb = sb("cb", [128, CB_W], BF16)
        rotc = sb("rotb", [128, 2 * TB], BF16)
        vecs = sb("vecs", [128, L * NVEC])
        lwb = sb("lwb", [128, 1536], BF16)
        rowf = sb("rowf", [128, 1024])
        rv33 = sb("rv33", [64, 1024])
        HL = sb("HL", [64, 1024], BF16)
        sinkx = sb("sinkx", [128, L * 8])
        eps6 = sb("eps6", [128, 1])
        P.dma("sp", cf.t[:, :], cf_d, cf)
        P.dma("sp", cb.t[:, :], cb_d, cb)
        P.dma("sp", vecs.t[:, :], vec_d, vecs)
        P.dma("sp", sinkx.t[:, :], bass.AP(sink_d.tensor, 0, [[0, 128], [1, L * 8]]), sinkx)
        P.act(sinkx.t[:, :], sinkx.t[:, :], AF.Exp, [sinkx], [sinkx])
        P.memset(eps6.t[:, :], 1e-6, [eps6])
        P.memset(HL.t[:, :], 0.0, [HL])

        def cba(col, w, p0=0, npart=128):
            return cb.ap(p0, npart, col, [[1, w]])

        def vcol(l, j, p0=0, npart=128):
            return vecs.ap(p0, npart, l * NVEC + j, [[1, 1]])

        xT = sb("xT", [128, KD, TB])
        hT = sb("hT", [128, KD, TB], BF16)
        NW = 3
        wring = [sb("w%d" % i, [128, PW], BF16) for i in range(NW)]
        ps = [Tl(es.enter_context(nc.psum_tensor("ps%d" % i, [128, 512], F32)), [128, 512], name="ps%d" % i)
              for i in range(8)]
        psb = [Tl(p.t.bitcast(BF16), [128, 1024], buf=p.buf) for p in ps]
        pctr = [0]

        def psum():
            i = pctr[0] % 8
            pctr[0] += 1
            return ps[i], psb[i]

        oT = sb("oT", [64, 8, TB], BF16)
        B1 = sb("B1", [128, 4096], BF16)
        BIG = sb("BIG", [128, 3 * 4096], BF16)
        mixed = sb("mixed", [128, KD, TB], BF16)
        tmpA = sb("tmpA", [128, TB])
        tmpB = sb("tmpB", [128, TB])
        tmpD = sb("tmpD", [128, TB], BF16)
        rgtm = sb("rgtm", [128, 4, 512], BF16)
        vtm = sb("vtm", [128, 4, 128], BF16)
        PT = [sb("PT0", [128, 1024], BF16), None]
        ktm = sb("ktm", [128, 4, 512], BF16)
        sT = sb("sT", [128, 1024], BF16)
        PT[1] = sT
        rhi = sT
        rwsb = sb("rwsb", [128, TB + 1])
        wdx = sb("wdx", [64, 2 * TB], BF16)
        adx = sb("adx", [64, 2 * TB], BF16)
        gdx = sb("gdx", [128, 2 * TB], BF16)
        a_tm = sb("a_tm", [128, 512])
        sig = sb("sig", [128, 512])
        g_tm = sb("g_tm", [128, 512])
        E3 = sb("E3", [128, 512])
        E2 = sb("E2", [128, 512])
        Q1 = sb("Q1", [128, 512])
        nk = sb("nk", [128, 512])
        Ysb = nk
        rstd = tmpB
        X3 = sb("X3", [128, 512], BF16)
        X2 = sb("X2", [128, 512], BF16)
        UV = sb("UV", [128, 512], BF16)
        obt = sb("obt", [128, 512], BF16)
        octm = obt
        X3T = sb("X3T", [64, 8, 128], BF16)
        X1T = sb("X1T", [64, 8, 128], BF16)
        S_sb = sb("S_sb", [128, 8, 128], BF16)
        Apow = [sb("Apow%d" % i, [64, 8, 64], BF16) for i in range(2)]
        Npow = [sb("Npow%d" % i, [64, 8, 64], BF16) for i in range(2)]
        Xf = sb("Xf", [64, 8, 64])
        Xb = sb("Xb", [64, 8, 64], BF16)
        sm = sb("sm", [128, 64])
        st_k = [sb("stk%d" % l, [128, 2, 128], BF16) for l in range(L)]
        st_v = [sb("stv%d" % l, [128, 128], BF16) for l in range(L)]
        st_R = [sb("stR%d" % l, [128, 4, 64]) for l in range(L)]
        st_Rb = [sb("stRb%d" % l, [128, 8, 64], BF16) for l in range(L)]
        st_H = [sb("stH%d" % l, [64, 8, 64]) for l in range(L)]
        st_Hb = [sb("stHb%d" % l, [64, 8, 64], BF16) for l in range(L)]
        st_sh = [sb("stsh%d" % l, [128, 16]) for l in range(L)]

        wq = {"i": 0}
        tbi = [0]

        def wpiece(l, j):
            tl = wring[wq["i"] % NW]
            wq["i"] += 1
            P.dma("pool", tl.ap(0, 128, 0, [[2048, 2], [1, 2048]]),
                  bass.AP(wpk_d.tensor, (l * NPIECE + j) * 128 * PW, [[PW, 128], [2048, 2], [1, 2048]]), tl)
            return tl

        def rmsnorm(l, gcol):
            sq = BIG
            P.act(sq.ap(0, 128, 0, [[1, KD * TB]]), xT.ap(0, 128, 0, [[1, KD * TB]]), AF.Square, [xT], [sq])
            pt, _ = psum()
            for k in range(KD):
                P.mm(pt.t[:, :], cba(CB_OND, 128), sq.ap(0, 128, k * TB, [[1, TB]]), k == 0, k == KD - 1, [cb, sq], [pt])
            P.act(rstd.t[:, :], pt.t[:, :], AF.Ln, [pt, eps6], [rstd], bias=eps6.t[:, 0:1])
            P.act(rstd.t[:, :], rstd.t[:, :], AF.Exp, [rstd], [rstd], scale=-0.5)
            for k in range(KD):
                P.stt(hT.t[:, k, :], xT.t[:, k, :], vcol(l, gcol + k), rstd.t[:, :], OP.mult, OP.mult,
                      [xT, vecs, rstd], [hT])

        def proj_fm(w, ncol, cb_fn, src=None, M=128):
            src = src or hT
            for m in range(ncol // M):
                pt, ptb = psum()
                for k in range(KD):
                    P.mm(pt.ap(0, M, 0, [[1, TB]]), w.ap(0, 128, k * ncol + m * M, [[1, M]]),
                         src.t[:, k, :], k == 0, k == KD - 1, [w, src], [pt])
                cb_fn(m, pt)

        def headnorm(pt, dst_ap, gcolap, dstT):
            P.act(tmpD.t[:, :], pt.t[:, :], AF.Square, [pt], [tmpD])
            p2, _ = psum()
            P.mm(p2.t[:, :], cba(CB_BLK, 128), tmpD.t[:, :], True, True, [cb, tmpD], [p2])
            P.act(tmpA.t[:, :], p2.t[:, :], AF.Ln, [p2, eps6], [tmpA], bias=eps6.t[:, 0:1])
            P.act(tmpA.t[:, :], tmpA.t[:, :], AF.Exp, [tmpA], [tmpA], scale=-0.5)
            P.stt(dst_ap, pt.t[:, :], gcolap, tmpA.t[:, :], OP.mult, OP.mult, [pt, vecs, tmpA], [dstT])

        def merge_branch(l, b):
            for hf in range(2):
                wo = wpiece(l, 10 + b * 4 + hf * 2)
                wg = wpiece(l, 11 + b * 4 + hf * 2)
                for m in range(4):
                    pg, _ = psum()
                    for k in range(KD):
                        P.mm(pg.t[:, :], wg.ap(0, 128, k * 512 + m * 128, [[1, 128]]), hT.t[:, k, :],
                             k == 0, k == KD - 1, [wg, hT], [pg])
                    po, _ = psum()
                    for h in range(8):
                        P.mm(po.t[:, :], wo.ap(0, 64, h * 512 + m * 128, [[1, 128]]), oT.t[:, h, :],
                             h == 0, h == 7, [wo, oT], [po])
                    P.act(tmpA.t[:, :], pg.t[:, :], AF.Sigmoid, [pg], [tmpA])
                    mi = hf * 4 + m
                    if b == 0:
                        P.tt(mixed.t[:, mi, :], po.t[:, :], tmpA.t[:, :], OP.mult, [po, tmpA], [mixed])
                    else:
                        P.tt(tmpB.t[:, :], po.t[:, :], tmpA.t[:, :], OP.mult, [po, tmpA], [tmpB])
                        P.tt(mixed.t[:, mi, :], mixed.t[:, mi, :], tmpB.t[:, :], OP.add, [mixed, tmpB], [mixed])
            if debug:
                P.dma("sp", dbg_d[b], oT.ap(0, 64, 0, [[1, 8 * TB]]), oT, load=False)

        class _Stop(Exception):
            pass

        def stage(i):
            import os as _os
            if int(_os.environ.get("KSTOP", "99")) < i:
                raise _Stop()

        def attention(l, first):
            try:
                attention_(l, first)
            except _Stop:
                pass

        def attention_(l, first):
            import os as _os
            if "KSTOP" in _os.environ:
                P.memset(oT.ap(0, 64, 0, [[1, 8 * TB]]), 0.0, [oT])
            w = wpiece(l, 0)
            proj_fm(w, 512, lambda m, pt: headnorm(pt, B1.ap(0, 128, m * 512, [[1, 512]]), vcol(l, 43), B1))
            stage(2)
            w = wpiece(l, 1)
            for m in range(2):
                pt, _ = psum()
                for k in range(KD):
                    P.mm(pt.t[:, :], w.ap(0, 128, k * 384 + m * 128, [[1, 128]]), hT.t[:, k, :],
                         k == 0, k == KD - 1, [w, hT], [pt])
                headnorm(pt, B1.ap(0, 128, 2048 + m * 512, [[1, 512]]), vcol(l, 44), B1)
            stage(3)
            for n in range(4):
                pt, _ = psum()
                for k in range(KD):
                    P.mm(pt.ap(0, 128, 0, [[1, 128]]), hT.t[:, k, n * 128:(n + 1) * 128],
                         w.ap(0, 128, k * 384 + 256, [[1, 128]]), k == 0, k == KD - 1, [w, hT], [pt])
                P.copy(vtm.t[:, n, :], pt.t[:, 0:128], [pt], [vtm], eng="act")
            stage(4)
            for n in range(4):
                blocks = []
                if not (first and n == 0):
                    blocks.append(0)
                blocks.append(1)
                pts = {}
                for jb in blocks:
                    pa, _ = psum()
                    pb, _ = psum()
                    for h in range(8):
                        g = h // 4
                        base = (h % 2) * 64
                        if jb == 1:
                            kap = B1.ap(base, 64, 2048 + g * 512 + n * 128, [[1, 128]])
                            kr = [B1]
                        elif n == 0:
                            kap = st_k[l].ap(base, 64, g * 128, [[1, 128]])
                            kr = [st_k[l]]
                        else:
                            kap = B1.ap(base, 64, 2048 + g * 512 + (n - 1) * 128, [[1, 128]])
                            kr = [B1]
                        qap = B1.ap(base, 64, (h // 2) * 512 + n * 128, [[1, 128]])
                        pt = pa if h % 2 == 0 else pb
                        P.mm(pt.ap(0, 128, (h // 2) * 128, [[1, 128]]), kap, qap, True, True,
                             kr + [B1], [pt], signal=(h // 2 == 3))
                    pts[jb] = (pa, pb)
                stage(5)
                for jb in blocks:
                    for par, pt in enumerate(pts[jb]):
                        P.act(PT[jb].ap(0, 128, par * 128, [[256, 4], [1, 128]]), pt.ap(0, 128, 0, [[128, 4], [1, 128]]),
                              AF.Exp, [pt], [PT[jb]], scale=0.125)
                    stage(6)
                    mcol = CB_MP if jb == 0 else CB_MD
                    P.tt(PT[jb].ap(0, 128, 0, [[128, 8], [1, 128]]), PT[jb].ap(0, 128, 0, [[128, 8], [1, 128]]),
                         cb.ap(0, 128, mcol, [[0, 8], [1, 128]]), OP.mult, [PT[jb], cb], [PT[jb]])
                stage(7)
                for g in range(2):
                    po, _ = psum()
                    pd, _ = psum()
                    for bi, jb in enumerate(blocks):
                        if jb == 1:
                            vap = vtm.ap(0, 128, n * 128 + g * 64, [[1, 64]])
                            vr = [vtm]
                        elif n == 0:
                            vap = st_v[l].ap(0, 128, g * 64, [[1, 64]])
                            vr = [st_v[l]]
                        else:
                            vap = vtm.ap(0, 128, (n - 1) * 128 + g * 64, [[1, 64]])
                            vr = [vtm]
                        rhs = PT[jb].ap(0, 128, g * 512, [[1, 512]])
                        P.mm(po.ap(0, 64, 0, [[1, 512]]), vap, rhs, bi == 0, bi == len(blocks) - 1, vr + [PT[jb]], [po])
                        P.mm(pd.ap(0, 64, 0, [[1, 512]]), cba(CB_ONES, 64), rhs, bi == 0, bi == len(blocks) - 1,
                             [cb, PT[jb]], [pd])
                    stage(8)
                    P.tt(tmpA.ap(0, 64, 0, [[128, 4], [1, 128]]), pd.ap(0, 64, 0, [[128, 4], [1, 128]]),
                         sinkx.ap(0, 64, l * 8 + g * 4, [[1, 4], [0, 128]]), OP.add, [pd, sinkx], [tmpA])
                    P.act(tmpA.ap(0, 64, 0, [[1, 512]]), tmpA.ap(0, 64, 0, [[1, 512]]), AF.Ln, [tmpA], [tmpA])
                    P.act(tmpA.ap(0, 64, 0, [[1, 512]]), tmpA.ap(0, 64, 0, [[1, 512]]), AF.Exp, [tmpA], [tmpA], scale=-1.0)
                    P.tt(oT.ap(0, 64, g * 4 * TB + n * 128, [[TB, 4], [1, 128]]),
                         po.ap(0, 64, 0, [[128, 4], [1, 128]]), tmpA.ap(0, 64, 0, [[128, 4], [1, 128]]),
                         OP.mult, [po, tmpA], [oT])
            for g in range(2):
                P.copy(st_k[l].t[:, g, :], B1.ap(0, 128, 2048 + g * 512 + 384, [[1, 128]]), [B1], [st_k[l]], eng="act")
            P.copy(st_v[l].t[:, :], vtm.t[:, 3, :], [vtm], [st_v[l]], eng="act")

        def retention(l):
            try:
                retention_(l)
            except _Stop:
                pass

        def retention_(l):
            import os as _os
            if "KSTOP" in _os.environ:
                P.memset(oT.ap(0, 64, 0, [[1, 8 * TB]]), 0.0, [oT])

            def rotary(pt, dst_ap, fac_col, m):
                P.copy(tmpD.t[:, :], pt.t[:, :], [pt], [tmpD], eng="act")
                p2, _ = psum()
                P.mm(p2.t[:, :], cba(CB_PERM, 128), tmpD.t[:, :], True, True, [cb, tmpD], [p2])
                import os as _os
                KR = _os.environ.get("KROT", "")
                if KR == "1":
                    P.copy(tmpA.t[:, :], p2.t[:, :], [p2], [tmpA])
                    P.copy(dst_ap, tmpA.ap(0, 128, 0, [[128, 4], [1, 128]]), [tmpA], [B1])
                    return
                if KR == "3":
                    P.tt(tmpA.t[:, :], pt.t[:, :], cf.ap(0, 128, 0, [[1, 512]]), OP.mult, [pt, cf], [tmpA])
                    P.tt(tmpB.t[:, :], p2.t[:, :], cf.ap(0, 128, 512, [[1, 512]]), OP.mult, [p2, cf], [tmpB])
                else:
                    P.copy(E3.t[:, :], pt.t[:, :], [pt], [E3], eng="act")
                    P.tt(tmpA.t[:, :], E3.t[:, :], rotc.ap(0, 128, 0, [[1, TB]]), OP.mult, [E3, rotc], [tmpA])
                    P.tt(tmpB.t[:, :], p2.t[:, :], rotc.ap(0, 128, TB, [[1, TB]]), OP.mult, [p2, rotc], [tmpB])
                P.tt(tmpA.t[:, :], tmpA.t[:, :], tmpB.t[:, :], OP.add, [tmpA, tmpB], [tmpA])
                if KR == "2":
                    P.copy(dst_ap, tmpA.ap(0, 128, 0, [[128, 4], [1, 128]]), [tmpA], [B1])
                    return
                P.tt(dst_ap, tmpA.ap(0, 128, 0, [[128, 4], [1, 128]]),
                     cf.ap(0, 128, fac_col + m * 128, [[0, 4], [1, 128]]), OP.mult, [tmpA, cf], [B1])

            w = wpiece(l, 2)
            proj_fm(w, 512, lambda m, pt: rotary(pt, B1.ap(0, 128, m * 512, [[128, 4], [1, 128]]), CF_XI, m))
            stage(10)
            w = wpiece(l, 3)
            proj_fm(w, 512, lambda m, pt: rotary(pt, B1.ap(0, 128, 2048 + m * 512, [[128, 4], [1, 128]]), CF_KF, m))
            stage(11)
            for n in range(4):
                _, ptb = psum()
                for m in range(4):
                    P.tr(ptb.ap(0, 128, m * 128, [[1, 128]]), B1.ap(0, 128, 2048 + m * 512 + n * 128, [[1, 128]]),
                         cba(CB_IDENT, 128), [B1, cb], [ptb], signal=(m == 3))
                P.copy(ktm.t[:, n, :], ptb.t[:, 0:512], [ptb], [ktm], eng="act")
            stage(12)
            w = wpiece(l, 4)
            for n in range(4):
                pt, _ = psum()
                for k in range(KD):
                    P.mm(pt.t[:, :], hT.t[:, k, n * 128:(n + 1) * 128], w.ap(0, 128, k * 512, [[1, 512]]),
                         k == 0, k == KD - 1, [w, hT], [pt])
                P.copy(rvtm.t[:, n, :], pt.t[:, :], [pt], [rvtm], eng="act")
            w = wpiece(l, 5)
            for n in range(4):
                pt, _ = psum()
                for k in range(KD):
                    P.mm(pt.t[:, :], hT.t[:, k, n * 128:(n + 1) * 128], w.ap(0, 128, k * 512, [[1, 512]]),
                         k == 0, k == KD - 1, [w, hT], [pt])
                P.act(rgtm.t[:, n, :], pt.t[:, :], AF.Silu, [pt], [rgtm])
            stage(13)
            for n in range(4):
                pa, _ = psum()
                pb, _ = psum()
                for h in range(8):
                    base = (h % 2) * 64
                    kap = B1.ap(base, 64, 2048 + (h // 2) * 512 + n * 128, [[1, 128]])
                    qap = B1.ap(base, 64, (h // 2) * 512 + n * 128, [[1, 128]])
                    pt = pa if h % 2 == 0 else pb
                    P.mm(pt.ap(0, 128, (h // 2) * 128, [[1, 128]]), kap, qap, True, True, [B1], [pt], signal=(h // 2 == 3))
                for par, pt in enumerate((pa, pb)):
                    P.act(sT.ap(0, 128, par * 128, [[256, 4], [1, 128]]), pt.ap(0, 128, 0, [[128, 4], [1, 128]]),
                          AF.Copy, [pt], [sT])
                P.tt(sT.ap(0, 128, 0, [[128, 8], [1, 128]]), sT.ap(0, 128, 0, [[128, 8], [1, 128]]),
                     cb.ap(0, 128, CB_MD, [[0, 8], [1, 128]]), OP.mult, [sT, cb], [sT])
                stage(14)
                po, _ = psum()
                for h in range(8):
                    o_ap = po.ap(0, 128, h * 64, [[1, 64]])
                    P.mm(o_ap, sT.ap(0, 128, h * 128, [[1, 128]]), rvtm.ap(0, 128, n * 512 + h * 64, [[1, 64]]),
                         True, False, [rvtm, sT], [po], signal=False)
                    P.mm(o_ap, B1.ap(0, 128, (h // 2) * 512 + n * 128, [[1, 128]]), st_Rb[l].ap(0, 128, h * 64, [[1, 64]]),
                         False, True, [st_Rb[l], B1], [po], signal=(h == 7))
                stage(15)
                pk0, _ = psum()
                pk1, _ = psum()
                for h in range(8):
                    base = (h % 2) * 64
                    pk = pk0 if h % 2 == 0 else pk1
                    P.mm(pk.ap(base, 64, (h // 2) * 64, [[1, 64]]), ktm.ap(0, 128, n * 512 + h * 64, [[1, 64]]),
                         rvtm.ap(0, 128, n * 512 + h * 64, [[1, 64]]), True, True, [ktm, rvtm], [pk], signal=(h >= 6))
                P.tt(st_R[l].ap(0, 64, 0, [[64, 4], [1, 64]]), st_R[l].ap(0, 64, 0, [[64, 4], [1, 64]]),
                     pk0.ap(0, 64, 0, [[64, 4], [1, 64]]), OP.add, [st_R[l], pk0], [st_R[l]])
                P.tt(st_R[l].ap(64, 64, 0, [[64, 4], [1, 64]]), st_R[l].ap(64, 64, 0, [[64, 4], [1, 64]]),
                     pk1.ap(64, 64, 0, [[64, 4], [1, 64]]), OP.add, [st_R[l], pk1], [st_R[l]])
                P.tt(st_R[l].t[:, :, :], st_R[l].t[:, :, :], cf.ap(0, 128, CF_GC, [[1, 4], [0, 64]]), OP.mult,
                     [st_R[l], cf], [st_R[l]])
                stage(16)
                P.act(tmpA.t[:, :], po.t[:, :], AF.Square, [po], [tmpA])
                P.red(sm.ap(0, 128, 0, [[1, 8]]), tmpA.ap(0, 128, 0, [[64, 8], [1, 64]]), OP.add, [tmpA], [sm])
                P.ts(sm.ap(0, 128, 0, [[1, 8]]), sm.ap(0, 128, 0, [[1, 8]]), 1.0 / 64, OP.mult, [sm], [sm], s2=1e-6, op1=OP.add)
                P.act(sm.ap(0, 128, 0, [[1, 8]]), sm.ap(0, 128, 0, [[1, 8]]), AF.Ln, [sm], [sm])
                P.act(sm.ap(0, 128, 0, [[1, 8]]), sm.ap(0, 128, 0, [[1, 8]]), AF.Exp, [sm], [sm], scale=-0.5)
                P.tt(tmpB.ap(0, 128, 0, [[64, 8], [1, 64]]), po.ap(0, 128, 0, [[64, 8], [1, 64]]),
                     sm.ap(0, 128, 0, [[1, 8], [0, 64]]), OP.mult, [po, sm], [tmpB])
                P.tt(octm.t[:, :], tmpB.t[:, :], rgtm.t[:, n, :], OP.mult, [tmpB, rgtm], [octm])
                _, pto = psum()
                for h in range(8):
                    P.tr(pto.ap(0, 64, h * 128, [[1, 128]]), octm.ap(0, 128, h * 64, [[1, 64]]), cba(CB_IDENT, 128),
                         [octm, cb], [pto], signal=(h == 7))
                P.copy(oT.ap(0, 64, n * 128, [[TB, 8], [1, 128]]), pto.ap(0, 64, 0, [[128, 8], [1, 128]]), [pto], [oT])
                P.copy(st_Rb[l].ap(0, 64, 0, [[128, 4], [1, 64]]), st_R[l].ap(0, 64, 0, [[64, 4], [1, 64]]),
                       [st_R[l]], [st_Rb[l]], eng="act")
                P.copy(st_Rb[l].ap(64, 64, 64, [[128, 4], [1, 64]]), st_R[l].ap(64, 64, 0, [[64, 4], [1, 64]]),
                       [st_R[l]], [st_Rb[l]], eng="act")

        def rwkv(l):
            ZR, ZK, ZV, KK, KA, RR = 0, 2048, 4096, 6144, 8192, 10240

            def shifted(pt, M, j, mucol, out_ap, outT):
                P.copy(rwsb.ap(0, M, 1, [[1, TB]]), pt.ap(0, M, 0, [[1, TB]]), [pt], [rwsb], eng="act")
                P.copy(rwsb.ap(0, M, 0, [[1, 1]]), st_sh[l].ap(0, M, j, [[1, 1]]), [st_sh[l]], [rwsb])
                P.copy(st_sh[l].ap(0, M, j, [[1, 1]]), rwsb.ap(0, M, TB, [[1, 1]]), [rwsb], [st_sh[l]])
                P.tt(tmpA.ap(0, M, 0, [[1, TB]]), rwsb.ap(0, M, 0, [[1, TB]]), rwsb.ap(0, M, 1, [[1, TB]]), OP.subtract,
                     [rwsb], [tmpA])
                P.stt(out_ap, tmpA.ap(0, M, 0, [[1, TB]]), vcol(l, mucol, 0, M), rwsb.ap(0, M, 1, [[1, TB]]),
                      OP.mult, OP.add, [tmpA, vecs, rwsb], [outT])

            for ti, zoff in enumerate((ZR, ZK, ZV)):
                w = wpiece(l, 6 + ti)
                proj_fm(w, 512, lambda m, pt, ti=ti, zoff=zoff: shifted(
                    pt, 128, ti * 4 + m, 16 + ti * 4 + m, BIG.ap(0, 128, zoff + m * 512, [[1, TB]]), BIG))
            for m in range(4):
                zk = BIG.ap(0, 128, ZK + m * 512, [[1, TB]])
                zr = BIG.ap(0, 128, ZR + m * 512, [[1, TB]])
                P.ts(BIG.ap(0, 128, KK + m * 512, [[1, TB]]), zk, vcol(l, 31 + m), OP.mult, [BIG, vecs], [BIG])
                P.ts(BIG.ap(0, 128, KA + m * 512, [[1, TB]]), zk, vcol(l, 35 + m), OP.mult, [BIG, vecs], [BIG])
                P.ts(BIG.ap(0, 128, RR + m * 512, [[1, TB]]), zr, vcol(l, 39 + m), OP.mult, [BIG, vecs], [BIG])
            w = wpiece(l, 9)
            for ji, (c0, M, dst, fn) in enumerate(((0, 64, wdx, AF.Tanh), (64, 64, adx, AF.Copy), (128, 128, gdx, AF.Sigmoid))):
                pt, _ = psum()
                for k in range(KD):
                    P.mm(pt.ap(0, M, 0, [[1, TB]]), w.ap(0, 128, k * 256 + c0, [[1, M]]), hT.t[:, k, :],
                         k == 0, k == KD - 1, [w, hT], [pt])
                shifted(pt, M, 12 + ji, 28 + ji, tmpB.ap(0, M, 0, [[1, TB]]), tmpB)
                for dup in range(2):
                    P.act(dst.ap(0, M, dup * 64, [[128, 8], [1, 64]]), tmpB.ap(0, M, 0, [[64, 8], [1, 64]]), fn, [tmpB], [dst])

            for ci in range(TB // 64):
                t0 = ci * 64
                pa_, _ = psum()
                P.mm(pa_.t[:, :], adx.ap(0, 64, 2 * t0, [[1, 128]]), lwb.ap(0, 64, 512, [[1, 512]]), True, False,
                     [adx, lwb], [pa_], signal=False)
                P.mm(pa_.t[:, :], cba(CB_ONES, 128, 0, 33), HL.ap(0, 33, 512, [[1, 512]]), False, True, [cb, HL], [pa_])
                P.act(a_tm.t[:, :], pa_.t[:, :], AF.Sigmoid, [pa_], [a_tm])
                pw_, _ = psum()
                P.mm(pw_.t[:, :], wdx.ap(0, 64, 2 * t0, [[1, 128]]), lwb.ap(0, 64, 0, [[1, 512]]), True, False,
                     [wdx, lwb], [pw_], signal=False)
                P.mm(pw_.t[:, :], cba(CB_ONES, 128, 0, 33), HL.ap(0, 33, 0, [[1, 512]]), False, True, [cb, HL], [pw_])
                P.act(sig.t[:, :], pw_.t[:, :], AF.Sigmoid, [pw_], [sig])
                pg_, _ = psum()
                P.mm(pg_.t[:, :], gdx.ap(0, 128, 2 * t0, [[1, 128]]), lwb.ap(0, 128, 1024, [[1, 512]]), True, True,
                     [gdx, lwb], [pg_])
                P.copy(g_tm.t[:, :], pg_.t[:, :], [pg_], [g_tm], eng="act")
                pc1, _ = psum()
                P.mm(pc1.t[:, :], cf.ap(0, 64, CF_TRI, [[1, 128]]), sig.ap(0, 64, 0, [[1, 512]]), True, True, [cf, sig], [pc1])
                P.act(E3.t[:, :], pc1.t[:, :], AF.Exp, [pc1], [E3])
                pc3, _ = psum()
                P.mm(pc3.t[:, :], cf.ap(0, 64, CF_TRI + 128, [[1, 128]]), sig.ap(0, 64, 0, [[1, 512]]), True, True,
                     [cf, sig], [pc3])
                P.act(E2.t[:, :], pc3.t[:, :], AF.Exp, [pc3], [E2])
                pgc, _ = psum()
                for h in range(8):
                    P.mm(pgc.ap(0, 64, h * 2, [[1, 2]]), sig.ap(0, 64, h * 64, [[1, 64]]), cf.ap(0, 64, CF_ONES, [[1, 2]]),
                         True, True, [sig, cf], [pgc], signal=(h == 7))
                P.act(sm.ap(0, 64, 16, [[1, 8]]), pgc.ap(0, 64, 0, [[2, 8]]), AF.Exp, [pgc], [sm])
                P.act(sm.ap(0, 64, 24, [[1, 8]]), pgc.ap(0, 64, 0, [[2, 8]]), AF.Exp, [pgc], [sm], scale=-1.0)
                tps = {}
                shared = None
                for name, off in (("kk", KK), ("k", ZK), ("ka", KA), ("r", ZR), ("rr", RR), ("v", ZV)):
                    if name == "k":
                        ptb = shared
                    else:
                        _, ptb = psum()
                    if name == "kk":
                        shared = ptb
                    p0 = 0 if name == "kk" else 64
                    for m in range(4):
                        P.tr(ptb.ap(p0, 64, m * 128, [[1, 128]]), BIG.ap(0, 128, off + m * 512 + t0, [[1, 64]]),
                             cba(CB_IDENT, 128), [BIG, cb], [ptb], signal=(m == 3))
                    tps[name] = ptb
                kkp = tps["kk"]
                P.act(tmpA.ap(0, 64, 0, [[1, 512]]), kkp.ap(0, 64, 0, [[1, 512]]), AF.Square, [kkp], [tmpA])
                P.red(sm.ap(0, 64, 0, [[1, 8]]), tmpA.ap(0, 64, 0, [[64, 8], [1, 64]]), OP.add, [tmpA], [sm])
                P.ts(sm.ap(0, 64, 8, [[1, 8]]), sm.ap(0, 64, 0, [[1, 8]]), 1e-19, OP.max, [sm], [sm])
                P.act(sm.ap(0, 64, 8, [[1, 8]]), sm.ap(0, 64, 8, [[1, 8]]), AF.Ln, [sm], [sm])
                P.act(sm.ap(0, 64, 8, [[1, 8]]), sm.ap(0, 64, 8, [[1, 8]]), AF.Exp, [sm], [sm], scale=-0.5)
                P.stt(nk.ap(0, 64, 0, [[64, 8], [1, 64]]), kkp.ap(0, 64, 0, [[64, 8], [1, 64]]), -1.0,
                      sm.ap(0, 64, 8, [[1, 8], [0, 64]]), OP.mult, OP.mult, [kkp, sm], [nk])
                P.tt(X3.ap(0, 64, 0, [[1, 512]]), nk.ap(0, 64, 0, [[1, 512]]), E3.ap(0, 64, 0, [[1, 512]]), OP.mult,
                     [nk, E3], [X3])
                P.stt(Q1.ap(0, 64, 0, [[1, 512]]), nk.ap(0, 64, 0, [[1, 512]]), -1.0, a_tm.ap(0, 64, 0, [[1, 512]]),
                      OP.mult, OP.mult, [nk, a_tm], [Q1])
                P.stt(tmpB.ap(64, 64, 0, [[1, 512]]), a_tm.ap(64, 64, 0, [[1, 512]]), 1.0, tps["ka"].ap(64, 64, 0, [[1, 512]]),
                      OP.subtract, OP.mult, [a_tm, tps["ka"]], [tmpB])
                P.tt(Q1.ap(64, 64, 0, [[1, 512]]), tmpB.ap(64, 64, 0, [[1, 512]]), tps["k"].ap(64, 64, 0, [[1, 512]]), OP.add,
                     [tmpB, tps["k"]], [Q1])
                P.tt(X3.ap(64, 64, 0, [[1, 512]]), tps["r"].ap(64, 64, 0, [[1, 512]]), E3.ap(64, 64, 0, [[1, 512]]), OP.mult,
                     [tps["r"], E3], [X3])
                P.copy(UV.ap(64, 64, 0, [[1, 512]]), tps["v"].ap(64, 64, 0, [[1, 512]]), [tps["v"]], [UV], eng="act")
                P.tt(tmpB.ap(64, 64, 0, [[1, 512]]), tps["rr"].ap(64, 64, 0, [[1, 512]]), Q1.ap(64, 64, 0, [[1, 512]]), OP.mult,
                     [tps["rr"], Q1], [tmpB])
                P.red(sm.ap(64, 64, 32, [[1, 8]]), tmpB.ap(64, 64, 0, [[64, 8], [1, 64]]), OP.add, [tmpB], [sm])
                P.tt(X2.t[:, :], Q1.t[:, :], E2.t[:, :], OP.mult, [Q1, E2], [X2])
                _, pt3 = psum()
                for h in range(8):
                    P.tr(pt3.ap(0, 64, h * 128, [[1, 128]]), X3.ap(0, 128, h * 64, [[1, 64]]), cba(CB_IDENT, 128),
                         [X3, cb], [pt3], signal=(h == 7))
                P.copy(X3T.ap(0, 64, 0, [[1, 1024]]), pt3.ap(0, 64, 0, [[1, 1024]]), [pt3], [X3T], eng="act")
                _, pt2 = psum()
                for h in range(8):
                    P.tr(pt2.ap(0, 64, h * 128, [[1, 128]]), X2.ap(0, 128, h * 64, [[1, 64]]), cba(CB_IDENT, 128),
                         [X2, cb], [pt2], signal=(h == 7))
                P.tt(X1T.ap(0, 64, 0, [[128, 8], [1, 128]]), pt2.ap(0, 64, 0, [[128, 8], [1, 128]]),
                     sm.ap(0, 64, 24, [[1, 8], [0, 128]]), OP.mult, [pt2, sm], [X1T])
                pS = [psum()[0], psum()[0]]
                for h in range(8):
                    pt = pS[h // 4]
                    P.mm(pt.ap(0, 128, (h % 4) * 128, [[1, 128]]), X1T.ap(0, 64, h * 128, [[1, 128]]),
                         X3T.ap(0, 64, h * 128, [[1, 128]]), True, True, [X1T, X3T], [pt], signal=(h % 4 == 3))
                for half in range(2):
                    P.tt(S_sb.ap(0, 128, half * 512, [[128, 4], [1, 128]]), pS[half].ap(0, 128, 0, [[128, 4], [1, 128]]),
                         cb.ap(0, 128, CB_M4, [[0, 4], [1, 128]]), OP.mult, [pS[half], cb], [S_sb])
                pA, _ = psum()
                for h in range(8):
                    P.mm(pA.ap(0, 64, h * 64, [[1, 64]]), X3T.ap(0, 64, h * 128, [[1, 64]]), X1T.ap(0, 64, h * 128, [[1, 64]]),
                         True, True, [X3T, X1T], [pA], signal=(h == 7))
                P.tt(Apow[0].ap(0, 64, 0, [[64, 8], [1, 64]]), pA.ap(0, 64, 0, [[64, 8], [1, 64]]),
                     cb.ap(0, 64, CB_MST, [[0, 8], [1, 64]]), OP.mult, [pA, cb], [Apow[0]])
                pX, _ = psum()
                pX2, _ = psum()
                for h in range(8):
                    P.mm(pX.ap(0, 64, h * 64, [[1, 64]]), X3T.ap(0, 64, h * 128, [[1, 64]]),
                         st_Hb[l].ap(0, 64, h * 64, [[1, 64]]), True, True, [X3T, st_Hb[l]], [pX], signal=(h == 7))
                for h in range(8):
                    P.mm(pX2.ap(0, 64, h * 64, [[1, 64]]), S_sb.ap(64, 64, h * 128, [[1, 64]]),
                         UV.ap(64, 64, h * 64, [[1, 64]]), True, True, [S_sb, UV], [pX2], signal=(h == 7))
                P.copy(Xf.ap(0, 64, 0, [[1, 512]]), pX.ap(0, 64, 0, [[1, 512]]), [pX], [Xf], eng="act")
                P.tt(Xb.ap(0, 64, 0, [[1, 512]]), Xf.ap(0, 64, 0, [[1, 512]]), pX2.ap(0, 64, 0, [[1, 512]]), OP.add,
                     [Xf, pX2], [Xb])
                P.tt(Xf.ap(0, 64, 0, [[1, 512]]), Xf.ap(0, 64, 0, [[1, 512]]), pX2.ap(0, 64, 0, [[1, 512]]), OP.add,
                     [Xf, pX2], [Xf])
                for i in range(6):
                    if i == 0:
                        def Nap(h, c0=0, w=64):
                            return S_sb.ap(0, 64, h * 128 + c0, [[1, w]])
                        Nt = S_sb
                    else:
                        def Nap(h, c0=0, w=64, i=i):
                            return Npow[i % 2].ap(0, 64, h * 64 + c0, [[1, w]])
                        Nt = Npow[i % 2]
                    At = Apow[i % 2]
                    pY, _ = psum()
                    for h in range(8):
                        P.mm(pY.ap(0, 64, h * 64, [[1, 64]]), Nap(h), Xb.ap(0, 64, h * 64, [[1, 64]]), True, True,
                             [Nt, Xb], [pY], signal=(h == 7))
                    if i < 5:
                        P.tt(Xb.ap(0, 64, 0, [[1, 512]]), Xf.ap(0, 64, 0, [[1, 512]]), pY.ap(0, 64, 0, [[1, 512]]), OP.add,
                             [Xf, pY], [Xb])
                        P.tt(Xf.ap(0, 64, 0, [[1, 512]]), Xf.ap(0, 64, 0, [[1, 512]]), pY.ap(0, 64, 0, [[1, 512]]), OP.add,
                             [Xf, pY], [Xf])
                        pN, _ = psum()
                        pA2, _ = psum()
                        for h in range(8):
                            P.mm(pN.ap(0, 64, h * 64, [[1, 64]]), At.ap(0, 64, h * 64, [[1, 64]]), Nap(h), True, True,
                                 [At, Nt], [pN], signal=(h == 7))
                        for h in range(8):
                            P.mm(pA2.ap(0, 64, h * 64, [[1, 64]]), Nap(h), At.ap(0, 64, h * 64, [[1, 64]]), True, True,
                                 [At, Nt], [pA2], signal=(h == 7))
                        P.copy(Npow[(i + 1) % 2].ap(0, 64, 0, [[1, 512]]), pN.ap(0, 64, 0, [[1, 512]]), [pN],
                               [Npow[(i + 1) % 2]], eng="act")
                        P.copy(Apow[(i + 1) % 2].ap(0, 64, 0, [[1, 512]]), pA2.ap(0, 64, 0, [[1, 512]]), [pA2],
                               [Apow[(i + 1) % 2]])
                    else:
                        P.tt(UV.ap(0, 64, 0, [[1, 512]]), Xf.ap(0, 64, 0, [[1, 512]]), pY.ap(0, 64, 0, [[1, 512]]), OP.add,
                             [Xf, pY], [UV])
                pYo, _ = psum()
                for h in range(8):
                    o_ap = pYo.ap(64, 64, h * 64, [[1, 64]])
                    P.mm(o_ap, X3T.ap(0, 64, h * 128 + 64, [[1, 64]]), st_Hb[l].ap(0, 64, h * 64, [[1, 64]]), True, False,
                         [X3T, st_Hb[l]], [pYo], signal=False)
                    P.mm(o_ap, S_sb.ap(0, 128, h * 128 + 64, [[1, 64]]), UV.ap(0, 128, h * 64, [[1, 64]]), False, True,
                         [S_sb, UV], [pYo], signal=(h == 7))
                pH, _ = psum()
                for h in range(8):
                    P.mm(pH.ap(0, 64, h * 64, [[1, 64]]), X2.ap(0, 128, h * 64, [[1, 64]]), UV.ap(0, 128, h * 64, [[1, 64]]),
                         True, True, [X2, UV], [pH], signal=(h == 7))
                P.tt(st_H[l].ap(0, 64, 0, [[64, 8], [1, 64]]), st_H[l].ap(0, 64, 0, [[64, 8], [1, 64]]),
                     sm.ap(0, 64, 16, [[1, 8], [0, 64]]), OP.mult, [st_H[l], sm], [st_H[l]])
                P.tt(st_H[l].ap(0, 64, 0, [[1, 512]]), st_H[l].ap(0, 64, 0, [[1, 512]]), pH.ap(0, 64, 0, [[1, 512]]), OP.add,
                     [st_H[l], pH], [st_H[l]])
                P.copy(st_Hb[l].ap(0, 64, 0, [[1, 512]]), st_H[l].ap(0, 64, 0, [[1, 512]]), [st_H[l]], [st_Hb[l]], eng="act")
                R64 = (64, 64)
                P.copy(Ysb.ap(64, 64, 0, [[1, 512]]), pYo.ap(64, 64, 0, [[1, 512]]), [pYo], [Ysb], eng="act")
                P.red(sm.ap(64, 64, 40, [[1, 8]]), Ysb.ap(64, 64, 0, [[64, 8], [1, 64]]), OP.add, [Ysb], [sm])
                P.act(tmpA.ap(64, 64, 0, [[1, 512]]), Ysb.ap(64, 64, 0, [[1, 512]]), AF.Square, [Ysb], [tmpA])
                P.red(sm.ap(64, 64, 48, [[1, 8]]), tmpA.ap(64, 64, 0, [[64, 8], [1, 64]]), OP.add, [tmpA], [sm])
                P.ts(sm.ap(64, 64, 40, [[1, 8]]), sm.ap(64, 64, 40, [[1, 8]]), 1.0 / 64, OP.mult, [sm], [sm])
                P.tt(sm.ap(64, 64, 56, [[1, 8]]), sm.ap(64, 64, 40, [[1, 8]]), sm.ap(64, 64, 40, [[1, 8]]), OP.mult, [sm], [sm])
                P.stt(sm.ap(64, 64, 48, [[1, 8]]), sm.ap(64, 64, 48, [[1, 8]]), 1.0 / 64, sm.ap(64, 64, 56, [[1, 8]]),
                      OP.mult, OP.subtract, [sm], [sm])
                P.ts(sm.ap(64, 64, 48, [[1, 8]]), sm.ap(64, 64, 48, [[1, 8]]), 64e-5, OP.add, [sm], [sm])
                P.act(sm.ap(64, 64, 48, [[1, 8]]), sm.ap(64, 64, 48, [[1, 8]]), AF.Ln, [sm], [sm])
                P.act(sm.ap(64, 64, 48, [[1, 8]]), sm.ap(64, 64, 48, [[1, 8]]), AF.Exp, [sm], [sm], scale=-0.5)
                Y3 = Ysb.ap(64, 64, 0, [[64, 8], [1, 64]])
                P.tt(Y3, Y3, sm.ap(64, 64, 40, [[1, 8], [0, 64]]), OP.subtract, [Ysb, sm], [Ysb])
                P.tt(Y3, Y3, sm.ap(64, 64, 48, [[1, 8], [0, 64]]), OP.mult, [Ysb, sm], [Ysb])
                Y2 = Ysb.ap(64, 64, 0, [[1, 512]])
                P.tt(Y2, Y2, rowf.ap(64, 64, 0, [[1, 512]]), OP.mult, [Ysb, rowf], [Ysb])
                P.tt(Y2, Y2, rowf.ap(64, 64, 512, [[1, 512]]), OP.add, [Ysb, rowf], [Ysb])
                P.tt(tmpA.ap(64, 64, 0, [[64, 8], [1, 64]]), UV.ap(64, 64, 0, [[64, 8], [1, 64]]),
                     sm.ap(64, 64, 32, [[1, 8], [0, 64]]), OP.mult, [UV, sm], [tmpA])
                P.tt(Y2, Y2, tmpA.ap(64, 64, 0, [[1, 512]]), OP.add, [Ysb, tmpA], [Ysb])
                P.tt(obt.ap(64, 64, 0, [[1, 512]]), Y2, g_tm.ap(64, 64, 0, [[1, 512]]), OP.mult, [Ysb, g_tm], [obt])
                _, pto = psum()
                for h in range(8):
                    P.tr(pto.ap(0, 64, h * 64, [[1, 64]]), obt.ap(64, 64, h * 64, [[1, 64]]),
                         cb.ap(64, 64, CB_IDENT + 64, [[1, 64]]), [obt, cb], [pto], signal=(h == 7))
                P.copy(oT.ap(0, 64, t0, [[TB, 8], [1, 64]]), pto.ap(0, 64, 0, [[64, 8], [1, 64]]), [pto], [oT])

        for s in range(NSEQ):
            for tb in range(NTB):
                c0 = s * T + tb * TB
                first = (tb == 0)
                tbi[0] = tb
                for hh in range(2):
                    P.dma("pool", rotc.ap(0, 128, hh * TB, [[1, TB]]),
                          bass.AP(rot_d.tensor, hh * T + tb * TB, [[2 * T, 128], [1, TB]]), rotc)
                P.dma("sp", xT.t[:, :, :], bass.AP(xT_d.tensor, c0, [[NT, 128], [128 * NT, KD], [1, TB]]), xT)
                for l in range(L):
                    if first:
                        for stt_ in (st_R[l], st_Rb[l], st_H[l], st_Hb[l], st_sh[l]):
                            P.memset(stt_.ap(0, stt_.shape[0], 0, [[1, stt_.rs]]), 0.0, [stt_])
                    P.dma("sp", rowf.t[:, :], bass.AP(rows_d.tensor, l * 2048 + 1024, [[0, 128], [1, 1024]]), rowf)
                    P.dma("pool", lwb.t[:, :], lw_d[:, l * 1536:(l + 1) * 1536], lwb)
                    P.dma("sp", rv33.t[0:33, :], bass.AP(rows_d.tensor, l * 2048, [[0, 33], [1, 1024]]), rv33)
                    P.copy(rhi.t[0:33, :], rv33.t[0:33, :], [rv33], [rhi])
                    P.tt(rv33.t[0:33, :], rv33.t[0:33, :], rhi.t[0:33, :], OP.subtract, [rv33, rhi], [rv33])
                    P.copy(HL.t[0:1, :], rhi.t[0:1, :], [rhi], [HL])
                    P.copy(HL.t[32:33, :], rv33.t[32:33, :], [rv33], [HL])
                    rmsnorm(l, 0)
                    import os as _os
                    SK = _os.environ.get("KSKIP", "")
                    if "a" not in SK:
                        attention(l, first)
                        merge_branch(l, 0)
                    else:
                        P.memset(mixed.ap(0, 128, 0, [[1, KD * TB]]), 0.0, [mixed])
                        if "m" in SK:
                            P.memset(oT.ap(0, 64, 0, [[1, 8 * TB]]), 0.0, [oT])
                            merge_branch(l, 0)
                    if "c" not in SK:
                        retention(l)
                        merge_branch(l, 2)
                    if "b" not in SK:
                        rwkv(l)
                        merge_branch(l, 1)
                    for hf in range(2):
                        w = wpiece(l, 22 + hf)

                        def resid(m, pt, hf=hf):
                            mi = hf * 4 + m
                            P.tt(xT.t[:, mi, :], xT.t[:, mi, :], pt.t[:, :], OP.add, [xT, pt], [xT])
                        proj_fm(w, 512, resid, src=mixed)
                    rmsnorm(l, 8)
                    actT = BIG
                    for i in range(6):
                        ncol = 512 if i < 5 else 256
                        wg = wpiece(l, 24 + 2 * i)
                        wu = wpiece(l, 25 + 2 * i)
                        for m in range(ncol // 128):
                            pg, _ = psum()
                            pu, _ = psum()
                            for k in range(KD):
                                P.mm(pg.t[:, :], wg.ap(0, 128, k * ncol + m * 128, [[1, 128]]), hT.t[:, k, :],
                                     k == 0, k == KD - 1, [wg, hT], [pg])
                            for k in range(KD):
                                P.mm(pu.t[:, :], wu.ap(0, 128, k * ncol + m * 128, [[1, 128]]), hT.t[:, k, :],
                                     k == 0, k == KD - 1, [wu, hT], [pu])
                            P.act(tmpA.t[:, :], pg.t[:, :], AF.Silu, [pg], [tmpA])
                            fi = i * 4 + m
                            P.tt(actT.ap(0, 128, fi * 512, [[1, 512]]), pu.t[:, :], tmpA.t[:, :], OP.mult, [pu, tmpA], [actT])
                    for m in range(8):
                        w = wpiece(l, 36 + m)
                        pt, _ = psum()
                        for k in range(KF):
                            P.mm(pt.t[:, :], w.ap(0, 128, k * 128, [[1, 128]]), actT.ap(0, 128, k * 512, [[1, 512]]),
                                 k == 0, k == KF - 1, [w, actT], [pt])
                        P.tt(xT.t[:, m, :], xT.t[:, m, :], pt.t[:, :], OP.add, [xT, pt], [xT])
                P.dma("sp", bass.AP(yT_d.tensor, c0, [[NT, 128], [128 * NT, KD], [1, TB]]), xT.t[:, :, :], xT, load=False)
        with nc.Block() as block:
            P.finish(block, [xT, oT])
        P.stats = {e: len(P.ins[e]) for e in P.ENGS}
        nc._prog_stats = (P.stats, P.nwaits, P.ndsem)
        nc._tags = P.tags
        nc._P = P
    return nc


def prep_inputs(inp, x_cores, T, L):
    consts = make_consts(T)
    wpk = np.stack([pack_layer(inp, l) for l in range(L)])
    vecs = pack_vecs(inp, L).reshape(128, L * NVEC)
    lw, rows, sinks = pack_small(inp, L)
    shared = {"wpk": wpk, "vecs": np.ascontiguousarray(vecs), "lw": np.ascontiguousarray(lw.reshape(128, L * 1536)),
              "rows": np.ascontiguousarray(rows.reshape(L, 2048)), "sinks": np.ascontiguousarray(sinks.reshape(1, L * 8)),
              "cf": consts["cf"], "cb": consts["cb"], "rot": consts["rot"]}
    maps = []
    for xc in x_cores:
        xT = np.ascontiguousarray(xc.reshape(-1, D).T)
        m = dict(shared)
        m["xT"] = xT
        maps.append(m)
    return maps


def kernel(**inputs):
    inp = {k: np.asarray(v, dtype=np.float32) for k, v in inputs.items()}
    x = inp["x"]
    B, T, _ = x.shape
    L = inp["w_in"].shape[0]
    ncores = 8
    nseq = B // ncores
    nc = build(nseq, T, L)
    maps = prep_inputs(inp, [x[c * nseq:(c + 1) * nseq] for c in range(ncores)], T, L)
    res = run_bass_kernel_spmd(nc, maps, core_ids=list(range(ncores)))
    out = np.empty((B, T, D), np.float32)
    for c in range(ncores):
        yT = np.asarray(res.results[c]["yT"])
        out[c * nseq:(c + 1) * nseq] = yT.T.reshape(nseq, T, D)
    return out
```
